# Optimizing a Trainium2 kernel written in Bass

```python
import math
import jax, jax.numpy as jnp
from jax import lax
import numpy as np

D_MODEL = 1024
BATCH = 2
SEQ = 8192
DEPTH = 2

CHUNK = 64
N_META = 16
SSM_WIDTH = D_MODEL // 2
SSM_GROUP = 16
SSM_GROUPS = SSM_WIDTH // SSM_GROUP
SSM_STATE = 64
DT_MIN = 0.001
DT_MAX = 0.1
CONV_WIDTH = D_MODEL // 2
CONV_K = 3
ATTN_HEADS = 8
ATTN_HEAD_DIM = 64
ATTN_VALUE_DIM = 2 * ATTN_HEAD_DIM
ATTN_WIDTH = ATTN_HEADS * ATTN_VALUE_DIM
ROPE_THETA = 10000.0
Q_BLOCK = 128
N_BRANCH = 3
IN_WIDTH = SSM_WIDTH + 3 * CONV_WIDTH + 3 * ATTN_WIDTH
N_EXPERTS = 32
TOP_K = 4
D_FF = D_MODEL
SWIGLU_LIMIT = 7.0
SWIGLU_ALPHA = 1.702
DEEPNORM_ALPHA = (2.0 * DEPTH) ** 0.25
DEEPNORM_BETA = (8.0 * DEPTH) ** -0.25
LN_EPS = 1e-5
RMS_EPS = 1e-5
NEG_INF = -1e30

kernel_name = "hybrid_s5_shortconv_diffattn_moe_deepnorm"


def layer_norm(x, g, b):
    xf = x.astype(jnp.float32)
    mu = xf.mean(-1, keepdims=True)
    var = jnp.square(xf - mu).mean(-1, keepdims=True)
    return ((xf - mu) * lax.rsqrt(var + LN_EPS) * g.astype(jnp.float32) + b.astype(jnp.float32)).astype(x.dtype)


def rope_tables(length):
    pos = jnp.arange(length, dtype=jnp.float32)
    inv = ROPE_THETA ** (-jnp.arange(0, ATTN_HEAD_DIM, 2, dtype=jnp.float32) / ATTN_HEAD_DIM)
    ang = pos[:, None] * inv[None, :]
    ang = jnp.concatenate([ang, ang], axis=-1)
    return jnp.cos(ang), jnp.sin(ang)


def apply_rope(x, cos, sin):
    xf = x.astype(jnp.float32)
    half = ATTN_HEAD_DIM // 2
    rot = jnp.concatenate([-xf[..., half:], xf[..., :half]], axis=-1)
    c = cos[None, :, None, None, :]
    s = sin[None, :, None, None, :]
    return (xf * c + rot * s).astype(x.dtype)


def chunk_ids(length, padded_len):
    p = jnp.arange(padded_len)
    cid = jnp.where(p < N_META, 0, 1 + (p - N_META) // CHUNK)
    return jnp.where(p < length, cid, padded_len)


def _complex_affine_combine(e1, e2):
    a1r, a1i, b1r, b1i = e1
    a2r, a2i, b2r, b2i = e2
    ar = a2r * a1r - a2i * a1i
    ai = a2r * a1i + a2i * a1r
    br = a2r * b1r - a2i * b1i + b2r
    bi = a2r * b1i + a2i * b1r + b2i
    return (ar, ai, br, bi)


def s5_branch(u, lam_re, lam_im, log_dt, b_re, b_im, c_re, c_im, d_skip, w_glu):
    bsz, length, _ = u.shape
    f32 = jnp.float32
    uf = u.astype(f32).reshape(bsz, length, SSM_GROUPS, SSM_GROUP)
    lr = lam_re.astype(f32)
    li = lam_im.astype(f32)
    dt = jnp.exp(log_dt.astype(f32))[:, None]
    mag = jnp.exp(lr * dt)
    ar = mag * jnp.cos(li * dt)
    ai = mag * jnp.sin(li * dt)
    denom = lr * lr + li * li
    nr, ni = ar - 1.0, ai
    coef_r = (nr * lr + ni * li) / denom
    coef_i = (ni * lr - nr * li) / denom
    br, bi = b_re.astype(f32), b_im.astype(f32)
    bbar_r = coef_r[..., None] * br - coef_i[..., None] * bi
    bbar_i = coef_r[..., None] * bi + coef_i[..., None] * br
    bu_r = jnp.einsum('blgc,gpc->blgp', uf, bbar_r)
    bu_i = jnp.einsum('blgc,gpc->blgp', uf, bbar_i)
    a_r = jnp.broadcast_to(ar, bu_r.shape)
    a_i = jnp.broadcast_to(ai, bu_i.shape)
    _, _, xr, xi = lax.associative_scan(_complex_affine_combine, (a_r, a_i, bu_r, bu_i), axis=1)
    y = (jnp.einsum('gcp,blgp->blgc', c_re.astype(f32), xr)
         - jnp.einsum('gcp,blgp->blgc', c_im.astype(f32), xi)
         + d_skip.astype(f32).reshape(SSM_GROUPS, SSM_GROUP) * uf)
    y = jax.nn.gelu(y.reshape(bsz, length, SSM_WIDTH))
    y = y * jax.nn.sigmoid(y @ w_glu.astype(f32))
    return y.astype(u.dtype)


def short_conv_branch(gate_b, gate_c, h, conv_w):
    z = gate_c * h
    length = z.shape[1]
    zp = jnp.pad(z, ((0, 0), (CONV_K - 1, 0), (0, 0)))
    y = zp[:, 0:length] * conv_w[0]
    for j in range(1, CONV_K):
        y = y + zp[:, j:j + length] * conv_w[j]
    return gate_b * y


def diff_attention(q, k, v, lam, lam_init, subln_g):
    bsz, length = q.shape[0], q.shape[1]
    padded = -(-length // Q_BLOCK) * Q_BLOCK
    n_blocks = padded // Q_BLOCK
    pad = padded - length
    q = jnp.pad(q, ((0, 0), (0, pad), (0, 0), (0, 0), (0, 0)))
    k = jnp.pad(k, ((0, 0), (0, pad), (0, 0), (0, 0), (0, 0)))
    v = jnp.pad(v, ((0, 0), (0, pad), (0, 0), (0, 0)))
    cid = chunk_ids(length, padded)
    q_blocks = q.reshape(bsz, n_blocks, Q_BLOCK, ATTN_HEADS, 2, ATTN_HEAD_DIM).transpose(1, 0, 2, 3, 4, 5)
    cid_blocks = cid.reshape(n_blocks, Q_BLOCK)
    scale = ATTN_HEAD_DIM ** -0.5

    def one_block(args):
        qi, ci = args
        s = jnp.einsum('bqhcd,bkhcd->bchqk', qi, k).astype(jnp.float32) * scale
        mask = ci[:, None] >= cid[None, :]
        s = jnp.where(mask, s, NEG_INF)
        p = jax.nn.softmax(s, axis=-1)
        p = p[:, 0] - lam * p[:, 1]
        return jnp.einsum('bhqk,bkhe->bqhe', p.astype(v.dtype), v)

    o = lax.map(one_block, (q_blocks, cid_blocks))
    o = o.transpose(1, 0, 2, 3, 4).reshape(bsz, padded, ATTN_HEADS, ATTN_VALUE_DIM)[:, :length]
    of = o.astype(jnp.float32)
    of = of * lax.rsqrt(jnp.mean(of * of, axis=-1, keepdims=True) + RMS_EPS)
    of = of * subln_g.astype(jnp.float32) * (1.0 - lam_init)
    return of.reshape(bsz, length, ATTN_WIDTH).astype(q.dtype)


def moe_ffn(h, router_w, router_b, w_gu, b_gu, w_down, b_down):
    bsz, length, d = h.shape
    xt = h.reshape(bsz * length, d)
    logits = (xt @ router_w + router_b).astype(jnp.float32)
    top_v, top_i = lax.top_k(logits, TOP_K)
    top_w = jax.nn.softmax(top_v, axis=-1)
    combine = jnp.einsum('tk,tke->te', top_w, jax.nn.one_hot(top_i, N_EXPERTS, dtype=jnp.float32))

    def expert_step(acc, ex):
        wgu, bgu, wd, bd, cw = ex
        gu = xt @ wgu + bgu
        gate = jnp.minimum(gu[:, :D_FF], SWIGLU_LIMIT)
        up = jnp.clip(gu[:, D_FF:], -SWIGLU_LIMIT, SWIGLU_LIMIT)
        hid = (up + 1.0) * gate * jax.nn.sigmoid(SWIGLU_ALPHA * gate)
        out = hid @ wd + bd
        return acc + cw[:, None] * out.astype(jnp.float32), None

    acc0 = jnp.zeros((bsz * length, d), jnp.float32)
    out, _ = lax.scan(expert_step, acc0, (w_gu, b_gu, w_down, b_down, combine.T))
    return out.reshape(bsz, length, d).astype(h.dtype)


def setup_inputs(seed: int = 0) -> dict:
    key = jax.random.key(seed)
    ks = jax.random.split(key, 40)
    f32 = jnp.float32

    def nrm(k, shape, scale):
        return jax.random.normal(k, shape, f32) * scale

    D = D_MODEL
    col_scale = jnp.concatenate([jnp.ones((IN_WIDTH - ATTN_WIDTH,), f32),
                                 jnp.full((ATTN_WIDTH,), DEEPNORM_BETA, f32)])
    n = jnp.arange(SSM_STATE, dtype=f32)
    return {
        "x": nrm(ks[0], (BATCH, SEQ, D), 1.0),
        "meta_tokens": nrm(ks[1], (N_META, D), 1.0),
        "ln_in_g": 1.0 + nrm(ks[2], (D,), 0.01),
        "ln_in_b": nrm(ks[3], (D,), 0.01),
        "w_in": nrm(ks[4], (DEPTH, D, IN_WIDTH), D ** -0.5) * col_scale,
        "ssm_lambda_re": -0.5 + nrm(ks[5], (DEPTH, SSM_GROUPS, SSM_STATE), 0.01),
        "ssm_lambda_im": math.pi * n + nrm(ks[6], (DEPTH, SSM_GROUPS, SSM_STATE), 0.01),
        "ssm_log_dt": jax.random.uniform(ks[7], (DEPTH, SSM_GROUPS), f32, math.log(DT_MIN), math.log(DT_MAX)),
        "ssm_b_re": nrm(ks[8], (DEPTH, SSM_GROUPS, SSM_STATE, SSM_GROUP), (2 * SSM_GROUP) ** -0.5),
        "ssm_b_im": nrm(ks[9], (DEPTH, SSM_GROUPS, SSM_STATE, SSM_GROUP), (2 * SSM_GROUP) ** -0.5),
        "ssm_c_re": nrm(ks[10], (DEPTH, SSM_GROUPS, SSM_GROUP, SSM_STATE), (2 * SSM_STATE) ** -0.5),
        "ssm_c_im": nrm(ks[11], (DEPTH, SSM_GROUPS, SSM_GROUP, SSM_STATE), (2 * SSM_STATE) ** -0.5),
        "ssm_d": nrm(ks[12], (DEPTH, SSM_WIDTH), 1.0),
        "ssm_w_glu": nrm(ks[13], (DEPTH, SSM_WIDTH, SSM_WIDTH), SSM_WIDTH ** -0.5),
        "ssm_w_out": nrm(ks[14], (DEPTH, SSM_WIDTH, D), SSM_WIDTH ** -0.5),
        "conv_w": nrm(ks[15], (DEPTH, CONV_K, CONV_WIDTH), CONV_K ** -0.5),
        "conv_w_out": nrm(ks[16], (DEPTH, CONV_WIDTH, D), CONV_WIDTH ** -0.5),
        "attn_lambda_q1": nrm(ks[17], (DEPTH, ATTN_HEAD_DIM), 0.1),
        "attn_lambda_k1": nrm(ks[18], (DEPTH, ATTN_HEAD_DIM), 0.1),
        "attn_lambda_q2": nrm(ks[19], (DEPTH, ATTN_HEAD_DIM), 0.1),
        "attn_lambda_k2": nrm(ks[20], (DEPTH, ATTN_HEAD_DIM), 0.1),
        "attn_subln_g": 1.0 + nrm(ks[21], (DEPTH, ATTN_VALUE_DIM), 0.01),
        "attn_w_out": nrm(ks[22], (DEPTH, ATTN_WIDTH, D), ATTN_WIDTH ** -0.5),
        "gate_w": nrm(ks[23], (DEPTH, D, N_BRANCH * D), D ** -0.5),
        "gate_b": nrm(ks[24], (DEPTH, N_BRANCH * D), 0.01),
        "w_o": nrm(ks[25], (DEPTH, D, D), D ** -0.5 * DEEPNORM_BETA),
        "ln1_g": 1.0 + nrm(ks[26], (DEPTH, D), 0.01),
        "ln1_b": nrm(ks[27], (DEPTH, D), 0.01),
        "router_w": nrm(ks[28], (DEPTH, D, N_EXPERTS), D ** -0.5),
        "router_b": nrm(ks[29], (DEPTH, N_EXPERTS), 0.01),
        "expert_w_gu": nrm(ks[30], (DEPTH, N_EXPERTS, D, 2 * D_FF), D ** -0.5),
        "expert_b_gu": nrm(ks[31], (DEPTH, N_EXPERTS, 2 * D_FF), 0.01),
        "expert_w_down": nrm(ks[32], (DEPTH, N_EXPERTS, D_FF, D), D_FF ** -0.5 * DEEPNORM_BETA),
        "expert_b_down": nrm(ks[33], (DEPTH, N_EXPERTS, D), 0.01),
        "ln2_g": 1.0 + nrm(ks[34], (DEPTH, D), 0.01),
        "ln2_b": nrm(ks[35], (DEPTH, D), 0.01),
    }


def reference(x, meta_tokens, ln_in_g, ln_in_b, w_in, ssm_lambda_re, ssm_lambda_im, ssm_log_dt,
              ssm_b_re, ssm_b_im, ssm_c_re, ssm_c_im, ssm_d, ssm_w_glu, ssm_w_out, conv_w, conv_w_out,
              attn_lambda_q1, attn_lambda_k1, attn_lambda_q2, attn_lambda_k2, attn_subln_g, attn_w_out,
              gate_w, gate_b, w_o, ln1_g, ln1_b, router_w, router_b, expert_w_gu, expert_b_gu,
              expert_w_down, expert_b_down, ln2_g, ln2_b):
    bsz, seq, d = x.shape
    meta = jnp.broadcast_to(meta_tokens.astype(x.dtype)[None], (bsz, N_META, d))
    h = jnp.concatenate([meta, x], axis=1)
    length = h.shape[1]
    h = layer_norm(h, ln_in_g, ln_in_b)
    cos, sin = rope_tables(length)
    s0 = SSM_WIDTH
    s1 = s0 + CONV_WIDTH
    s2 = s1 + CONV_WIDTH
    s3 = s2 + CONV_WIDTH
    s4 = s3 + ATTN_WIDTH
    s5 = s4 + ATTN_WIDTH
    for l in range(DEPTH):
        proj = h @ w_in[l]
        u_ssm = proj[..., :s0]
        cb, cc, ch = proj[..., s0:s1], proj[..., s1:s2], proj[..., s2:s3]
        q = proj[..., s3:s4].reshape(bsz, length, ATTN_HEADS, 2, ATTN_HEAD_DIM)
        k = proj[..., s4:s5].reshape(bsz, length, ATTN_HEADS, 2, ATTN_HEAD_DIM)
        v = proj[..., s5:].reshape(bsz, length, ATTN_HEADS, ATTN_VALUE_DIM)
        y_ssm = s5_branch(u_ssm, ssm_lambda_re[l], ssm_lambda_im[l], ssm_log_dt[l], ssm_b_re[l], ssm_b_im[l],
                          ssm_c_re[l], ssm_c_im[l], ssm_d[l], ssm_w_glu[l]) @ ssm_w_out[l]
        y_conv = short_conv_branch(cb, cc, ch, conv_w[l]) @ conv_w_out[l]
        lam_init = 0.8 - 0.6 * math.exp(-0.3 * l)
        lam = (jnp.exp(jnp.sum(attn_lambda_q1[l].astype(jnp.float32) * attn_lambda_k1[l].astype(jnp.float32)))
               - jnp.exp(jnp.sum(attn_lambda_q2[l].astype(jnp.float32) * attn_lambda_k2[l].astype(jnp.float32)))
               + lam_init)
        y_attn = diff_attention(apply_rope(q, cos, sin), apply_rope(k, cos, sin), v, lam, lam_init,
                                attn_subln_g[l]) @ attn_w_out[l]
        g = jax.nn.sigmoid(h @ gate_w[l] + gate_b[l]).reshape(bsz, length, N_BRANCH, d)
        merged = g[:, :, 0] * y_ssm + g[:, :, 1] * y_conv + g[:, :, 2] * y_attn
        h = layer_norm(DEEPNORM_ALPHA * h + merged @ w_o[l], ln1_g[l], ln1_b[l])
        ffn = moe_ffn(h, router_w[l], router_b[l], expert_w_gu[l], expert_b_gu[l], expert_w_down[l], expert_b_down[l])
        h = layer_norm(DEEPNORM_ALPHA * h + ffn, ln2_g[l], ln2_b[l])
    return h[:, N_META:]
```

```python
import math
from contextlib import ExitStack

import numpy as np
import concourse.bass as bass
import concourse.mybir as mybir
from concourse.bass_utils import run_bass_kernel_spmd

F32 = mybir.dt.float32
BF16 = mybir.dt.bfloat16
I32 = mybir.dt.int32
AF = mybir.ActivationFunctionType
ALU = mybir.AluOpType
AX = mybir.AxisListType

D = 1024
BATCH = 2
SEQ = 8192
DEPTH = 2
N_META = 16
L = SEQ + N_META
LP = 8320
NBLK = LP // 128
SSM_W = 512
CONV_W = 512
HEADS = 8
HD = 64
VD = 128
ATT_W = 1024
IN_W = 5120
NE = 32
TOPK = 4
DFF = 1024
SW_LIMIT = 7.0
SW_ALPHA = 1.702
DN_ALPHA = (2.0 * DEPTH) ** 0.25
LN_EPS = 1e-5
RMS_EPS = 1e-5
ROPE_THETA = 10000.0
NCORES = 8
TOK_PER_CORE = (BATCH * L) // NCORES
TWO_PI = 2.0 * math.pi


STRICT_SAME_ENGINE = True


class Prog:
    ENGS = ("pe", "act", "dve", "pool", "sp")

    def __init__(self, nc):
        self.nc = nc
        self.es = ExitStack()
        self.ops = []
        self.pes = ExitStack()
        self.mes = ExitStack()
        self.nphase = 0

    def sbm(self, name, shape, dtype):
        return self.mes.enter_context(self.nc.sbuf_tensor("m%d_%s" % (self.nphase, name), list(shape), dtype, side="right"))

    def sb(self, name, shape, dtype):
        return self.pes.enter_context(self.nc.sbuf_tensor("p%d_%s" % (self.nphase, name), list(shape), dtype, side="left"))

    def ps(self, name, shape, dtype=F32):
        return self.pes.enter_context(self.nc.psum_tensor("p%d_%s" % (self.nphase, name), list(shape), dtype))

    def new_phase(self, keep_mid=False):
        self.pes.close()
        self.pes = ExitStack()
        if not keep_mid:
            self.mes.close()
            self.mes = ExitStack()
        self.nphase += 1
        self.ops.append(dict(barrier=True, eng=None, dma=False))

    def op(self, eng, fn, reads=(), writes=(), dma=False, semkey=None):
        assert eng in self.ENGS
        self.ops.append(dict(eng=eng, fn=fn, reads=tuple(reads), writes=tuple(writes), dma=dma, semkey=semkey))

    def dma(self, eng, out, in_, reads=(), writes=(), semkey=None):
        self.op(eng, lambda e: e.dma_start(out=out, in_=in_), reads, writes, dma=True, semkey=semkey)

    def emit(self, final_dram_writes=True):
        nc = self.nc
        ops = self.ops
        n = len(ops)
        last_writer = {}
        readers = {}
        deps = [dict() for _ in range(n)]
        last_on_eng = {}
        last_dma = {}
        pending = {}
        for i, o in enumerate(ops):
            if o.get("barrier"):
                bl = list(last_on_eng.values()) + list(last_dma.values())
                pending = {e: bl for e in self.ENGS}
                last_writer = {}
                readers = {}
                continue
            if pending.get(o["eng"]):
                for j in pending[o["eng"]]:
                    deps[i][j] = "raw"
                pending[o["eng"]] = None
            if o["dma"]:
                last_dma[(o["semkey"], o["writes"], o["reads"])] = i
            else:
                last_on_eng[o["eng"]] = i
            for r in o["reads"]:
                j = last_writer.get(r)
                if j is not None:
                    deps[i][j] = "raw"
            for w in o["writes"]:
                j = last_writer.get(w)
                if j is not None and deps[i].get(j) != "raw":
                    deps[i][j] = "war"
                for j in readers.get(w, ()):
                    if j != i and deps[i].get(j) != "raw":
                        deps[i][j] = "war"
            for r in o["reads"]:
                readers.setdefault(r, []).append(i)
            for w in o["writes"]:
                last_writer[w] = i
                readers[w] = []
        for i, o in enumerate(ops):
            if o.get("barrier"):
                continue
            for j in list(deps[i].keys()):
                p = ops[j]
                if p["dma"]:
                    continue
                if p["eng"] == o["eng"] and not o["dma"]:
                    if o["eng"] == "pe" or (deps[i][j] == "war" and not STRICT_SAME_ENGINE):
                        del deps[i][j]
        dma_keys = {}
        for i, o in enumerate(ops):
            if o.get("barrier"):
                continue
            if o["dma"]:
                k = o["semkey"]
                if k is None:
                    k = o["writes"][0] if (o["writes"] and not o["writes"][0].startswith("dram:")) else o["reads"][0]
                o["_key"] = k
                dma_keys.setdefault(k, 0)
                dma_keys[k] += 1
                o["_cnt"] = 16 * dma_keys[k]
        needed = set()
        for i in range(n):
            for j in deps[i]:
                if not ops[j]["dma"]:
                    needed.add(j)
        cnt = {e: 0 for e in self.ENGS}
        for i, o in enumerate(ops):
            if o.get("barrier"):
                continue
            if not o["dma"] and i in needed:
                cnt[o["eng"]] += 1
                o["_cnt"] = cnt[o["eng"]]
        sems = {}
        for e in self.ENGS:
            sems["eng:" + e] = self.es.enter_context(nc.semaphore("sem_" + e))
        for k in dma_keys:
            sems["dma:" + k] = self.es.enter_context(nc.semaphore("dsem_%d" % len(sems)))
        assert len(sems) <= 100, "too many semaphores: %d" % len(sems)
        per_eng = {e: [] for e in self.ENGS}
        for i, o in enumerate(ops):
            if not o.get("barrier"):
                per_eng[o["eng"]].append(i)
        final_waits = [("dma:" + k, 16 * v) for k, v in dma_keys.items()]

        def run_engine(ename, eobj):
            waited = {}
            for i in per_eng[ename]:
                o = ops[i]
                need = {}
                for j in deps[i]:
                    p = ops[j]
                    sk = ("dma:" + p["_key"]) if p["dma"] else ("eng:" + p["eng"])
                    need[sk] = max(need.get(sk, 0), p["_cnt"])
                for sk, v in need.items():
                    if waited.get(sk, 0) < v:
                        eobj.wait_ge(sems[sk], v)
                        waited[sk] = v
                ins = o["fn"](eobj)
                if o["dma"]:
                    ins.then_inc(sems["dma:" + o["_key"]], 16)
                elif i in needed:
                    ins.then_inc(sems["eng:" + ename], 1)
            if ename == "sp":
                for sk, v in final_waits:
                    if waited.get(sk, 0) < v:
                        eobj.wait_ge(sems[sk], v)

        with nc.Block() as block:
            @block.tensor
            def _(e):
                run_engine("pe", e)

            @block.scalar
            def _(e):
                run_engine("act", e)

            @block.vector
            def _(e):
                run_engine("dve", e)

            @block.gpsimd
            def _(e):
                run_engine("pool", e)

            @block.sync
            def _(e):
                run_engine("sp", e)
        self.pes.close()
        self.mes.close()
        self.es.close()


CH = 342
NCH = TOK_PER_CORE // CH


def col_layout(v):
    v = np.ascontiguousarray(v, dtype=np.float32)
    return np.ascontiguousarray(v.reshape(-1, 128).T)


def emit_layernorm(P, x32, xres, N, ones_f, g_col, b_col, out32, outres, tag, out16=None, out16res=None):
    ps_m, rm = P._ln_ps[0]
    ps_q, rq = P._ln_ps[1]
    sq = P._ln_sq
    mean = P._ln_mean
    rstd = P._ln_rstd
    tmp = P._ln_tmp
    for k in range(8):
        P.op("pe", lambda e, k=k: e.matmul(ps_m[:, :N], ones_f[:], x32[:, k, :N], start=(k == 0), stop=(k == 7)),
             reads=[xres, "ones_f"], writes=[rm])
    for k in range(8):
        sb_ = sq[k % 2]
        sr = "ln_sq%d" % (k % 2)
        P.op("act", lambda e, k=k, sb_=sb_: e.activation(out=sb_[:, :N], in_=x32[:, k, :N], func=AF.Square), reads=[xres], writes=[sr])
        P.op("pe", lambda e, k=k, sb_=sb_: e.matmul(ps_q[:, :N], ones_f[:], sb_[:, :N], start=(k == 0), stop=(k == 7)),
             reads=[sr, "ones_f"], writes=[rq])
    P.op("act", lambda e: e.copy(out=mean[:, :N], in_=ps_m[:, :N]), reads=[rm], writes=["ln_mean"])
    P.op("dve", lambda e: e.tensor_tensor(out=rstd[:, :N], in0=mean[:, :N], in1=mean[:, :N], op=ALU.mult), reads=["ln_mean"], writes=["ln_rstd"])
    P.op("dve", lambda e: e.tensor_tensor(out=rstd[:, :N], in0=ps_q[:, :N], in1=rstd[:, :N], op=ALU.subtract), reads=[rq, "ln_rstd"], writes=["ln_rstd"])
    P.op("dve", lambda e: e.tensor_scalar(out=rstd[:, :N], in0=rstd[:, :N], scalar1=LN_EPS, scalar2=None, op0=ALU.add), reads=["ln_rstd"], writes=["ln_rstd"])
    P.op("act", lambda e: e.sqrt(out=rstd[:, :N], in_=rstd[:, :N]), reads=["ln_rstd"], writes=["ln_rstd"])
    P.op("dve", lambda e: e.reciprocal(out=rstd[:, :N], in_=rstd[:, :N]), reads=["ln_rstd"], writes=["ln_rstd"])
    for k in range(8):
        eng = "dve" if k % 2 == 0 else "pool"
        tb = tmp[k % 2]
        tr = "ln_tmp%d" % (k % 2)
        P.op(eng, lambda e, k=k, tb=tb: e.tensor_tensor(out=tb[:, :N], in0=x32[:, k, :N], in1=mean[:, :N], op=ALU.subtract),
             reads=[xres, "ln_mean"], writes=[tr])
        P.op(eng, lambda e, k=k, tb=tb: e.tensor_tensor(out=tb[:, :N], in0=tb[:, :N], in1=rstd[:, :N], op=ALU.mult),
             reads=[tr, "ln_rstd"], writes=[tr])
        P.op("dve", lambda e, k=k, tb=tb: e.tensor_scalar(out=out32[:, k, :N], in0=tb[:, :N], scalar1=g_col[:, k:k + 1], scalar2=b_col[:, k:k + 1],
                                                          op0=ALU.mult, op1=ALU.add),
             reads=[tr, "ln_gb_" + tag], writes=[outres])
        if out16 is not None:
            P.op("act", lambda e, k=k: e.copy(out=out16[:, k, :N], in_=out32[:, k, :N]), reads=[outres], writes=[out16res])


def ln_alloc(P, ps=None):
    if ps is None:
        ps = [(P.ps("ln_ps_m", [128, 512]), "ln_ps_m"), (P.ps("ln_ps_q", [128, 512]), "ln_ps_q")]
    P._ln_ps = ps
    P._ln_sq = [P.sb("ln_sq%d" % i, [128, CH], F32) for i in range(2)]
    P._ln_mean = P.sb("ln_mean", [128, CH], F32)
    P._ln_rstd = P.sb("ln_rstd", [128, CH], F32)
    P._ln_tmp = [P.sb("ln_tmp%d" % i, [128, CH], F32) for i in range(2)]


def emit_ident(P, ident, res="ident"):
    it = P.sb("ident_i", [128, 128], I32)
    P.op("pool", lambda e: e.iota(it[:], pattern=[[1, 128]], base=0, channel_multiplier=-1), writes=["ident_i"])
    P.op("dve", lambda e: e.tensor_single_scalar(out=ident[:], in_=it[:], scalar=0, op=ALU.is_equal), reads=["ident_i"], writes=[res])


def emit_B1(P, dr, h1T):
    nc = P.nc
    N = CH
    hT_v = dr["hT"].rearrange("(k p) t -> p k t", p=128)
    ssm_v = dr["ssmT"].rearrange("(k p) t -> p k t", p=128)
    conv_v = dr["convT"].rearrange("(k p) t -> p k t", p=128)
    attn_v = dr["attnT"].rearrange("(k p) t -> p k t", p=128)
    h1T_v = h1T.rearrange("(k p) t -> p k t", p=128)
    wglu = P.sb("wglu", [128, 4, 512], BF16)
    wsout = P.sb("wsout", [128, 4, 1024], BF16)
    wcout = P.sb("wcout", [128, 4, 1024], BF16)
    waout = P.sb("waout", [128, 8, 1024], BF16)
    wgate = P.sb("wgate", [128, 8, 3072], BF16)
    wo = P.sb("wo", [128, 8, 1024], BF16)
    for t, name, src in ((wglu, "wglu", "w_glu"), (wsout, "wsout", "w_sout"), (wcout, "wcout", "w_cout"),
                         (waout, "waout", "w_aout"), (wo, "wo", "w_o")):
        P.dma("pool", t[:], dr[src].rearrange("(k p) n -> p k n", p=128), writes=[name])
    gv = dr["w_gate"].rearrange("(k p) n -> p k n", p=128)
    for i in range(3):
        P.dma("pool", wgate[:, :, i * 1024:(i + 1) * 1024], gv[:, :, i * 1024:(i + 1) * 1024], writes=["wgate%d" % i])
    gb = P.sb("gb", [128, 24], F32)
    P.dma("sp", gb[:], dr["gate_b_c"], writes=["gb"])
    lg = P.sb("lng", [128, 8], F32)
    lb = P.sb("lnb", [128, 8], F32)
    P.dma("sp", lg[:], dr["ln1_g_c"], writes=["ln_gb_1"])
    P.dma("sp", lb[:], dr["ln1_b_c"], writes=["ln_gb_1"], semkey="lnb1")
    ones_f = P.sb("ones_f", [128, 128], F32)
    P.op("dve", lambda e: e.memset(ones_f[:], 1.0 / D), writes=["ones_f"])
    ln_alloc(P)
    h32_b = [P.sb("h32_0", [128, 8, N], F32)] * 2
    h16 = P.sb("h16", [128, 8, N], BF16)
    s32_b = [P.sb("s32_%d" % i, [128, 4, N], F32) for i in range(2)]
    s16 = P.sb("s16", [128, 4, N], BF16)
    c16_b = [P.sb("c16_%d" % i, [128, 4, N], BF16) for i in range(2)]
    a16_b = [P.sb("a16_%d" % i, [128, 8, N], BF16) for i in range(2)]

    def b1_load(c):
        t0 = c * N
        i = c % 2
        P.dma("sp", s32_b[i][:], ssm_v[:, :, t0:t0 + N], writes=["s32_%d" % i])
        P.dma("pool", c16_b[i][:], conv_v[:, :, t0:t0 + N], writes=["c16_%d" % i])
        P.dma("pool", a16_b[i][:], attn_v[:, :, t0:t0 + N], writes=["a16_%d" % i])
    sg16 = P.sb("sg16", [128, 4, N], BF16)
    sig = [P.sb("sig%d" % i, [128, N], F32) for i in range(2)]
    gt = [P.sb("gt%d" % i, [128, N], F32) for i in range(2)]
    macc = P.sb("macc", [128, N], F32)
    mtmp = [P.sb("mtmp%d" % i, [128, N], F32) for i in range(2)]
    m16 = P.sb("m16", [128, 8, N], BF16)
    res32 = P.sb("res32", [128, 8, N], F32)
    o32 = P.sb("o32", [128, 8, N], F32)
    psY = [P.ps("psY%d" % i, [128, 512]) for i in range(2)]
    psG = [P.ps("psG%d" % i, [128, 512]) for i in range(2)]
    cnt = 0
    b1_load(0)
    P.dma("sp", h32_b[0][:], hT_v[:, :, 0:N], writes=["h32_0"])
    for c in range(NCH):
        t0 = c * N
        if c + 1 < NCH:
            b1_load(c + 1)
        h32, s32, c16, a16 = h32_b[c % 2], s32_b[c % 2], c16_b[c % 2], a16_b[c % 2]
        H32, S32, C16, A16 = "h32_0", "s32_%d" % (c % 2), "c16_%d" % (c % 2), "a16_%d" % (c % 2)
        P.op("act", lambda e, h32=h32: e.copy(out=h16[:], in_=h32[:]), reads=[H32], writes=["h16"])
        P.op("act", lambda e, s32=s32: e.copy(out=s16[:], in_=s32[:]), reads=[S32], writes=["s16"])
        for j in range(4):
            b = cnt % 2
            cnt += 1
            for k in range(4):
                P.op("pe", lambda e, j=j, k=k, b=b: e.matmul(psY[b][:, :N], wglu[:, k, j * 128:(j + 1) * 128], s16[:, k, :], start=(k == 0), stop=(k == 3)),
                     reads=["wglu", "s16"], writes=["psY%d" % b])
            P.op("act", lambda e, b=b: e.activation(out=sig[b][:], in_=psY[b][:, :N], func=AF.Sigmoid), reads=["psY%d" % b], writes=["sig%d" % b])
            P.op("dve", lambda e, j=j, b=b, s32=s32: e.tensor_tensor(out=sg16[:, j, :], in0=s32[:, j, :], in1=sig[b][:], op=ALU.mult),
                 reads=[S32, "sig%d" % b], writes=["sg16"])
        branches = ((wsout, "wsout", sg16, "sg16", 4), (wcout, "wcout", c16, C16, 4), (waout, "waout", a16, A16, 8))
        for m in range(8):
            for br, (wt, wname, xt, xname, nk) in enumerate(branches):
                b = cnt % 2
                cnt += 1
                for k in range(nk):
                    P.op("pe", lambda e, wt=wt, xt=xt, k=k, m=m, b=b, nk=nk: e.matmul(psY[b][:, :N], wt[:, k, m * 128:(m + 1) * 128], xt[:, k, :],
                                                                                    start=(k == 0), stop=(k == nk - 1)),
                         reads=[wname, xname], writes=["psY%d" % b])
                gc0 = br * 1024 + m * 128
                for k in range(8):
                    P.op("pe", lambda e, k=k, gc0=gc0, b=b: e.matmul(psG[b][:, :N], wgate[:, k, gc0:gc0 + 128], h16[:, k, :], start=(k == 0), stop=(k == 7)),
                         reads=["wgate%d" % br, "h16"], writes=["psG%d" % b])
                P.op("act", lambda e, b=b, br=br, m=m: e.activation(out=gt[b][:], in_=psG[b][:, :N], func=AF.Sigmoid, bias=gb[:, br * 8 + m:br * 8 + m + 1]),
                     reads=["psG%d" % b, "gb"], writes=["gt%d" % b])
                if br == 0:
                    P.op("dve", lambda e, b=b: e.tensor_tensor(out=macc[:], in0=gt[b][:], in1=psY[b][:, :N], op=ALU.mult),
                         reads=["gt%d" % b, "psY%d" % b], writes=["macc"])
                else:
                    P.op("dve", lambda e, b=b: e.tensor_tensor(out=mtmp[b][:], in0=gt[b][:], in1=psY[b][:, :N], op=ALU.mult),
                         reads=["gt%d" % b, "psY%d" % b], writes=["mtmp%d" % b])
                    if br == 1:
                        P.op("pool", lambda e, b=b: e.tensor_tensor(out=macc[:], in0=macc[:], in1=mtmp[b][:], op=ALU.add),
                             reads=["macc", "mtmp%d" % b], writes=["macc"])
                    else:
                        P.op("pool", lambda e, b=b, m=m: e.tensor_tensor(out=m16[:, m, :], in0=macc[:], in1=mtmp[b][:], op=ALU.add),
                             reads=["macc", "mtmp%d" % b], writes=["m16"])
        for m in range(8):
            b = cnt % 2
            cnt += 1
            for k in range(8):
                P.op("pe", lambda e, k=k, m=m, b=b: e.matmul(psY[b][:, :N], wo[:, k, m * 128:(m + 1) * 128], m16[:, k, :], start=(k == 0), stop=(k == 7)),
                     reads=["wo", "m16"], writes=["psY%d" % b])
            P.op("dve", lambda e, m=m, b=b, h32=h32: e.scalar_tensor_tensor(out=res32[:, m, :], in0=h32[:, m, :], scalar=DN_ALPHA, in1=psY[b][:, :N],
                                                                   op0=ALU.mult, op1=ALU.add),
                 reads=[H32, "psY%d" % b], writes=["res32"])
        if c + 1 < NCH:
            P.dma("sp", h32_b[0][:], hT_v[:, :, t0 + N:t0 + 2 * N], writes=["h32_0"])
        emit_layernorm(P, res32, "res32", N, ones_f, lg, lb, o32, "o32", "1")
        P.dma("sp", h1T_v[:, :, t0:t0 + N], o32[:], reads=["o32"], writes=["dram:h1T"])


def emit_B2(P, dr, h1T, cwT_d, outT, n_experts=NE):
    nc = P.nc
    N = CH
    TOK = TOK_PER_CORE
    h1T_v = h1T.rearrange("(k p) t -> p k t", p=128)
    outT_v = outT.rearrange("(k p) t -> p k t", p=128)
    h16 = P.sbm("h16", [128, 8, TOK], BF16)
    acc = P.sbm("acc", [128, 8, TOK], F32)
    bgu = P.sbm("bgu", [128, NE, 16], F32)
    lg2 = P.sbm("lng2", [128, 8], F32)
    lb2 = P.sbm("lnb2", [128, 8], F32)
    ones_f = P.sbm("ones_f", [128, 128], F32)
    P.dma("sp", bgu[:], dr["bgu_c"], writes=["bgu"])
    P.dma("sp", lg2[:], dr["ln2_g_c"], writes=["ln_gb_2"])
    P.dma("sp", lb2[:], dr["ln2_b_c"], writes=["ln_gb_2"], semkey="lnb2")
    P.op("dve", lambda e: e.memset(ones_f[:], 1.0 / D), writes=["ones_f"])
    P.op("dve", lambda e: e.tensor_scalar(out=bgu[:, :, 8:16], in0=bgu[:, :, 8:16], scalar1=1.0, scalar2=None, op0=ALU.add),
         reads=["bgu"], writes=["bgu"])
    ident = P.sb("ident", [128, 128], F32)
    emit_ident(P, ident)
    rw = P.sb("rw", [128, 8, NE], F32)
    P.dma("sp", rw[:], dr["router_w"].rearrange("(k p) e -> p k e", p=128), writes=["rw"])
    rb = P.sb("rb", [128, NE], F32)
    P.dma("sp", rb[:], dr["router_b_bc"], writes=["rb"])
    bd = P.sb("bd", [NE, D], F32)
    P.dma("sp", bd[:], dr["b_down"], writes=["bd"])
    cwT = P.sb("cwT", [NE, TOK], F32)
    h32 = [P.sb("h32_%d" % i, [128, 8, N], F32) for i in range(2)]
    lgt = P.sb("lgt", [128, NE], F32)
    m8 = P.sb("m8", [128, 8], F32)
    negm = P.sb("negm", [128, 1], F32)
    ex = P.sb("ex", [128, NE], F32)
    exm = P.sb("exm", [128, NE], F32)
    ssum = P.sb("ssum", [128, 1], F32)
    cw = P.sb("cw", [128, NE], F32)
    ps_r = P.ps("ps_r", [128, 512])
    ps_t = P.ps("ps_t", [128, 512])
    ps_b = [P.ps("ps_b%d" % i, [128, 512]) for i in range(2)]
    for c in range(NCH):
        t0 = c * N
        hb = h32[c % 2]
        hres = "h32_%d" % (c % 2)
        P.dma("sp", hb[:], h1T_v[:, :, t0:t0 + N], reads=["dram:h1T"], writes=[hres])
        P.op("act", lambda e, hb=hb, t0=t0: e.copy(out=h16[:, :, t0:t0 + N], in_=hb[:]), reads=[hres], writes=["h16"])
        off = 0
        while off < N:
            ts = min(128, N - off)
            for k in range(8):
                P.op("pe", lambda e, hb=hb, k=k, off=off, ts=ts: e.matmul(ps_r[:ts, :NE], hb[:, k, off:off + ts], rw[:, k, :], start=(k == 0), stop=(k == 7)),
                     reads=[hres, "rw"], writes=["ps_r"])
            P.op("dve", lambda e, ts=ts: e.tensor_tensor(out=lgt[:ts, :], in0=ps_r[:ts, :NE], in1=rb[:ts, :], op=ALU.add), reads=["ps_r", "rb"], writes=["lgt"])
            P.op("dve", lambda e, ts=ts: e.max(out=m8[:ts, :], in_=lgt[:ts, :]), reads=["lgt"], writes=["m8"])
            P.op("dve", lambda e, ts=ts: e.tensor_scalar(out=negm[:ts, :], in0=m8[:ts, 0:1], scalar1=-1.0, scalar2=None, op0=ALU.mult), reads=["m8"], writes=["negm"])
            P.op("act", lambda e, ts=ts: e.activation(out=ex[:ts, :], in_=lgt[:ts, :], func=AF.Exp, bias=negm[:ts, :]), reads=["lgt", "negm"], writes=["ex"])
            P.op("dve", lambda e, ts=ts: e.scalar_tensor_tensor(out=exm[:ts, :], in0=lgt[:ts, :], scalar=m8[:ts, 3:4], in1=ex[:ts, :], op0=ALU.is_ge, op1=ALU.mult,
                                                                accum_out=ssum[:ts, :]),
                 reads=["lgt", "m8", "ex"], writes=["exm", "ssum"])
            P.op("dve", lambda e, ts=ts: e.reciprocal(out=ssum[:ts, :], in_=ssum[:ts, :]), reads=["ssum"], writes=["ssum"])
            P.op("dve", lambda e, ts=ts: e.tensor_scalar(out=cw[:ts, :], in0=exm[:ts, :], scalar1=ssum[:ts, 0:1], scalar2=None, op0=ALU.mult), reads=["exm", "ssum"], writes=["cw"])
            P.op("pe", lambda e, ts=ts: e.transpose(ps_t[:NE, :ts], cw[:ts, :], ident[:ts, :ts]), reads=["cw", "ident"], writes=["ps_t"])
            P.op("act", lambda e, ts=ts, a=t0 + off: e.copy(out=cwT[:, a:a + ts], in_=ps_t[:NE, :ts]), reads=["ps_t"], writes=["cwT"])
            off += ts
        for m in range(8):
            b = m % 2
            P.op("pe", lambda e, m=m, b=b, t0=t0: e.matmul(ps_b[b][:, :N], bd[:, m * 128:(m + 1) * 128], cwT[:, t0:t0 + N], start=True, stop=True),
                 reads=["bd", "cwT"], writes=["ps_b%d" % b])
            P.op("dve", lambda e, m=m, b=b, hb=hb, t0=t0: e.scalar_tensor_tensor(out=acc[:, m, t0:t0 + N], in0=hb[:, m, :], scalar=DN_ALPHA, in1=ps_b[b][:, :N],
                                                                                 op0=ALU.mult, op1=ALU.add),
                 reads=[hres, "ps_b%d" % b], writes=["acc%d" % c])
    P.dma("sp", cwT_d, cwT[:], reads=["cwT"], writes=["dram:cwT"])
    P.new_phase(keep_mid=True)
    hid = P.sb("hid", [128, 8, TOK], BF16)
    NSLOT = 4
    ring = [P.sb("ring%d" % i, [128, 8, 512], BF16) for i in range(NSLOT)]
    cwB = [P.sb("cwB%d" % i, [128, TOK], F32) for i in range(2)]
    gc = [P.sb("gc%d" % i, [128, N], F32) for i in range(2)]
    sg = [P.sb("sg%d" % i, [128, N], F32) for i in range(2)]
    uc = [P.sb("uc%d" % i, [128, N], F32) for i in range(2)]
    tt = [P.sb("tt%d" % i, [128, N], F32) for i in range(2)]
    hd = [P.sb("hd%d" % i, [128, N], F32) for i in range(2)]
    psG = [P.ps("psG%d" % i, [128, 512]) for i in range(2)]
    psU = [P.ps("psU%d" % i, [128, 512]) for i in range(2)]
    psO = [P.ps("psO%d" % i, [128, 512]) for i in range(2)]
    it = 0
    io = 0
    wunits = [(ex_i, kind, idx) for ex_i in range(n_experts) for kind, idx in (("gu", 0), ("gu", 1), ("gu", 2), ("gu", 3), ("dn", 0), ("dn", 1))]

    def load_unit(u):
        ex_i, kind, idx = wunits[u]
        W = ring[u % NSLOT]
        wres = "ring%d" % (u % NSLOT)
        if kind == "gu":
            wgu_v = dr["w_gu"][ex_i].rearrange("(k p) n -> p k n", p=128)
            P.dma("pool", W[:, :, 0:256], wgu_v[:, :, idx * 256:(idx + 1) * 256], writes=[wres])
            P.dma("pool", W[:, :, 256:512], wgu_v[:, :, DFF + idx * 256:DFF + (idx + 1) * 256], writes=[wres], semkey=wres + "u")
        else:
            wd_v = dr["w_down"][ex_i].rearrange("(k p) n -> p k n", p=128)
            P.dma("pool", W[:], wd_v[:, :, idx * 512:(idx + 1) * 512], writes=[wres])

    PF = 2
    for u in range(min(PF, len(wunits))):
        load_unit(u)
    for u, (ex_i, kind, idx) in enumerate(wunits):
        if u + PF < len(wunits):
            load_unit(u + PF)
        W = ring[u % NSLOT]
        wres = "ring%d" % (u % NSLOT)
        cb = cwB[ex_i % 2]
        cres = "cwB%d" % (ex_i % 2)
        if kind == "gu" and idx == 0:
            P.dma("sp", cb[:], cwT_d[ex_i:ex_i + 1, :].partition_broadcast(128), reads=["dram:cwT"], writes=[cres])
        if kind == "gu":
            q = idx
            for c in range(NCH):
                t0 = c * N
                for jj in range(2):
                    j = 2 * q + jj
                    b = it % 2
                    it += 1
                    for k in range(8):
                        P.op("pe", lambda e, W=W, k=k, jj=jj, b=b, t0=t0: e.matmul(psG[b][:, :N], W[:, k, jj * 128:(jj + 1) * 128], h16[:, k, t0:t0 + N],
                                                                                  start=(k == 0), stop=(k == 7)),
                             reads=[wres, "h16"], writes=["psG%d" % b])
                    for k in range(8):
                        P.op("pe", lambda e, W=W, k=k, jj=jj, b=b, t0=t0: e.matmul(psU[b][:, :N], W[:, k, 256 + jj * 128:256 + (jj + 1) * 128], h16[:, k, t0:t0 + N],
                                                                                  start=(k == 0), stop=(k == 7)),
                             reads=[wres, "h16"], writes=["psU%d" % b])
                    P.op("dve", lambda e, b=b, j=j, ex_i=ex_i: e.tensor_scalar(out=gc[b][:], in0=psG[b][:, :N], scalar1=bgu[:, ex_i, j:j + 1], scalar2=SW_LIMIT,
                                                                               op0=ALU.add, op1=ALU.min),
                         reads=["psG%d" % b, "bgu"], writes=["gc%d" % b])
                    P.op("act", lambda e, b=b: e.activation(out=sg[b][:], in_=gc[b][:], func=AF.Sigmoid, scale=SW_ALPHA), reads=["gc%d" % b], writes=["sg%d" % b])
                    P.op("dve", lambda e, b=b, j=j, ex_i=ex_i: e.tensor_scalar(out=uc[b][:], in0=psU[b][:, :N], scalar1=bgu[:, ex_i, 8 + j:9 + j], scalar2=SW_LIMIT + 1.0,
                                                                               op0=ALU.add, op1=ALU.min),
                         reads=["psU%d" % b, "bgu"], writes=["uc%d" % b])
                    P.op("pool", lambda e, b=b: e.tensor_tensor(out=tt[b][:], in0=gc[b][:], in1=sg[b][:], op=ALU.mult),
                         reads=["gc%d" % b, "sg%d" % b], writes=["tt%d" % b])
                    P.op("dve", lambda e, b=b: e.scalar_tensor_tensor(out=hd[b][:], in0=uc[b][:], scalar=-SW_LIMIT + 1.0, in1=tt[b][:], op0=ALU.max, op1=ALU.mult),
                         reads=["uc%d" % b, "tt%d" % b], writes=["hd%d" % b])
                    P.op("pool", lambda e, b=b, j=j, cb=cb, t0=t0: e.tensor_tensor(out=hid[:, j, t0:t0 + N], in0=hd[b][:], in1=cb[:, t0:t0 + N], op=ALU.mult),
                         reads=["hd%d" % b, cres], writes=["hid%d" % c])
        else:
            mh = idx
            for c in range(NCH):
                t0 = c * N
                for mm in range(4):
                    m = 4 * mh + mm
                    b = io % 2
                    io += 1
                    for j in range(8):
                        P.op("pe", lambda e, W=W, j=j, mm=mm, b=b, t0=t0: e.matmul(psO[b][:, :N], W[:, j, mm * 128:(mm + 1) * 128], hid[:, j, t0:t0 + N],
                                                                                  start=(j == 0), stop=(j == 7)),
                             reads=[wres, "hid%d" % c], writes=["psO%d" % b])
                    P.op("dve", lambda e, m=m, b=b, t0=t0: e.tensor_tensor(out=acc[:, m, t0:t0 + N], in0=acc[:, m, t0:t0 + N], in1=psO[b][:, :N], op=ALU.add),
                         reads=["acc%d" % c, "psO%d" % b], writes=["acc%d" % c])
    P.new_phase(keep_mid=True)
    ln_alloc(P)
    o32 = [P.sb("o32_%d" % i, [128, 8, N], F32) for i in range(2)]
    for c in range(NCH):
        t0 = c * N
        ob = o32[c % 2]
        ores = "o32_%d" % (c % 2)
        emit_layernorm(P, acc[:, :, t0:t0 + N], "acc%d" % c, N, ones_f, lg2, lb2, ob, ores, "2")
        P.dma("sp", outT_v[:, :, t0:t0 + N], ob[:], reads=[ores], writes=["dram:outT"])


B_INPUTS = [("hT", [D, TOK_PER_CORE]), ("ssmT", [SSM_W, TOK_PER_CORE]), ("convT", [CONV_W, TOK_PER_CORE]), ("attnT", [ATT_W, TOK_PER_CORE]),
            ("w_glu", [SSM_W, SSM_W]), ("w_sout", [SSM_W, D]), ("w_cout", [CONV_W, D]), ("w_aout", [ATT_W, D]), ("w_gate", [D, 3 * D]),
            ("gate_b_c", [128, 24]), ("w_o", [D, D]), ("ln1_g_c", [128, 8]), ("ln1_b_c", [128, 8]), ("router_w", [D, NE]),
            ("router_b_bc", [128, NE]), ("w_gu", [NE, D, 2 * DFF]), ("bgu_c", [128, NE, 16]), ("w_down", [NE, DFF, D]), ("b_down", [NE, D]),
            ("ln2_g_c", [128, 8]), ("ln2_b_c", [128, 8])]


def build_B(n_experts=NE):
    nc = bass.Bass("TRN2", target_bir_lowering=False)
    dr = {name: nc.dram_tensor(name, shape, F32, kind="ExternalInput").ap() for name, shape in B_INPUTS}
    outT = nc.dram_tensor("outT", [D, TOK_PER_CORE], F32, kind="ExternalOutput").ap()
    h1T = nc.dram_tensor("h1T_scr", [D, TOK_PER_CORE], F32, kind="Internal").ap()
    cwT_d = nc.dram_tensor("cwT_scr", [NE, TOK_PER_CORE], F32, kind="Internal").ap()
    P = Prog(nc)
    emit_B1(P, dr, h1T)
    P.new_phase()
    emit_B2(P, dr, h1T, cwT_d, outT, n_experts=n_experts)
    P.emit()
    return nc


def host_B_weights(inp, l):
    f = np.float32
    bgu = np.asarray(inp["expert_b_gu"][l], f)
    return {
        "w_glu": np.ascontiguousarray(inp["ssm_w_glu"][l], f), "w_sout": np.ascontiguousarray(inp["ssm_w_out"][l], f),
        "w_cout": np.ascontiguousarray(inp["conv_w_out"][l], f), "w_aout": np.ascontiguousarray(inp["attn_w_out"][l], f),
        "w_gate": np.ascontiguousarray(inp["gate_w"][l], f), "gate_b_c": col_layout(inp["gate_b"][l]),
        "w_o": np.ascontiguousarray(inp["w_o"][l], f), "ln1_g_c": col_layout(inp["ln1_g"][l]), "ln1_b_c": col_layout(inp["ln1_b"][l]),
        "router_w": np.ascontiguousarray(inp["router_w"][l], f),
        "router_b_bc": np.ascontiguousarray(np.broadcast_to(np.asarray(inp["router_b"][l], f)[None, :], (128, NE))),
        "w_gu": np.ascontiguousarray(inp["expert_w_gu"][l], f),
        "bgu_c": np.ascontiguousarray(bgu.reshape(NE, 16, 128).transpose(2, 0, 1)),
        "w_down": np.ascontiguousarray(inp["expert_w_down"][l], f), "b_down": np.ascontiguousarray(inp["expert_b_down"][l], f),
        "ln2_g_c": col_layout(inp["ln2_g"][l]), "ln2_b_c": col_layout(inp["ln2_b"][l]),
    }


CW1 = 6.28125
CW2 = TWO_PI - 6.28125


def sincos_alloc(P, shape, tag):
    p, n = shape
    return [P.sb("sc_%s_%s" % (x, tag), [p, n], I32 if x == "k" else F32) for x in ("y", "k", "kf", "r", "r2", "m")]


def emit_sincos(P, scr, ang, ares, cos_out, cres, sin_out, sres, tag):
    y, ki, kf, r, r2, m = scr
    ry, rk, rkf, rr, rr2, rm = ["sc_%s_%s" % (x, tag) for x in ("y", "k", "kf", "r", "r2", "m")]
    P.op("dve", lambda e: e.tensor_scalar(out=y[:], in0=ang, scalar1=1.0 / TWO_PI, scalar2=None, op0=ALU.mult), reads=[ares], writes=[ry])
    P.op("dve", lambda e: e.tensor_copy(out=ki[:], in_=y[:]), reads=[ry], writes=[rk])
    P.op("dve", lambda e: e.tensor_copy(out=kf[:], in_=ki[:]), reads=[rk], writes=[rkf])
    P.op("dve", lambda e: e.scalar_tensor_tensor(out=r[:], in0=kf[:], scalar=-CW1, in1=ang, op0=ALU.mult, op1=ALU.add), reads=[rkf, ares], writes=[rr])
    P.op("dve", lambda e: e.scalar_tensor_tensor(out=r[:], in0=kf[:], scalar=-CW2, in1=r[:], op0=ALU.mult, op1=ALU.add), reads=[rkf, rr], writes=[rr])
    P.op("dve", lambda e: e.tensor_scalar(out=m[:], in0=r[:], scalar1=math.pi / 2, scalar2=-TWO_PI, op0=ALU.is_gt, op1=ALU.mult), reads=[rr], writes=[rm])
    P.op("dve", lambda e: e.scalar_tensor_tensor(out=r2[:], in0=r[:], scalar=math.pi / 2, in1=m[:], op0=ALU.add, op1=ALU.add), reads=[rr, rm], writes=[rr2])
    P.op("dve", lambda e: e.tensor_scalar(out=r[:], in0=r[:], scalar1=-math.pi, scalar2=math.pi, op0=ALU.max, op1=ALU.min), reads=[rr], writes=[rr])
    P.op("dve", lambda e: e.tensor_scalar(out=r2[:], in0=r2[:], scalar1=-math.pi, scalar2=math.pi, op0=ALU.max, op1=ALU.min), reads=[rr2], writes=[rr2])
    P.op("act", lambda e: e.activation(out=sin_out, in_=r[:], func=AF.Sin), reads=[rr], writes=[sres])
    P.op("act", lambda e: e.activation(out=cos_out, in_=r2[:], func=AF.Sin), reads=[rr2], writes=[cres])


NCHA = 17
NSUB = 3
SSM_EVERY = 10
NEG = -1.0e30


def chunkA(c):
    t0 = c * 512
    return t0, min(512, LP - t0)


def emit_A(P, dr, lam_init):
    nc = P.nc
    hT_v = dr["hT"].rearrange("(k p) t -> p k t", p=128)
    uT = P.sbm("uT", [128, LP], BF16)
    qT = [P.sbm("qT%d" % h, [128, LP], BF16) for h in range(2)]
    kT = [P.sbm("kT%d" % h, [128, LP], BF16) for h in range(2)]
    Va = [P.sbm("Va%d" % h, [128, NBLK, 130], BF16) for h in range(2)]
    ident = P.sbm("ident", [128, 128], F32)
    ident16 = P.sbm("ident16", [128, 128], BF16)
    emit_ident(P, ident)
    P.op("act", lambda e: e.copy(out=ident16[:], in_=ident[:]), reads=["ident"], writes=["ident16"])
    w16 = P.sb("w16", [128, 8, 1280], BF16)
    wv = dr["w_sel"].rearrange("(k p) n -> p k n", p=128)
    for i in range(2):
        P.dma("pool", w16[:, :, i * 640:(i + 1) * 640], wv[:, :, i * 640:(i + 1) * 640], writes=["w16_%d" % i])
    WR = ["w16_0", "w16_1"]
    wrot = P.sb("wrot", [128, 8, 512], BF16)
    for t in range(4):
        src0 = 512 + t * 128
        for c2 in range(2):
            s = src0 + c2 * 64
            d = t * 128 + c2 * 64
            P.op("act", lambda e, s=s, d=d: e.activation(out=wrot[:, :, d:d + 32], in_=w16[:, :, s + 32:s + 64], func=AF.Copy, scale=-1.0),
                 reads=WR, writes=["wrot"])
            P.op("act", lambda e, s=s, d=d: e.copy(out=wrot[:, :, d + 32:d + 64], in_=w16[:, :, s:s + 32]), reads=WR, writes=["wrot"])
    invf = P.sb("invf", [128, 1], F32)
    P.dma("sp", invf[:], dr["inv_c"], writes=["invf"])
    cw3 = P.sb("cw3", [128, 3], F32)
    P.dma("sp", cw3[:], dr["conv_w_c"], writes=["cw3"])
    h16 = [P.sb("h16_%d" % i, [128, 8, 512], BF16) for i in range(2)]
    posi = P.sb("posi", [128, 512], I32)
    posf = P.sb("posf", [128, 512], F32)
    ang = P.sb("ang", [128, 512], F32)
    cosT = P.sb("cosT", [128, 512], F32)
    sinT = P.sb("sinT", [128, 512], F32)
    sc_rope = sincos_alloc(P, [128, 512], "rope")
    zb = P.sb("zb", [128, 514], F32)
    ccs = P.sb("ccs", [128, 512], F32)
    cy = P.sb("cy", [128, 512], F32)
    cyo = [P.sb("cyo%d" % i, [128, 512], F32) for i in range(2)]
    rq1 = P.sb("rq1", [128, 512], F32)
    rq2 = P.sb("rq2", [128, 512], F32)
    psA = [P.ps("psA%d" % i, [128, 512]) for i in range(4)]
    psV = [P.ps("psV%d" % i, [128, 512]) for i in range(2)]
    P.op("dve", lambda e: e.memset(zb[:, 0:2], 0.0), writes=["zb"])
    for h in range(2):
        P.op("pool", lambda e, h=h: e.memset(Va[h][:, :, 128:130], 1.0), writes=["Va%d" % h])
    P.op("pool", lambda e: e.iota(posi[:], pattern=[[1, 512]], base=0, channel_multiplier=0), writes=["posi"])
    P.op("dve", lambda e: e.tensor_copy(out=posf[:], in_=posi[:]), reads=["posi"], writes=["posf"])
    pa = 0
    convT_out = dr["convT_out"]

    def proj(wt, wres, col0, hb, hres, n, ps, pres):
        for k in range(8):
            P.op("pe", lambda e, k=k: e.matmul(ps[:, :n], wt[:, k, col0:col0 + 128], hb[:, k, :n], start=(k == 0), stop=(k == 7)),
                 reads=wres + [hres], writes=[pres])

    for c in range(NCHA):
        t0, n = chunkA(c)
        hb = h16[c % 2]
        hres = "h16_%d" % (c % 2)
        if c == 0:
            P.dma("pool", hb[:, :, :n], hT_v[:, :, t0:t0 + n], writes=[hres])
        if c + 1 < NCHA:
            t0n, nn = chunkA(c + 1)
            P.dma("pool", h16[(c + 1) % 2][:, :, :nn], hT_v[:, :, t0n:t0n + nn], writes=["h16_%d" % ((c + 1) % 2)])
        P.op("dve", lambda e, t0=t0: e.tensor_scalar(out=ang[:], in0=posf[:], scalar1=float(t0), scalar2=invf[:, 0:1], op0=ALU.add, op1=ALU.mult),
             reads=["posf", "invf"], writes=["ang"])
        emit_sincos(P, sc_rope, ang[:], "ang", cosT[:], "cosT", sinT[:], "sinT", "rope")
        ps = psA[pa % 4]; pres = "psA%d" % (pa % 4); pa += 1
        proj(w16, WR, 0, hb, hres, n, ps, pres)
        P.op("act", lambda e, ps=ps, t0=t0, n=n: e.copy(out=uT[:, t0:t0 + n], in_=ps[:, :n]), reads=[pres], writes=["uT"])
        psb = psA[pa % 4]; rb_ = "psA%d" % (pa % 4); pa += 1
        proj(w16, WR, 128, hb, hres, n, psb, rb_)
        psc = psA[pa % 4]; rc_ = "psA%d" % (pa % 4); pa += 1
        proj(w16, WR, 256, hb, hres, n, psc, rc_)
        psh = psA[pa % 4]; rh_ = "psA%d" % (pa % 4); pa += 1
        proj(w16, WR, 384, hb, hres, n, psh, rh_)
        P.op("act", lambda e, psc=psc, n=n: e.copy(out=ccs[:, :n], in_=psc[:, :n]), reads=[rc_], writes=["ccs"])
        P.op("dve", lambda e, psh=psh, n=n: e.tensor_tensor(out=zb[:, 2:2 + n], in0=ccs[:, :n], in1=psh[:, :n], op=ALU.mult), reads=["ccs", rh_, "zb"], writes=["zb"])
        P.op("dve", lambda e, n=n: e.tensor_scalar(out=cy[:, :n], in0=zb[:, 2:2 + n], scalar1=cw3[:, 2:3], scalar2=None, op0=ALU.mult), reads=["zb", "cw3"], writes=["cy"])
        P.op("dve", lambda e, n=n: e.scalar_tensor_tensor(out=cy[:, :n], in0=zb[:, 1:1 + n], scalar=cw3[:, 1:2], in1=cy[:, :n], op0=ALU.mult, op1=ALU.add),
             reads=["zb", "cw3", "cy"], writes=["cy"])
        P.op("dve", lambda e, n=n: e.scalar_tensor_tensor(out=cy[:, :n], in0=zb[:, 0:n], scalar=cw3[:, 0:1], in1=cy[:, :n], op0=ALU.mult, op1=ALU.add),
             reads=["zb", "cw3", "cy"], writes=["cy"])
        co = cyo[c % 2]; cores_ = "cyo%d" % (c % 2)
        P.op("dve", lambda e, psb=psb, n=n, co=co: e.tensor_tensor(out=co[:, :n], in0=cy[:, :n], in1=psb[:, :n], op=ALU.mult), reads=["cy", rb_], writes=[cores_])
        nv = min(n, L - t0)
        if nv > 0:
            P.dma("sp", convT_out[:, t0:t0 + nv], co[:, :nv], reads=[cores_], writes=["dram:convT_out"])
        P.op("act", lambda e, n=n: e.copy(out=zb[:, 0:2], in_=zb[:, n:n + 2]), reads=["zb"], writes=["zb"])
        for t in range(4):
            dst = (qT, kT)[t // 2][t % 2]
            dres = ("qT%d", "kT%d")[t // 2] % (t % 2)
            p1 = psA[pa % 4]; r1 = "psA%d" % (pa % 4); pa += 1
            proj(w16, WR, 512 + t * 128, hb, hres, n, p1, r1)
            p2 = psA[pa % 4]; r2 = "psA%d" % (pa % 4); pa += 1
            proj(wrot, ["wrot"], t * 128, hb, hres, n, p2, r2)
            P.op("dve", lambda e, p1=p1, n=n: e.tensor_tensor(out=rq1[:, :n], in0=cosT[:, :n], in1=p1[:, :n], op=ALU.mult), reads=["cosT", r1], writes=["rq1"])
            P.op("dve", lambda e, p2=p2, n=n: e.tensor_tensor(out=rq2[:, :n], in0=sinT[:, :n], in1=p2[:, :n], op=ALU.mult), reads=["sinT", r2], writes=["rq2"])
            P.op("pool", lambda e, dst=dst, t0=t0, n=n: e.tensor_tensor(out=dst[:, t0:t0 + n], in0=rq1[:, :n], in1=rq2[:, :n], op=ALU.add),
                 reads=["rq1", "rq2"], writes=[dres])
        for s in range(n // 128):
            pv = psV[s % 2]; rv = "psV%d" % (s % 2)
            for k in range(8):
                P.op("pe", lambda e, k=k, s=s, pv=pv, hb=hb: e.matmul(pv[:, :256], hb[:, k, s * 128:(s + 1) * 128], w16[:, k, 1024:1280], start=(k == 0), stop=(k == 7)),
                     reads=WR + [hres], writes=[rv])
            blk = t0 // 128 + s
            for h in range(2):
                P.op("act", lambda e, h=h, blk=blk, pv=pv: e.copy(out=Va[h][:, blk, 0:128], in_=pv[:, h * 128:(h + 1) * 128]), reads=[rv], writes=["Va%d" % h])
    P.new_phase(keep_mid=True)
    T = emit_A2_ssm(P, dr, uT, ident)
    P.new_phase(keep_mid=True)
    it_s = ssm_steps(P, dr, uT, T)
    k = 0
    for _ in attn_steps(P, dr, qT, kT, Va, ident16, lam_init):
        k += 1
        if k % SSM_EVERY == 0:
            next(it_s, None)
    for _ in it_s:
        pass


TC = 256
NCHS = (LP + TC - 1) // TC


def chunkS(c):
    t0 = c * TC
    return t0, min(TC, LP - t0)


def emit_A2_ssm(P, dr, uT, ident):
    nc = P.nc
    ssmT_out = dr["ssmT_out"]
    lr = P.sb("lr", [128, 4], F32)
    li = P.sb("li", [128, 4], F32)
    ldt = P.sb("ldt", [128, 4], F32)
    bre = P.sb("bre", [128, 4, 16], F32)
    bim = P.sb("bim", [128, 4, 16], F32)
    cre = P.sb("cre", [128, 4, 16], F32)
    cim = P.sb("cim", [128, 4, 16], F32)
    dcol = P.sb("dcol", [128, 1], F32)
    for t, nm, src in ((lr, "lr", "lam_re_c"), (li, "li", "lam_im_c"), (ldt, "ldt", "log_dt_c"), (bre, "bre", "b_re_c"), (bim, "bim", "b_im_c"),
                       (cre, "cre", "c_reT_c"), (cim, "cim", "c_imT_c"), (dcol, "dcol", "d_c")):
        P.dma("sp", t[:], dr[src], writes=[nm])
    V = lambda name: P.sb(name, [128, 4], F32)
    dt, x, mag, th, cth, sth, ar, ai, den, cfr, cfi, t1, t2 = [V(n) for n in ("dt", "x", "mag", "th", "cth", "sth", "ar", "ai", "den", "cfr", "cfi", "t1", "t2")]
    thT = V("thT")
    cTc = P.sbm("cTc", [128, 4], F32)
    sTc = P.sbm("sTc", [128, 4], F32)

    def ts(out, in0, s1, s2, op0, op1=None, rd=(), wr=()):
        if op1 is None:
            P.op("dve", lambda e: e.tensor_scalar(out=out, in0=in0, scalar1=s1, scalar2=None, op0=op0), reads=rd, writes=wr)
        else:
            P.op("dve", lambda e: e.tensor_scalar(out=out, in0=in0, scalar1=s1, scalar2=s2, op0=op0, op1=op1), reads=rd, writes=wr)

    def tt(out, a, b, op, rd=(), wr=(), eng="dve"):
        P.op(eng, lambda e: e.tensor_tensor(out=out, in0=a, in1=b, op=op), reads=rd, writes=wr)

    P.op("act", lambda e: e.activation(out=dt[:], in_=ldt[:], func=AF.Exp), reads=["ldt"], writes=["dt"])
    tt(x[:], lr[:], dt[:], ALU.mult, ["lr", "dt"], ["x"])
    ts(mag[:], x[:], 1.0 / 720, 1.0 / 120, ALU.mult, ALU.add, ["x"], ["mag"])
    for cf in (1.0 / 24, 1.0 / 6, 0.5, 1.0, 1.0):
        tt(mag[:], mag[:], x[:], ALU.mult, ["mag", "x"], ["mag"])
        ts(mag[:], mag[:], cf, None, ALU.add, None, ["mag"], ["mag"])
    tt(th[:], li[:], dt[:], ALU.mult, ["li", "dt"], ["th"])
    sc_s = sincos_alloc(P, [128, 4], "ssm4")
    emit_sincos(P, sc_s, th[:], "th", cth[:], "cth", sth[:], "sth", "ssm4")
    ts(thT[:], th[:], float(TC), None, ALU.mult, None, ["th"], ["thT"])
    emit_sincos(P, sc_s, thT[:], "thT", cTc[:], "cTc", sTc[:], "sTc", "ssm4")
    tt(ar[:], mag[:], cth[:], ALU.mult, ["mag", "cth"], ["ar"])
    tt(ai[:], mag[:], sth[:], ALU.mult, ["mag", "sth"], ["ai"])
    tt(den[:], lr[:], lr[:], ALU.mult, ["lr"], ["den"])
    tt(t1[:], li[:], li[:], ALU.mult, ["li"], ["t1"])
    tt(den[:], den[:], t1[:], ALU.add, ["den", "t1"], ["den"])
    P.op("dve", lambda e: e.reciprocal(out=den[:], in_=den[:]), reads=["den"], writes=["den"])
    ts(ar[:], ar[:], -1.0, None, ALU.add, None, ["ar"], ["ar"])
    tt(t1[:], ar[:], lr[:], ALU.mult, ["ar", "lr"], ["t1"])
    tt(t2[:], ai[:], li[:], ALU.mult, ["ai", "li"], ["t2"])
    tt(cfr[:], t1[:], t2[:], ALU.add, ["t1", "t2"], ["cfr"])
    tt(cfr[:], cfr[:], den[:], ALU.mult, ["cfr", "den"], ["cfr"])
    tt(t1[:], ai[:], lr[:], ALU.mult, ["ai", "lr"], ["t1"])
    tt(t2[:], ar[:], li[:], ALU.mult, ["ar", "li"], ["t2"])
    tt(cfi[:], t1[:], t2[:], ALU.subtract, ["t1", "t2"], ["cfi"])
    tt(cfi[:], cfi[:], den[:], ALU.mult, ["cfi", "den"], ["cfi"])
    BTr = P.sbm("BTr", [128, 4, 128], BF16)
    BTi = P.sbm("BTi", [128, 4, 128], BF16)
    CTr = P.sbm("CTr", [128, 4, 128], BF16)
    CTi = P.sbm("CTi", [128, 4, 128], BF16)
    Dd = P.sbm("Dd", [128, 128], BF16)
    bfull = P.sb("bfull", [128, 128], F32)
    b16a = P.sb("b16a", [128, 16], F32)
    b16b = P.sb("b16b", [128, 16], F32)
    psT = P.ps("psT", [128, 512])
    P.op("dve", lambda e: e.memset(CTr[:], 0.0), writes=["CTr"])
    P.op("dve", lambda e: e.memset(CTi[:], 0.0), writes=["CTi"])
    P.op("dve", lambda e: e.tensor_scalar(out=Dd[:], in0=ident[:], scalar1=dcol[:, 0:1], scalar2=None, op0=ALU.mult), reads=["ident", "dcol"], writes=["Dd"])
    for i in range(4):
        for (dst, sa, ca, sb_, cb_, opc) in ((BTr, bre, cfr, bim, cfi, ALU.subtract), (BTi, bim, cfr, bre, cfi, ALU.add)):
            ts(b16a[:], sa[:, i, :], ca[:, i:i + 1], None, ALU.mult, None, ["bre", "bim", "cfr"], ["b16a"])
            ts(b16b[:], sb_[:, i, :], cb_[:, i:i + 1], None, ALU.mult, None, ["bre", "bim", "cfi"], ["b16b"])
            tt(b16a[:], b16a[:], b16b[:], opc, ["b16a", "b16b"], ["b16a"])
            P.op("dve", lambda e: e.memset(bfull[:], 0.0), writes=["bfull"])
            P.op("dve", lambda e, i=i: e.tensor_copy(out=bfull[0:64, 32 * i:32 * i + 16], in_=b16a[0:64, :]), reads=["b16a"], writes=["bfull"])
            P.op("dve", lambda e, i=i: e.tensor_copy(out=bfull[64:128, 32 * i + 16:32 * i + 32], in_=b16a[64:128, :]), reads=["b16a"], writes=["bfull"])
            P.op("pe", lambda e: e.transpose(psT[:, :128], bfull[:], ident[:]), reads=["bfull", "ident"], writes=["psT"])
            P.op("act", lambda e, dst=dst, i=i: e.copy(out=dst[:, i, :], in_=psT[:, :128]), reads=["psT"], writes=[("BTr" if dst is BTr else "BTi")])
        P.op("dve", lambda e, i=i: e.tensor_copy(out=CTr[0:64, i, 32 * i:32 * i + 16], in_=cre[0:64, i, :]), reads=["cre"], writes=["CTr"])
        P.op("dve", lambda e, i=i: e.tensor_copy(out=CTr[64:128, i, 32 * i + 16:32 * i + 32], in_=cre[64:128, i, :]), reads=["cre"], writes=["CTr"])
        P.op("dve", lambda e, i=i: e.tensor_scalar(out=CTi[0:64, i, 32 * i:32 * i + 16], in0=cim[0:64, i, :], scalar1=-1.0, scalar2=None, op0=ALU.mult),
             reads=["cim"], writes=["CTi"])
        P.op("dve", lambda e, i=i: e.tensor_scalar(out=CTi[64:128, i, 32 * i + 16:32 * i + 32], in0=cim[64:128, i, :], scalar1=-1.0, scalar2=None, op0=ALU.mult),
             reads=["cim"], writes=["CTi"])
    posi = P.sb("posi", [128, TC], I32)
    posf = P.sb("posf", [128, TC], F32)
    angj = P.sb("angj", [128, TC], F32)
    P.op("pool", lambda e: e.iota(posi[:], pattern=[[1, TC]], base=0, channel_multiplier=0), writes=["posi"])
    P.op("dve", lambda e: e.tensor_copy(out=posf[:], in_=posi[:]), reads=["posi"], writes=["posf"])
    sc_t = sincos_alloc(P, [128, TC], "ssmT")
    cosJ = [P.sbm("cosJ%d" % i, [128, TC], F32) for i in range(4)]
    sinJ = [P.sbm("sinJ%d" % i, [128, TC], F32) for i in range(4)]
    rfull = [P.sbm("rfull%d" % i, [128, TC], F32) for i in range(4)]
    for i in range(4):
        ts(angj[:], posf[:], th[:, i:i + 1], None, ALU.mult, None, ["posf", "th"], ["angj"])
        emit_sincos(P, sc_t, angj[:], "angj", cosJ[i][:], "cosJ%d" % i, sinJ[i][:], "sinJ%d" % i, "ssmT")
        P.op("dve", lambda e, i=i: e.memset(rfull[i][:], 1.0), writes=["rfull%d" % i])
        ts(rfull[i][:], rfull[i][:], mag[:, i:i + 1], None, ALU.mult, None, ["rfull%d" % i, "mag"], ["rfull%d" % i])
    return dict(BTr=BTr, BTi=BTi, CTr=CTr, CTi=CTi, Dd=Dd, cosJ=cosJ, sinJ=sinJ, rfull=rfull, cTc=cTc, sTc=sTc)


def ssm_steps(P, dr, uT, T):
    ssmT_out = dr["ssmT_out"]
    BTr, BTi, CTr, CTi, Dd = T["BTr"], T["BTi"], T["CTr"], T["CTi"], T["Dd"]
    cosJ, sinJ, rfull, cTc, sTc = T["cosJ"], T["sinJ"], T["rfull"], T["cTc"], T["sTc"]

    def ts(out, in0, s1, s2, op0, op1=None, rd=(), wr=()):
        if op1 is None:
            P.op("dve", lambda e: e.tensor_scalar(out=out, in0=in0, scalar1=s1, scalar2=None, op0=op0), reads=rd, writes=wr)
        else:
            P.op("dve", lambda e: e.tensor_scalar(out=out, in0=in0, scalar1=s1, scalar2=s2, op0=op0, op1=op1), reads=rd, writes=wr)

    def tt(out, a, b, op, rd=(), wr=(), eng="dve"):
        P.op(eng, lambda e: e.tensor_tensor(out=out, in0=a, in1=b, op=op), reads=rd, writes=wr)

    br32 = [P.sb("br32_%d" % i, [128, TC], F32) for i in range(2)]
    bi32 = [P.sb("bi32_%d" % i, [128, TC], F32) for i in range(2)]
    m1 = P.sb("m1", [128, TC], F32)
    m2 = P.sb("m2", [128, TC], F32)
    m3 = P.sb("m3", [128, TC], F32)
    m4 = P.sb("m4", [128, TC], F32)
    rbr = P.sb("rbr", [128, TC], F32)
    rbi = P.sb("rbi", [128, TC], F32)
    wre = [P.sb("wre%d" % i, [128, TC], F32) for i in range(4)]
    wim = [P.sb("wim%d" % i, [128, TC], F32) for i in range(4)]
    xre = [P.sb("xre%d" % i, [128, TC], BF16) for i in range(2)]
    xim = [P.sb("xim%d" % i, [128, TC], BF16) for i in range(2)]
    ini = P.sb("ini", [128, 4, 2], F32)
    itmp = P.sb("itmp", [128, 2], F32)
    gx = [P.sb("gx%d" % i, [128, TC], F32) for i in range(2)]
    g2 = [P.sb("g2%d" % i, [128, TC], F32) for i in range(2)]
    gs = [P.sb("gs%d" % i, [128, TC], F32) for i in range(2)]
    yo = [P.sb("yo%d" % i, [128, TC], F32) for i in range(2)]
    psB = P.ps("psB", [128, 2, TC])
    py = P.ps("psYs", [128, 512])
    pyr = "psYs"
    P.op("dve", lambda e: e.memset(ini[:], 0.0), writes=["ini"])
    steps = [(c, i) for c in range(NCHS) for i in range(4)]
    NS = len(steps)

    def stA(k):
        c, i = steps[k]
        t0, n = chunkS(c)
        b = k % 2
        P.op("pe", lambda e, i=i, t0=t0, n=n: e.matmul(psB[:, 0, :n], BTr[:, i, :], uT[:, t0:t0 + n], start=True, stop=True), reads=["BTr", "uT"], writes=["psB"])
        P.op("pe", lambda e, i=i, t0=t0, n=n: e.matmul(psB[:, 1, :n], BTi[:, i, :], uT[:, t0:t0 + n], start=True, stop=True, skip_group_check=True), reads=["BTi", "uT"], writes=["psB"])
        P.op("act", lambda e, n=n, b=b: e.copy(out=br32[b][:, :n], in_=psB[:, 0, :n]), reads=["psB"], writes=["br32_%d" % b])
        P.op("act", lambda e, n=n, b=b: e.copy(out=bi32[b][:, :n], in_=psB[:, 1, :n]), reads=["psB"], writes=["bi32_%d" % b])

    def stB(k):
        c, i = steps[k]
        t0, n = chunkS(c)
        b = k % 2
        brb, bib = br32[b], bi32[b]
        brn, bin_ = "br32_%d" % b, "bi32_%d" % b
        cj, sj = cosJ[i], sinJ[i]
        cr, sr = "cosJ%d" % i, "sinJ%d" % i
        tt(m1[:, :n], cj[:, :n], brb[:, :n], ALU.mult, [cr, brn], ["m1"], "dve")
        tt(m2[:, :n], sj[:, :n], bib[:, :n], ALU.mult, [sr, bin_], ["m2"], "pool")
        tt(m3[:, :n], cj[:, :n], bib[:, :n], ALU.mult, [cr, bin_], ["m3"], "pool")
        tt(m4[:, :n], sj[:, :n], brb[:, :n], ALU.mult, [sr, brn], ["m4"], "dve")
        tt(rbr[:, :n], m1[:, :n], m2[:, :n], ALU.add, ["m1", "m2"], ["rbr"], "dve")
        tt(rbi[:, :n], m3[:, :n], m4[:, :n], ALU.subtract, ["m3", "m4"], ["rbi"], "pool")
        wr_, wi_ = wre[i], wim[i]
        wrn, win = "wre%d" % i, "wim%d" % i
        if c > 0:
            ts(itmp[:, 0:1], wi_[:, TC - 1:TC], sTc[:, i:i + 1], None, ALU.mult, None, [win, "sTc"], ["itmp"])
            P.op("dve", lambda e, i=i, wr_=wr_: e.scalar_tensor_tensor(out=ini[:, i, 0:1], in0=wr_[:, TC - 1:TC], scalar=cTc[:, i:i + 1], in1=itmp[:, 0:1],
                                                                      op0=ALU.mult, op1=ALU.subtract), reads=[wrn, "cTc", "itmp"], writes=["ini"])
            ts(itmp[:, 1:2], wr_[:, TC - 1:TC], sTc[:, i:i + 1], None, ALU.mult, None, [wrn, "sTc"], ["itmp"])
            P.op("dve", lambda e, i=i, wi_=wi_: e.scalar_tensor_tensor(out=ini[:, i, 1:2], in0=wi_[:, TC - 1:TC], scalar=cTc[:, i:i + 1], in1=itmp[:, 1:2],
                                                                      op0=ALU.mult, op1=ALU.add), reads=[win, "cTc", "itmp"], writes=["ini"])
        P.op("dve", lambda e, i=i, wr_=wr_, n=n: e.tensor_tensor_scan(out=wr_[:, :n], data0=rfull[i][:, :n], data1=rbr[:, :n], initial=ini[:, i, 0:1],
                                                                     op0=ALU.mult, op1=ALU.add), reads=["rfull%d" % i, "rbr", "ini"], writes=[wrn])
        P.op("dve", lambda e, i=i, wi_=wi_, n=n: e.tensor_tensor_scan(out=wi_[:, :n], data0=rfull[i][:, :n], data1=rbi[:, :n], initial=ini[:, i, 1:2],
                                                                     op0=ALU.mult, op1=ALU.add), reads=["rfull%d" % i, "rbi", "ini"], writes=[win])
        xr, xi = xre[b], xim[b]
        xrn, xin = "xre%d" % b, "xim%d" % b
        tt(m1[:, :n], cj[:, :n], wr_[:, :n], ALU.mult, [cr, wrn], ["m1"], "dve")
        tt(m2[:, :n], sj[:, :n], wi_[:, :n], ALU.mult, [sr, win], ["m2"], "pool")
        tt(m3[:, :n], sj[:, :n], wr_[:, :n], ALU.mult, [sr, wrn], ["m3"], "pool")
        tt(m4[:, :n], cj[:, :n], wi_[:, :n], ALU.mult, [cr, win], ["m4"], "dve")
        tt(xr[:, :n], m1[:, :n], m2[:, :n], ALU.subtract, ["m1", "m2"], [xrn], "dve")
        tt(xi[:, :n], m3[:, :n], m4[:, :n], ALU.add, ["m3", "m4"], [xin], "pool")

    def stC(k):
        c, i = steps[k]
        t0, n = chunkS(c)
        b = k % 2
        xr, xi = xre[b], xim[b]
        xrn, xin = "xre%d" % b, "xim%d" % b
        P.op("pe", lambda e, i=i, xr=xr, n=n: e.matmul(py[:, :n], CTr[:, i, :], xr[:, :n], start=(i == 0), stop=False), reads=["CTr", xrn], writes=[pyr])
        P.op("pe", lambda e, i=i, xi=xi, n=n: e.matmul(py[:, :n], CTi[:, i, :], xi[:, :n], start=False, stop=False), reads=["CTi", xin], writes=[pyr])
        if i == 3:
            g = c % 2
            P.op("pe", lambda e, t0=t0, n=n: e.matmul(py[:, :n], Dd[:], uT[:, t0:t0 + n], start=False, stop=True), reads=["Dd", "uT"], writes=[pyr])
            P.op("act", lambda e, n=n, g=g: e.copy(out=gx[g][:, :n], in_=py[:, :n]), reads=[pyr], writes=["gx%d" % g])
            P.op("act", lambda e, n=n, g=g: e.activation(out=g2[g][:, :n], in_=py[:, :n], func=AF.Square), reads=[pyr], writes=["g2%d" % g])

    def stD2(c):
        t0, n = chunkS(c)
        g = c % 2
        ts(g2[g][:, :n], g2[g][:, :n], 0.044715, 1.0, ALU.mult, ALU.add, ["g2%d" % g], ["g2%d" % g])
        tt(g2[g][:, :n], g2[g][:, :n], gx[g][:, :n], ALU.mult, ["g2%d" % g, "gx%d" % g], ["g2%d" % g], "pool")

    def stD3(c):
        t0, n = chunkS(c)
        g = c % 2
        P.op("act", lambda e, n=n, g=g: e.activation(out=gs[g][:, :n], in_=g2[g][:, :n], func=AF.Sigmoid, scale=1.5957691216057308), reads=["g2%d" % g], writes=["gs%d" % g])

    def stD4(c):
        t0, n = chunkS(c)
        g = c % 2
        tt(yo[g][:, :n], gx[g][:, :n], gs[g][:, :n], ALU.mult, ["gx%d" % g, "gs%d" % g], ["yo%d" % g], "pool")
        nv = min(n, L - t0)
        if nv > 0:
            P.dma("sp", ssmT_out[:, t0:t0 + nv], yo[g][:, :nv], reads=["yo%d" % g], writes=["dram:ssmT_out"])

    tails = {}
    t = 0
    while t < NS + 2 or any(k >= t for k in tails):
        if t < NS:
            stA(t)
        if 0 <= t - 1 < NS:
            stB(t - 1)
        if 0 <= t - 2 < NS:
            stC(t - 2)
            c, i = steps[t - 2]
            if i == 3:
                tails.setdefault(t + 1, []).append(lambda c=c: (stD2(c), stD3(c)))
                tails.setdefault(t + 2, []).append(lambda c=c: stD4(c))
        for f in tails.pop(t, []):
            f()
        t += 1
        yield


def attn_steps(P, dr, qT, kT, Va, ident16, lam_init):
    nc = P.nc
    attn_out = dr["attn_out"]
    scale = HD ** -0.5
    nm_d = P.sb("nm_d", [128, 128], BF16)
    nm_n = P.sb("nm_n", [128, 128], BF16)
    P.op("dve", lambda e: e.memset(nm_d[:], NEG), writes=["nm_d"])
    P.op("dve", lambda e: e.memset(nm_d[0:16, :], 0.0), writes=["nm_d"])
    P.op("dve", lambda e: e.memset(nm_d[0:80, 16:128], 0.0), writes=["nm_d"])
    P.op("dve", lambda e: e.memset(nm_d[:, 80:128], 0.0), writes=["nm_d"])
    P.op("dve", lambda e: e.memset(nm_n[:], NEG), writes=["nm_n"])
    mhalf = P.sb("mhalf", [128, 1], F32)
    P.op("dve", lambda e: e.memset(mhalf[:], -0.5), writes=["mhalf"])
    P.op("dve", lambda e: e.memset(nm_n[0:16, 80:128], 0.0), writes=["nm_n"])
    lq = P.sb("lq", [128, 4, 64], F32)
    P.dma("sp", lq[:], dr["lam_qk_bc"], writes=["lq"])
    lp = P.sb("lp", [128, 2, 64], F32)
    lsum = P.sb("lsum", [128, 2], F32)
    nlam = P.sb("nlam", [128, 1], F32)
    P.op("dve", lambda e: e.tensor_tensor(out=lp[:, 0, :], in0=lq[:, 0, :], in1=lq[:, 1, :], op=ALU.mult), reads=["lq"], writes=["lp"])
    P.op("dve", lambda e: e.tensor_tensor(out=lp[:, 1, :], in0=lq[:, 2, :], in1=lq[:, 3, :], op=ALU.mult), reads=["lq"], writes=["lp"])
    P.op("dve", lambda e: e.reduce_sum(out=lsum[:], in_=lp[:], axis=AX.X), reads=["lp"], writes=["lsum"])
    P.op("act", lambda e: e.activation(out=lsum[:], in_=lsum[:], func=AF.Exp), reads=["lsum"], writes=["lsum"])
    P.op("dve", lambda e: e.tensor_tensor(out=nlam[:], in0=lsum[:, 1:2], in1=lsum[:, 0:1], op=ALU.subtract), reads=["lsum"], writes=["nlam"])
    P.op("dve", lambda e: e.tensor_scalar(out=nlam[:], in0=nlam[:], scalar1=-lam_init, scalar2=None, op0=ALU.add), reads=["nlam"], writes=["nlam"])
    gsc = P.sb("gsc", [128, 128], F32)
    P.dma("sp", gsc[:], dr["subln_g_bc"], writes=["gsc"])
    P.op("dve", lambda e: e.tensor_scalar(out=gsc[:], in0=gsc[:], scalar1=1.0 - lam_init, scalar2=None, op0=ALU.mult), reads=["gsc"], writes=["gsc"])
    psSS = [P.ps("psSS_%d" % b, [128, 2, 512]) for b in range(2)]
    psS = [[psSS[b][:, c, :] for b in range(2)] for c in range(2)]
    psO1 = [P.ps("psO%d" % c, [128, NSUB, 129]) for c in range(2)]
    sbO = [[P.sb("sbO%d_%d" % (c, b), [128, NSUB, 129], F32) for b in range(2)] for c in range(2)]
    PtP = [P.sb("PtP_%d" % b, [128, 2, NSUB * 128], BF16) for b in range(2)]
    Pt = [[PtP[b][:, c, :] for b in range(2)] for c in range(2)]
    sq16 = P.sb("sq16", [128, 512], BF16)
    ones_c = [P.sb("ones_c%d" % c, [128, 128], BF16) for c in range(2)]
    for c in range(2):
        P.op("dve", lambda e, c=c: e.memset(ones_c[c][:], 0.0), writes=["ones_c%d" % c])
        P.op("dve", lambda e, c=c: e.memset(ones_c[c][c * 64:(c + 1) * 64, :], 1.0), writes=["ones_c%d" % c])
    mx = P.sb("mx", [128, 2, 2, NCHA], F32)
    mxr = P.sb("mxr", [128, 2, 2], F32)
    negM = P.sb("negM", [128, 2], F32)
    negMc = P.sb("negMc", [128, 1], F32)
    r0 = P.sb("r0", [128, 1], F32)
    r1 = P.sb("r1", [128, 1], F32)
    ot = P.sb("ot", [128, 128], F32)
    junk = P.sb("junk", [128, 128], F32)
    ss = P.sb("ss", [128, 1], F32)
    ob = [P.sb("ob%d" % i, [128, 128], F32) for i in range(2)]
    nsb = (NBLK + NSUB - 1) // NSUB
    sbuf_i = 0
    obi = 0
    for h in range(2):
        for which, src, sres in ((0, qT[h], "qT%d" % h), (1, kT[h], "kT%d" % h)):
            for c in range(NCHA):
                t0, n = chunkA(c)
                P.op("dve", lambda e, src=src, t0=t0, n=n: e.tensor_tensor(out=sq16[:, :n], in0=src[:, t0:t0 + n], in1=src[:, t0:t0 + n], op=ALU.mult),
                     reads=[sres], writes=["sq16"])
                for m in range(2):
                    ps = psS[m][0]
                    P.op("pe", lambda e, m=m, ps=ps, n=n: e.matmul(ps[:, :n], ones_c[m][:], sq16[:, :n], start=True, stop=True),
                         reads=["ones_c%d" % m, "sq16"], writes=["psS%d_0" % m])
                    P.op("dve", lambda e, m=m, ps=ps, n=n, which=which, c=c: e.reduce_max(out=mx[:, which, m, c:c + 1], in_=ps[:, :n], axis=AX.X),
                         reads=["psS%d_0" % m], writes=["mx"])
        P.op("dve", lambda e: e.reduce_max(out=mxr[:], in_=mx[:], axis=AX.X), reads=["mx"], writes=["mxr"])
        P.op("dve", lambda e: e.tensor_tensor(out=negM[:], in0=mxr[:, 0, :], in1=mxr[:, 1, :], op=ALU.add), reads=["mxr"], writes=["negM"])
        P.op("dve", lambda e: e.tensor_scalar(out=negM[:], in0=negM[:], scalar1=-0.5 * scale, scalar2=None, op0=ALU.mult), reads=["negM"], writes=["negM"])
        P.op("dve", lambda e: e.tensor_tensor(out=negMc[:], in0=negM[:, 0:1], in1=negM[:, 1:2], op=ALU.min), reads=["negM"], writes=["negMc"])
        if "dbg_q" in dr and h == 0:
            P.dma("sp", dr["dbg_q"], qT[0][:], reads=["qT0"], writes=["dram:dbg_q"], semkey="dbg1")
            P.dma("sp", dr["dbg_k"], kT[0][:], reads=["kT0"], writes=["dram:dbg_k"], semkey="dbg2")
            P.dma("sp", dr["dbg_v"], Va[0][:], reads=["Va0"], writes=["dram:dbg_v"], semkey="dbg3")
            P.dma("sp", dr["dbg_m"], negM[:], reads=["negM"], writes=["dram:dbg_m"], semkey="dbg4")
            P.dma("sp", dr["dbg_l"], nlam[:], reads=["nlam"], writes=["dram:dbg_l"], semkey="dbg5")
        for I in range(nsb):
            i0 = I * NSUB
            nq = min(NSUB, NBLK - i0)
            q0 = i0 * 128
            jmax = min(i0 + nq, NBLK - 1)
            ab = sbuf_i % 2
            sbuf_i += 1
            units = [(j, c) for j in range(jmax + 1) for c in range(2)]

            def geom(j):
                s_lo = max(0, j - 1 - i0)
                return s_lo, s_lo * 128, nq * 128

            def emit_S(u, h=h, i0=i0, nq=nq, q0=q0):
                j, c = units[u]
                s_lo, c0, c1 = geom(j)
                S = psS[c][j % 2]
                sres = "psS%d_%d" % (c, j % 2)
                masks = [(s, nm_d if i0 + s == j else nm_n) for s in range(s_lo, nq) if i0 + s in (j, j - 1)]
                P.op("pe", lambda e, c=c, S=S, j=j, c0=c0, c1=c1, q0=q0, h=h, last=(len(masks) == 0): e.matmul(
                    S[:, c0:c1], kT[h][c * 64:(c + 1) * 64, j * 128:(j + 1) * 128], qT[h][c * 64:(c + 1) * 64, q0 + c0:q0 + c1], start=True, stop=last, skip_group_check=True),
                    reads=["kT%d" % h, "qT%d" % h], writes=[sres])
                for mi, (s, nm) in enumerate(masks):
                    P.op("pe", lambda e, S=S, s=s, nm=nm, last=(mi == len(masks) - 1): e.matmul(S[:, s * 128:(s + 1) * 128], ident16[:], nm[:], start=False, stop=last, skip_group_check=True),
                         reads=["ident16", "nm_d", "nm_n"], writes=[sres])

            def emit_E(u):
                j, c = units[u]
                s_lo, c0, c1 = geom(j)
                b = j % 2
                P.op("act", lambda e, b=b, c0=c0, c1=c1: e.activation(out=PtP[b][:, :, c0:c1], in_=psSS[b][:, :, c0:c1], func=AF.Exp, scale=scale, bias=negMc[:, 0:1]),
                     reads=["psS0_%d" % b, "psS1_%d" % b, "negMc"], writes=["Pt0_%d" % b, "Pt1_%d" % b])

            def emit_PV(u, h=h, ab=ab, nq=nq):
                j, c = units[u]
                s_lo, c0, c1 = geom(j)
                pt = Pt[c][j % 2]
                for s in range(s_lo, nq):
                    P.op("pe", lambda e, c=c, s=s, pt=pt, j=j, h=h, first=(j == 0 and s == s_lo): e.matmul(
                        psO1[c][:, s, :], pt[:, s * 128:(s + 1) * 128], Va[h][:, j, 0:129], start=first, stop=True, skip_group_check=True),
                         reads=["Pt%d_%d" % (c, j % 2), "Va%d" % h], writes=["psO%d" % c])

            emit_S(0)
            emit_S(1)
            for u in range(0, len(units), 2):
                emit_E(u)
                if u + 2 < len(units):
                    emit_S(u + 2)
                    emit_S(u + 3)
                emit_PV(u)
                emit_PV(u + 1)
                yield
            for c in range(2):
                P.op("act", lambda e, c=c, ab=ab: e.copy(out=sbO[c][ab][:], in_=psO1[c][:]), reads=["psO%d" % c], writes=["sbO%d_%d" % (c, ab)])
            psO = [[sbO[0][0], sbO[0][1]], [sbO[1][0], sbO[1][1]]]
            for s in range(nq):
                blk = i0 + s
                o0 = "sbO0_%d" % ab
                o1 = "sbO1_%d" % ab
                P.op("dve", lambda e, ab=ab, s=s: e.reciprocal(out=r0[:], in_=psO[0][ab][:, s, 128:129]), reads=[o0], writes=["r0"])
                P.op("dve", lambda e, ab=ab, s=s: e.reciprocal(out=r1[:], in_=psO[1][ab][:, s, 128:129]), reads=[o1], writes=["r1"])
                P.op("dve", lambda e: e.tensor_tensor(out=r1[:], in0=r1[:], in1=nlam[:], op=ALU.mult), reads=["r1", "nlam"], writes=["r1"])
                P.op("dve", lambda e, ab=ab, s=s: e.tensor_scalar(out=ot[:], in0=psO[0][ab][:, s, 0:128], scalar1=r0[:, 0:1], scalar2=None, op0=ALU.mult),
                     reads=[o0, "r0"], writes=["ot"])
                P.op("dve", lambda e, ab=ab, s=s: e.scalar_tensor_tensor(out=ot[:], in0=psO[1][ab][:, s, 0:128], scalar=r1[:, 0:1], in1=ot[:], op0=ALU.mult, op1=ALU.add),
                     reads=[o1, "r1", "ot"], writes=["ot"])
                P.op("dve", lambda e: e.scalar_tensor_tensor(out=junk[:], in0=ot[:], scalar=1.0, in1=ot[:], op0=ALU.mult, op1=ALU.mult, accum_out=ss[:]),
                     reads=["ot"], writes=["junk", "ss"])
                P.op("dve", lambda e: e.tensor_scalar(out=ss[:], in0=ss[:], scalar1=1.0 / VD, scalar2=RMS_EPS, op0=ALU.mult, op1=ALU.add), reads=["ss"], writes=["ss"])
                P.op("pool", lambda e: e.tensor_tensor(out=ss[:], in0=ss[:], in1=mhalf[:], op=ALU.pow), reads=["ss", "mhalf"], writes=["ss"])
                o = ob[obi % 2]
                ores = "ob%d" % (obi % 2)
                obi += 1
                P.op("dve", lambda e, o=o: e.scalar_tensor_tensor(out=o[:], in0=ot[:], scalar=ss[:, 0:1], in1=gsc[:], op0=ALU.mult, op1=ALU.mult),
                     reads=["ot", "ss", "gsc"], writes=[ores])
                P.dma("sp", attn_out[blk * 128:(blk + 1) * 128, h * 128:(h + 1) * 128], o[:], reads=[ores], writes=["dram:attn_out"])


A_INPUTS = [("hT", [D, LP]), ("w_sel", [D, 1280]), ("inv_c", [128, 1]), ("conv_w_c", [128, 3]),
            ("lam_re_c", [128, 4]), ("lam_im_c", [128, 4]), ("log_dt_c", [128, 4]), ("b_re_c", [128, 4, 16]), ("b_im_c", [128, 4, 16]),
            ("c_reT_c", [128, 4, 16]), ("c_imT_c", [128, 4, 16]), ("d_c", [128, 1]), ("lam_qk_bc", [128, 4, 64]), ("subln_g_bc", [128, 128])]


def build_A(layer, debug=False):
    nc = bass.Bass("TRN2", target_bir_lowering=False)
    dr = {name: nc.dram_tensor(name, shape, F32, kind="ExternalInput").ap() for name, shape in A_INPUTS}
    if debug:
        dr["dbg_q"] = nc.dram_tensor("dbg_q", [128, LP], BF16, kind="ExternalOutput").ap()
        dr["dbg_k"] = nc.dram_tensor("dbg_k", [128, LP], BF16, kind="ExternalOutput").ap()
        dr["dbg_v"] = nc.dram_tensor("dbg_v", [128, NBLK, 130], BF16, kind="ExternalOutput").ap()
        dr["dbg_m"] = nc.dram_tensor("dbg_m", [128, 2], F32, kind="ExternalOutput").ap()
        dr["dbg_l"] = nc.dram_tensor("dbg_l", [128, 1], F32, kind="ExternalOutput").ap()
    dr["ssmT_out"] = nc.dram_tensor("ssmT_out", [128, L], F32, kind="ExternalOutput").ap()
    dr["convT_out"] = nc.dram_tensor("convT_out", [128, L], F32, kind="ExternalOutput").ap()
    dr["attn_out"] = nc.dram_tensor("attn_out", [LP, 256], F32, kind="ExternalOutput").ap()
    P = Prog(nc)
    emit_A(P, dr, 0.8 - 0.6 * math.exp(-0.3 * layer))
    P.emit()
    return nc


def host_A_inputs(inp, l, core, hT_b):
    f = np.float32
    j = core % 4
    w_in = np.asarray(inp["w_in"][l], f)
    s0, s1, s2, s3 = SSM_W, SSM_W + CONV_W, SSM_W + 2 * CONV_W, SSM_W + 3 * CONV_W
    s4, s5 = s3 + ATT_W, s3 + 2 * ATT_W
    cols = [np.arange(j * 128, (j + 1) * 128)]
    for base in (s0, s1, s2):
        cols.append(base + np.arange(j * 128, (j + 1) * 128))
    for base in (s3, s4, s5):
        cols.append(base + np.arange(j * 256, (j + 1) * 256))
    cols = np.concatenate(cols)
    g0 = 8 * j

    def st(a):
        a = np.asarray(a, f)[g0:g0 + 8]
        return np.ascontiguousarray(a.reshape(4, 2, 64).transpose(1, 2, 0).reshape(128, 4))
    ldt = np.repeat(np.asarray(inp["ssm_log_dt"][l], f)[:, None], 64, axis=1)

    def sb3(a):
        a = np.asarray(a, f)[g0:g0 + 8]
        return np.ascontiguousarray(a.reshape(4, 2, 64, 16).transpose(1, 2, 0, 3).reshape(128, 4, 16))
    i = np.arange(128) % 64 % 32
    inv = (np.float32(ROPE_THETA) ** (-(2 * i).astype(np.float32) / np.float32(HD))).astype(f)
    lam_qk = np.stack([np.asarray(inp[k][l], f) for k in ("attn_lambda_q1", "attn_lambda_k1", "attn_lambda_q2", "attn_lambda_k2")])
    return {
        "hT": hT_b, "w_sel": np.ascontiguousarray(w_in[:, cols]), "inv_c": np.ascontiguousarray(inv[:, None]),
        "conv_w_c": np.ascontiguousarray(np.asarray(inp["conv_w"][l], f)[:, j * 128:(j + 1) * 128].T),
        "lam_re_c": st(inp["ssm_lambda_re"][l]), "lam_im_c": st(inp["ssm_lambda_im"][l]), "log_dt_c": st(ldt),
        "b_re_c": sb3(inp["ssm_b_re"][l]), "b_im_c": sb3(inp["ssm_b_im"][l]),
        "c_reT_c": sb3(np.asarray(inp["ssm_c_re"][l], f).transpose(0, 2, 1)), "c_imT_c": sb3(np.asarray(inp["ssm_c_im"][l], f).transpose(0, 2, 1)),
        "d_c": np.ascontiguousarray(np.asarray(inp["ssm_d"][l], f)[j * 128:(j + 1) * 128, None]),
        "lam_qk_bc": np.ascontiguousarray(np.broadcast_to(lam_qk[None], (128, 4, 64))),
        "subln_g_bc": np.ascontiguousarray(np.broadcast_to(np.asarray(inp["attn_subln_g"][l], f)[None], (128, 128))),
    }


def build_LN():
    nc = bass.Bass("TRN2", target_bir_lowering=False)
    xT = nc.dram_tensor("xT", [D, TOK_PER_CORE], F32, kind="ExternalInput").ap()
    g_c = nc.dram_tensor("g_c", [128, 8], F32, kind="ExternalInput").ap()
    b_c = nc.dram_tensor("b_c", [128, 8], F32, kind="ExternalInput").ap()
    oT = nc.dram_tensor("oT", [D, TOK_PER_CORE], F32, kind="ExternalOutput").ap()
    P = Prog(nc)
    N = CH
    xv = xT.rearrange("(k p) t -> p k t", p=128)
    ov = oT.rearrange("(k p) t -> p k t", p=128)
    lg = P.sb("lg", [128, 8], F32)
    lb = P.sb("lb", [128, 8], F32)
    P.dma("sp", lg[:], g_c, writes=["ln_gb_0"])
    P.dma("sp", lb[:], b_c, writes=["ln_gb_0"], semkey="lnb0")
    ones_f = P.sb("ones_f", [128, 128], F32)
    P.op("dve", lambda e: e.memset(ones_f[:], 1.0 / D), writes=["ones_f"])
    ln_alloc(P)
    x32 = [P.sb("x32_%d" % i, [128, 8, N], F32) for i in range(2)]
    o32 = [P.sb("o32_%d" % i, [128, 8, N], F32) for i in range(2)]
    for c in range(NCH):
        t0 = c * N
        xb, xr = x32[c % 2], "x32_%d" % (c % 2)
        ob, orr = o32[c % 2], "o32_%d" % (c % 2)
        P.dma("sp", xb[:], xv[:, :, t0:t0 + N], writes=[xr])
        emit_layernorm(P, xb, xr, N, ones_f, lg, lb, ob, orr, "0")
        P.dma("sp", ov[:, :, t0:t0 + N], ob[:], reads=[orr], writes=["dram:oT"])
    P.emit()
    return nc


_CACHE = {}


def _prog(key, fn):
    if key not in _CACHE:
        _CACHE[key] = fn()
    return _CACHE[key]


def kernel(**inp):
    f = np.float32
    x = np.asarray(inp["x"], f)
    meta = np.asarray(inp["meta_tokens"], f)
    hin = np.concatenate([np.broadcast_to(meta[None], (BATCH, N_META, D)), x], axis=1)
    xT_all = np.ascontiguousarray(hin.reshape(BATCH * L, D).T)
    cores = list(range(NCORES))
    T = TOK_PER_CORE
    maps = [{"xT": np.ascontiguousarray(xT_all[:, c * T:(c + 1) * T]), "g_c": col_layout(inp["ln_in_g"]), "b_c": col_layout(inp["ln_in_b"])} for c in cores]
    res = run_bass_kernel_spmd(_prog("ln", build_LN), maps, core_ids=cores)
    hT_all = np.concatenate([r["oT"] for r in res.results], axis=1)
    for l in range(DEPTH):
        maps = []
        for c in cores:
            b = c // 4
            hT = np.zeros((D, LP), f)
            hT[:, :L] = hT_all[:, b * L:(b + 1) * L]
            maps.append(host_A_inputs(inp, l, c, hT))
        res = run_bass_kernel_spmd(_prog(("A", l), lambda: build_A(l)), maps, core_ids=cores)
        ssmT = np.zeros((SSM_W, BATCH * L), f)
        convT = np.zeros((CONV_W, BATCH * L), f)
        attnT = np.zeros((ATT_W, BATCH * L), f)
        for c in cores:
            b, j = c // 4, c % 4
            r = res.results[c]
            ssmT[j * 128:(j + 1) * 128, b * L:(b + 1) * L] = r["ssmT_out"]
            convT[j * 128:(j + 1) * 128, b * L:(b + 1) * L] = r["convT_out"]
            attnT[j * 256:(j + 1) * 256, b * L:(b + 1) * L] = r["attn_out"][:L].T
        W = host_B_weights(inp, l)
        maps = []
        for c in cores:
            sl = slice(c * T, (c + 1) * T)
            m = dict(W)
            m.update(hT=np.ascontiguousarray(hT_all[:, sl]), ssmT=np.ascontiguousarray(ssmT[:, sl]),
                     convT=np.ascontiguousarray(convT[:, sl]), attnT=np.ascontiguousarray(attnT[:, sl]))
            maps.append(m)
        res = run_bass_kernel_spmd(_prog("B", build_B), maps, core_ids=cores)
        hT_all = np.concatenate([r["outT"] for r in res.results], axis=1)
    out = hT_all.T.reshape(BATCH, L, D)[:, N_META:]
    return np.ascontiguousarray(out, dtype=f)
```

```python
import math
from contextlib import ExitStack

import numpy as np
import concourse.bass as bass
import concourse.mybir as mybir
from concourse.bass_utils import run_bass_kernel_spmd

F32 = mybir.dt.float32
BF16 = mybir.dt.bfloat16
I32 = mybir.dt.int32
AF = mybir.ActivationFunctionType
ALU = mybir.AluOpType
AX = mybir.AxisListType

D = 1024
BATCH = 2
SEQ = 8192
DEPTH = 2
N_META = 16
L = SEQ + N_META
LP = 8320
NBLK = LP // 128
SSM_W = 512
CONV_W = 512
HEADS = 8
HD = 64
VD = 128
ATT_W = 1024
IN_W = 5120
NE = 32
TOPK = 4
DFF = 1024
SW_LIMIT = 7.0
SW_ALPHA = 1.702
DN_ALPHA = (2.0 * DEPTH) ** 0.25
LN_EPS = 1e-5
RMS_EPS = 1e-5
ROPE_THETA = 10000.0
NCORES = 8
TOK_PER_CORE = (BATCH * L) // NCORES
TWO_PI = 2.0 * math.pi


STRICT_SAME_ENGINE = True


class Prog:
    ENGS = ("pe", "act", "dve", "pool", "sp")

    def __init__(self, nc):
        self.nc = nc
        self.es = ExitStack()
        self.ops = []
        self.pes = ExitStack()
        self.mes = ExitStack()
        self.nphase = 0

    def sbm(self, name, shape, dtype):
        return self.mes.enter_context(self.nc.sbuf_tensor("m%d_%s" % (self.nphase, name), list(shape), dtype, side="right"))

    def sb(self, name, shape, dtype):
        return self.pes.enter_context(self.nc.sbuf_tensor("p%d_%s" % (self.nphase, name), list(shape), dtype, side="left"))

    def ps(self, name, shape, dtype=F32):
        return self.pes.enter_context(self.nc.psum_tensor("p%d_%s" % (self.nphase, name), list(shape), dtype))

    def new_phase(self, keep_mid=False):
        self.pes.close()
        self.pes = ExitStack()
        if not keep_mid:
            self.mes.close()
            self.mes = ExitStack()
        self.nphase += 1
        self.ops.append(dict(barrier=True, eng=None, dma=False))

    def op(self, eng, fn, reads=(), writes=(), dma=False, semkey=None):
        assert eng in self.ENGS
        self.ops.append(dict(eng=eng, fn=fn, reads=tuple(reads), writes=tuple(writes), dma=dma, semkey=semkey))

    def dma(self, eng, out, in_, reads=(), writes=(), semkey=None):
        self.op(eng, lambda e: e.dma_start(out=out, in_=in_), reads, writes, dma=True, semkey=semkey)

    def emit(self, final_dram_writes=True):
        nc = self.nc
        ops = self.ops
        n = len(ops)
        last_writer = {}
        readers = {}
        deps = [dict() for _ in range(n)]
        last_on_eng = {}
        last_dma = {}
        pending = {}
        for i, o in enumerate(ops):
            if o.get("barrier"):
                bl = list(last_on_eng.values()) + list(last_dma.values())
                pending = {e: bl for e in self.ENGS}
                last_writer = {}
                readers = {}
                continue
            if pending.get(o["eng"]):
                for j in pending[o["eng"]]:
                    deps[i][j] = "raw"
                pending[o["eng"]] = None
            if o["dma"]:
                last_dma[(o["semkey"], o["writes"], o["reads"])] = i
            else:
                last_on_eng[o["eng"]] = i
            for r in o["reads"]:
                j = last_writer.get(r)
                if j is not None:
                    deps[i][j] = "raw"
            for w in o["writes"]:
                j = last_writer.get(w)
                if j is not None and deps[i].get(j) != "raw":
                    deps[i][j] = "war"
                for j in readers.get(w, ()):
                    if j != i and deps[i].get(j) != "raw":
                        deps[i][j] = "war"
            for r in o["reads"]:
                readers.setdefault(r, []).append(i)
            for w in o["writes"]:
                last_writer[w] = i
                readers[w] = []
        for i, o in enumerate(ops):
            if o.get("barrier"):
                continue
            for j in list(deps[i].keys()):
                p = ops[j]
                if p["dma"]:
                    continue
                if p["eng"] == o["eng"] and not o["dma"]:
                    if o["eng"] == "pe" or (deps[i][j] == "war" and not STRICT_SAME_ENGINE):
                        del deps[i][j]
        dma_keys = {}
        for i, o in enumerate(ops):
            if o.get("barrier"):
                continue
            if o["dma"]:
                k = o["semkey"]
                if k is None:
                    k = o["writes"][0] if (o["writes"] and not o["writes"][0].startswith("dram:")) else o["reads"][0]
                o["_key"] = k
                dma_keys.setdefault(k, 0)
                dma_keys[k] += 1
                o["_cnt"] = 16 * dma_keys[k]
        needed = set()
        for i in range(n):
            for j in deps[i]:
                if not ops[j]["dma"]:
                    needed.add(j)
        cnt = {e: 0 for e in self.ENGS}
        for i, o in enumerate(ops):
            if o.get("barrier"):
                continue
            if not o["dma"] and i in needed:
                cnt[o["eng"]] += 1
                o["_cnt"] = cnt[o["eng"]]
        sems = {}
        for e in self.ENGS:
            sems["eng:" + e] = self.es.enter_context(nc.semaphore("sem_" + e))
        for k in dma_keys:
            sems["dma:" + k] = self.es.enter_context(nc.semaphore("dsem_%d" % len(sems)))
        assert len(sems) <= 100, "too many semaphores: %d" % len(sems)
        per_eng = {e: [] for e in self.ENGS}
        for i, o in enumerate(ops):
            if not o.get("barrier"):
                per_eng[o["eng"]].append(i)
        final_waits = [("dma:" + k, 16 * v) for k, v in dma_keys.items()]

        def run_engine(ename, eobj):
            waited = {}
            for i in per_eng[ename]:
                o = ops[i]
                need = {}
                for j in deps[i]:
                    p = ops[j]
                    sk = ("dma:" + p["_key"]) if p["dma"] else ("eng:" + p["eng"])
                    need[sk] = max(need.get(sk, 0), p["_cnt"])
                for sk, v in need.items():
                    if waited.get(sk, 0) < v:
                        eobj.wait_ge(sems[sk], v)
                        waited[sk] = v
                ins = o["fn"](eobj)
                if o["dma"]:
                    ins.then_inc(sems["dma:" + o["_key"]], 16)
                elif i in needed:
                    ins.then_inc(sems["eng:" + ename], 1)
            if ename == "sp":
                for sk, v in final_waits:
                    if waited.get(sk, 0) < v:
                        eobj.wait_ge(sems[sk], v)

        with nc.Block() as block:
            @block.tensor
            def _(e):
                run_engine("pe", e)

            @block.scalar
            def _(e):
                run_engine("act", e)

            @block.vector
            def _(e):
                run_engine("dve", e)

            @block.gpsimd
            def _(e):
                run_engine("pool", e)

            @block.sync
            def _(e):
                run_engine("sp", e)
        self.pes.close()
        self.mes.close()
        self.es.close()


CH = 342
NCH = TOK_PER_CORE // CH


def col_layout(v):
    v = np.ascontiguousarray(v, dtype=np.float32)
    return np.ascontiguousarray(v.reshape(-1, 128).T)


def emit_layernorm(P, x32, xres, N, ones_f, g_col, b_col, out32, outres, tag, out16=None, out16res=None):
    ps_m, rm = P._ln_ps[0]
    ps_q, rq = P._ln_ps[1]
    sq = P._ln_sq
    mean = P._ln_mean
    rstd = P._ln_rstd
    tmp = P._ln_tmp
    for k in range(8):
        P.op("pe", lambda e, k=k: e.matmul(ps_m[:, :N], ones_f[:], x32[:, k, :N], start=(k == 0), stop=(k == 7)),
             reads=[xres, "ones_f"], writes=[rm])
    for k in range(8):
        sb_ = sq[k % 2]
        sr = "ln_sq%d" % (k % 2)
        P.op("act", lambda e, k=k, sb_=sb_: e.activation(out=sb_[:, :N], in_=x32[:, k, :N], func=AF.Square), reads=[xres], writes=[sr])
        P.op("pe", lambda e, k=k, sb_=sb_: e.matmul(ps_q[:, :N], ones_f[:], sb_[:, :N], start=(k == 0), stop=(k == 7)),
             reads=[sr, "ones_f"], writes=[rq])
    P.op("act", lambda e: e.copy(out=mean[:, :N], in_=ps_m[:, :N]), reads=[rm], writes=["ln_mean"])
    P.op("dve", lambda e: e.tensor_tensor(out=rstd[:, :N], in0=mean[:, :N], in1=mean[:, :N], op=ALU.mult), reads=["ln_mean"], writes=["ln_rstd"])
    P.op("dve", lambda e: e.tensor_tensor(out=rstd[:, :N], in0=ps_q[:, :N], in1=rstd[:, :N], op=ALU.subtract), reads=[rq, "ln_rstd"], writes=["ln_rstd"])
    P.op("dve", lambda e: e.tensor_scalar(out=rstd[:, :N], in0=rstd[:, :N], scalar1=LN_EPS, scalar2=None, op0=ALU.add), reads=["ln_rstd"], writes=["ln_rstd"])
    P.op("act", lambda e: e.sqrt(out=rstd[:, :N], in_=rstd[:, :N]), reads=["ln_rstd"], writes=["ln_rstd"])
    P.op("dve", lambda e: e.reciprocal(out=rstd[:, :N], in_=rstd[:, :N]), reads=["ln_rstd"], writes=["ln_rstd"])
    for k in range(8):
        eng = "dve" if k % 2 == 0 else "pool"
        tb = tmp[k % 2]
        tr = "ln_tmp%d" % (k % 2)
        P.op(eng, lambda e, k=k, tb=tb: e.tensor_tensor(out=tb[:, :N], in0=x32[:, k, :N], in1=mean[:, :N], op=ALU.subtract),
             reads=[xres, "ln_mean"], writes=[tr])
        P.op(eng, lambda e, k=k, tb=tb: e.tensor_tensor(out=tb[:, :N], in0=tb[:, :N], in1=rstd[:, :N], op=ALU.mult),
             reads=[tr, "ln_rstd"], writes=[tr])
        P.op("dve", lambda e, k=k, tb=tb: e.tensor_scalar(out=out32[:, k, :N], in0=tb[:, :N], scalar1=g_col[:, k:k + 1], scalar2=b_col[:, k:k + 1],
                                                          op0=ALU.mult, op1=ALU.add),
             reads=[tr, "ln_gb_" + tag], writes=[outres])
        if out16 is not None:
            P.op("act", lambda e, k=k: e.copy(out=out16[:, k, :N], in_=out32[:, k, :N]), reads=[outres], writes=[out16res])


def ln_alloc(P, ps=None):
    if ps is None:
        ps = [(P.ps("ln_ps_m", [128, 512]), "ln_ps_m"), (P.ps("ln_ps_q", [128, 512]), "ln_ps_q")]
    P._ln_ps = ps
    P._ln_sq = [P.sb("ln_sq%d" % i, [128, CH], F32) for i in range(2)]
    P._ln_mean = P.sb("ln_mean", [128, CH], F32)
    P._ln_rstd = P.sb("ln_rstd", [128, CH], F32)
    P._ln_tmp = [P.sb("ln_tmp%d" % i, [128, CH], F32) for i in range(2)]


def emit_ident(P, ident, res="ident"):
    it = P.sb("ident_i", [128, 128], I32)
    P.op("pool", lambda e: e.iota(it[:], pattern=[[1, 128]], base=0, channel_multiplier=-1), writes=["ident_i"])
    P.op("dve", lambda e: e.tensor_single_scalar(out=ident[:], in_=it[:], scalar=0, op=ALU.is_equal), reads=["ident_i"], writes=[res])


def emit_B1(P, dr, h1T):
    nc = P.nc
    N = CH
    hT_v = dr["hT"].rearrange("(k p) t -> p k t", p=128)
    ssm_v = dr["ssmT"].rearrange("(k p) t -> p k t", p=128)
    conv_v = dr["convT"].rearrange("(k p) t -> p k t", p=128)
    attn_v = dr["attnT"].rearrange("(k p) t -> p k t", p=128)
    h1T_v = h1T.rearrange("(k p) t -> p k t", p=128)
    wglu = P.sb("wglu", [128, 4, 512], BF16)
    wsout = P.sb("wsout", [128, 4, 1024], BF16)
    wcout = P.sb("wcout", [128, 4, 1024], BF16)
    waout = P.sb("waout", [128, 8, 1024], BF16)
    wgate = P.sb("wgate", [128, 8, 3072], BF16)
    wo = P.sb("wo", [128, 8, 1024], BF16)
    for t, name, src in ((wglu, "wglu", "w_glu"), (wsout, "wsout", "w_sout"), (wcout, "wcout", "w_cout"),
                         (waout, "waout", "w_aout"), (wo, "wo", "w_o")):
        P.dma("pool", t[:], dr[src].rearrange("(k p) n -> p k n", p=128), writes=[name])
    gv = dr["w_gate"].rearrange("(k p) n -> p k n", p=128)
    for i in range(3):
        P.dma("pool", wgate[:, :, i * 1024:(i + 1) * 1024], gv[:, :, i * 1024:(i + 1) * 1024], writes=["wgate%d" % i])
    gb = P.sb("gb", [128, 24], F32)
    P.dma("sp", gb[:], dr["gate_b_c"], writes=["gb"])
    lg = P.sb("lng", [128, 8], F32)
    lb = P.sb("lnb", [128, 8], F32)
    P.dma("sp", lg[:], dr["ln1_g_c"], writes=["ln_gb_1"])
    P.dma("sp", lb[:], dr["ln1_b_c"], writes=["ln_gb_1"], semkey="lnb1")
    ones_f = P.sb("ones_f", [128, 128], F32)
    P.op("dve", lambda e: e.memset(ones_f[:], 1.0 / D), writes=["ones_f"])
    ln_alloc(P)
    h32_b = [P.sb("h32_0", [128, 8, N], F32)] * 2
    h16 = P.sb("h16", [128, 8, N], BF16)
    s32_b = [P.sb("s32_%d" % i, [128, 4, N], F32) for i in range(2)]
    s16 = P.sb("s16", [128, 4, N], BF16)
    c16_b = [P.sb("c16_%d" % i, [128, 4, N], BF16) for i in range(2)]
    a16_b = [P.sb("a16_%d" % i, [128, 8, N], BF16) for i in range(2)]

    def b1_load(c):
        t0 = c * N
        i = c % 2
        P.dma("sp", s32_b[i][:], ssm_v[:, :, t0:t0 + N], writes=["s32_%d" % i])
        P.dma("pool", c16_b[i][:], conv_v[:, :, t0:t0 + N], writes=["c16_%d" % i])
        P.dma("pool", a16_b[i][:], attn_v[:, :, t0:t0 + N], writes=["a16_%d" % i])
    sg16 = P.sb("sg16", [128, 4, N], BF16)
    sig = [P.sb("sig%d" % i, [128, N], F32) for i in range(2)]
    gt = [P.sb("gt%d" % i, [128, N], F32) for i in range(2)]
    macc = P.sb("macc", [128, N], F32)
    mtmp = [P.sb("mtmp%d" % i, [128, N], F32) for i in range(2)]
    m16 = P.sb("m16", [128, 8, N], BF16)
    res32 = P.sb("res32", [128, 8, N], F32)
    o32 = P.sb("o32", [128, 8, N], F32)
    psY = [P.ps("psY%d" % i, [128, 512]) for i in range(2)]
    psG = [P.ps("psG%d" % i, [128, 512]) for i in range(2)]
    cnt = 0
    b1_load(0)
    P.dma("sp", h32_b[0][:], hT_v[:, :, 0:N], writes=["h32_0"])
    for c in range(NCH):
        t0 = c * N
        if c + 1 < NCH:
            b1_load(c + 1)
        h32, s32, c16, a16 = h32_b[c % 2], s32_b[c % 2], c16_b[c % 2], a16_b[c % 2]
        H32, S32, C16, A16 = "h32_0", "s32_%d" % (c % 2), "c16_%d" % (c % 2), "a16_%d" % (c % 2)
        P.op("act", lambda e, h32=h32: e.copy(out=h16[:], in_=h32[:]), reads=[H32], writes=["h16"])
        P.op("act", lambda e, s32=s32: e.copy(out=s16[:], in_=s32[:]), reads=[S32], writes=["s16"])
        for j in range(4):
            b = cnt % 2
            cnt += 1
            for k in range(4):
                P.op("pe", lambda e, j=j, k=k, b=b: e.matmul(psY[b][:, :N], wglu[:, k, j * 128:(j + 1) * 128], s16[:, k, :], start=(k == 0), stop=(k == 3)),
                     reads=["wglu", "s16"], writes=["psY%d" % b])
            P.op("act", lambda e, b=b: e.activation(out=sig[b][:], in_=psY[b][:, :N], func=AF.Sigmoid), reads=["psY%d" % b], writes=["sig%d" % b])
            P.op("dve", lambda e, j=j, b=b, s32=s32: e.tensor_tensor(out=sg16[:, j, :], in0=s32[:, j, :], in1=sig[b][:], op=ALU.mult),
                 reads=[S32, "sig%d" % b], writes=["sg16"])
        branches = ((wsout, "wsout", sg16, "sg16", 4), (wcout, "wcout", c16, C16, 4), (waout, "waout", a16, A16, 8))
        for m in range(8):
            for br, (wt, wname, xt, xname, nk) in enumerate(branches):
                b = cnt % 2
                cnt += 1
                for k in range(nk):
                    P.op("pe", lambda e, wt=wt, xt=xt, k=k, m=m, b=b, nk=nk: e.matmul(psY[b][:, :N], wt[:, k, m * 128:(m + 1) * 128], xt[:, k, :],
                                                                                    start=(k == 0), stop=(k == nk - 1)),
                         reads=[wname, xname], writes=["psY%d" % b])
                gc0 = br * 1024 + m * 128
                for k in range(8):
                    P.op("pe", lambda e, k=k, gc0=gc0, b=b: e.matmul(psG[b][:, :N], wgate[:, k, gc0:gc0 + 128], h16[:, k, :], start=(k == 0), stop=(k == 7)),
                         reads=["wgate%d" % br, "h16"], writes=["psG%d" % b])
                P.op("act", lambda e, b=b, br=br, m=m: e.activation(out=gt[b][:], in_=psG[b][:, :N], func=AF.Sigmoid, bias=gb[:, br * 8 + m:br * 8 + m + 1]),
                     reads=["psG%d" % b, "gb"], writes=["gt%d" % b])
                if br == 0:
                    P.op("dve", lambda e, b=b: e.tensor_tensor(out=macc[:], in0=gt[b][:], in1=psY[b][:, :N], op=ALU.mult),
                         reads=["gt%d" % b, "psY%d" % b], writes=["macc"])
                else:
                    P.op("dve", lambda e, b=b: e.tensor_tensor(out=mtmp[b][:], in0=gt[b][:], in1=psY[b][:, :N], op=ALU.mult),
                         reads=["gt%d" % b, "psY%d" % b], writes=["mtmp%d" % b])
                    if br == 1:
                        P.op("pool", lambda e, b=b: e.tensor_tensor(out=macc[:], in0=macc[:], in1=mtmp[b][:], op=ALU.add),
                             reads=["macc", "mtmp%d" % b], writes=["macc"])
                    else:
                        P.op("pool", lambda e, b=b, m=m: e.tensor_tensor(out=m16[:, m, :], in0=macc[:], in1=mtmp[b][:], op=ALU.add),
                             reads=["macc", "mtmp%d" % b], writes=["m16"])
        for m in range(8):
            b = cnt % 2
            cnt += 1
            for k in range(8):
                P.op("pe", lambda e, k=k, m=m, b=b: e.matmul(psY[b][:, :N], wo[:, k, m * 128:(m + 1) * 128], m16[:, k, :], start=(k == 0), stop=(k == 7)),
                     reads=["wo", "m16"], writes=["psY%d" % b])
            P.op("dve", lambda e, m=m, b=b, h32=h32: e.scalar_tensor_tensor(out=res32[:, m, :], in0=h32[:, m, :], scalar=DN_ALPHA, in1=psY[b][:, :N],
                                                                   op0=ALU.mult, op1=ALU.add),
                 reads=[H32, "psY%d" % b], writes=["res32"])
        if c + 1 < NCH:
            P.dma("sp", h32_b[0][:], hT_v[:, :, t0 + N:t0 + 2 * N], writes=["h32_0"])
        emit_layernorm(P, res32, "res32", N, ones_f, lg, lb, o32, "o32", "1")
        P.dma("sp", h1T_v[:, :, t0:t0 + N], o32[:], reads=["o32"], writes=["dram:h1T"])


def emit_B2(P, dr, h1T, cwT_d, outT, n_experts=NE):
    nc = P.nc
    N = CH
    TOK = TOK_PER_CORE
    h1T_v = h1T.rearrange("(k p) t -> p k t", p=128)
    outT_v = outT.rearrange("(k p) t -> p k t", p=128)
    h16 = P.sbm("h16", [128, 8, TOK], BF16)
    acc = P.sbm("acc", [128, 8, TOK], F32)
    bgu = P.sbm("bgu", [128, NE, 16], F32)
    lg2 = P.sbm("lng2", [128, 8], F32)
    lb2 = P.sbm("lnb2", [128, 8], F32)
    ones_f = P.sbm("ones_f", [128, 128], F32)
    P.dma("sp", bgu[:], dr["bgu_c"], writes=["bgu"])
    P.dma("sp", lg2[:], dr["ln2_g_c"], writes=["ln_gb_2"])
    P.dma("sp", lb2[:], dr["ln2_b_c"], writes=["ln_gb_2"], semkey="lnb2")
    P.op("dve", lambda e: e.memset(ones_f[:], 1.0 / D), writes=["ones_f"])
    P.op("dve", lambda e: e.tensor_scalar(out=bgu[:, :, 8:16], in0=bgu[:, :, 8:16], scalar1=1.0, scalar2=None, op0=ALU.add),
         reads=["bgu"], writes=["bgu"])
    ident = P.sb("ident", [128, 128], F32)
    emit_ident(P, ident)
    rw = P.sb("rw", [128, 8, NE], F32)
    P.dma("sp", rw[:], dr["router_w"].rearrange("(k p) e -> p k e", p=128), writes=["rw"])
    rb = P.sb("rb", [128, NE], F32)
    P.dma("sp", rb[:], dr["router_b_bc"], writes=["rb"])
    bd = P.sb("bd", [NE, D], F32)
    P.dma("sp", bd[:], dr["b_down"], writes=["bd"])
    cwT = P.sb("cwT", [NE, TOK], F32)
    h32 = [P.sb("h32_%d" % i, [128, 8, N], F32) for i in range(2)]
    lgt = P.sb("lgt", [128, NE], F32)
    m8 = P.sb("m8", [128, 8], F32)
    negm = P.sb("negm", [128, 1], F32)
    ex = P.sb("ex", [128, NE], F32)
    exm = P.sb("exm", [128, NE], F32)
    ssum = P.sb("ssum", [128, 1], F32)
    cw = P.sb("cw", [128, NE], F32)
    ps_r = P.ps("ps_r", [128, 512])
    ps_t = P.ps("ps_t", [128, 512])
    ps_b = [P.ps("ps_b%d" % i, [128, 512]) for i in range(2)]
    for c in range(NCH):
        t0 = c * N
        hb = h32[c % 2]
        hres = "h32_%d" % (c % 2)
        P.dma("sp", hb[:], h1T_v[:, :, t0:t0 + N], reads=["dram:h1T"], writes=[hres])
        P.op("act", lambda e, hb=hb, t0=t0: e.copy(out=h16[:, :, t0:t0 + N], in_=hb[:]), reads=[hres], writes=["h16"])
        off = 0
        while off < N:
            ts = min(128, N - off)
            for k in range(8):
                P.op("pe", lambda e, hb=hb, k=k, off=off, ts=ts: e.matmul(ps_r[:ts, :NE], hb[:, k, off:off + ts], rw[:, k, :], start=(k == 0), stop=(k == 7)),
                     reads=[hres, "rw"], writes=["ps_r"])
            P.op("dve", lambda e, ts=ts: e.tensor_tensor(out=lgt[:ts, :], in0=ps_r[:ts, :NE], in1=rb[:ts, :], op=ALU.add), reads=["ps_r", "rb"], writes=["lgt"])
            P.op("dve", lambda e, ts=ts: e.max(out=m8[:ts, :], in_=lgt[:ts, :]), reads=["lgt"], writes=["m8"])
            P.op("dve", lambda e, ts=ts: e.tensor_scalar(out=negm[:ts, :], in0=m8[:ts, 0:1], scalar1=-1.0, scalar2=None, op0=ALU.mult), reads=["m8"], writes=["negm"])
            P.op("act", lambda e, ts=ts: e.activation(out=ex[:ts, :], in_=lgt[:ts, :], func=AF.Exp, bias=negm[:ts, :]), reads=["lgt", "negm"], writes=["ex"])
            P.op("dve", lambda e, ts=ts: e.scalar_tensor_tensor(out=exm[:ts, :], in0=lgt[:ts, :], scalar=m8[:ts, 3:4], in1=ex[:ts, :], op0=ALU.is_ge, op1=ALU.mult,
                                                                accum_out=ssum[:ts, :]),
                 reads=["lgt", "m8", "ex"], writes=["exm", "ssum"])
            P.op("dve", lambda e, ts=ts: e.reciprocal(out=ssum[:ts, :], in_=ssum[:ts, :]), reads=["ssum"], writes=["ssum"])
            P.op("dve", lambda e, ts=ts: e.tensor_scalar(out=cw[:ts, :], in0=exm[:ts, :], scalar1=ssum[:ts, 0:1], scalar2=None, op0=ALU.mult), reads=["exm", "ssum"], writes=["cw"])
            P.op("pe", lambda e, ts=ts: e.transpose(ps_t[:NE, :ts], cw[:ts, :], ident[:ts, :ts]), reads=["cw", "ident"], writes=["ps_t"])
            P.op("act", lambda e, ts=ts, a=t0 + off: e.copy(out=cwT[:, a:a + ts], in_=ps_t[:NE, :ts]), reads=["ps_t"], writes=["cwT"])
            off += ts
        for m in range(8):
            b = m % 2
            P.op("pe", lambda e, m=m, b=b, t0=t0: e.matmul(ps_b[b][:, :N], bd[:, m * 128:(m + 1) * 128], cwT[:, t0:t0 + N], start=True, stop=True),
                 reads=["bd", "cwT"], writes=["ps_b%d" % b])
            P.op("dve", lambda e, m=m, b=b, hb=hb, t0=t0: e.scalar_tensor_tensor(out=acc[:, m, t0:t0 + N], in0=hb[:, m, :], scalar=DN_ALPHA, in1=ps_b[b][:, :N],
                                                                                 op0=ALU.mult, op1=ALU.add),
                 reads=[hres, "ps_b%d" % b], writes=["acc%d" % c])
    P.dma("sp", cwT_d, cwT[:], reads=["cwT"], writes=["dram:cwT"])
    P.new_phase(keep_mid=True)
    hid = P.sb("hid", [128, 8, TOK], BF16)
    NSLOT = 4
    ring = [P.sb("ring%d" % i, [128, 8, 512], BF16) for i in range(NSLOT)]
    cwB = [P.sb("cwB%d" % i, [128, TOK], F32) for i in range(2)]
    gc = [P.sb("gc%d" % i, [128, N], F32) for i in range(3)]
    sg = [P.sb("sg%d" % i, [128, N], F32) for i in range(3)]
    uc = [P.sb("uc%d" % i, [128, N], F32) for i in range(3)]
    tt = [P.sb("tt%d" % i, [128, N], F32) for i in range(3)]
    hd = [P.sb("hd%d" % i, [128, N], F32) for i in range(3)]
    psG = [P.ps("psG%d" % i, [128, 512]) for i in range(3)]
    psU = [P.ps("psU%d" % i, [128, 512]) for i in range(3)]
    psO = [P.ps("psO%d" % i, [128, 512]) for i in range(2)]
    it = 0
    io = 0
    wunits = [(ex_i, kind, idx) for ex_i in range(n_experts) for kind, idx in (("gu", 0), ("gu", 1), ("gu", 2), ("gu", 3), ("dn", 0), ("dn", 1))]

    def load_unit(u):
        ex_i, kind, idx = wunits[u]
        W = ring[u % NSLOT]
        wres = "ring%d" % (u % NSLOT)
        if kind == "gu":
            wgu_v = dr["w_gu"][ex_i].rearrange("(k p) n -> p k n", p=128)
            P.dma("pool", W[:, :, 0:256], wgu_v[:, :, idx * 256:(idx + 1) * 256], writes=[wres])
            P.dma("pool", W[:, :, 256:512], wgu_v[:, :, DFF + idx * 256:DFF + (idx + 1) * 256], writes=[wres], semkey=wres + "u")
        else:
            wd_v = dr["w_down"][ex_i].rearrange("(k p) n -> p k n", p=128)
            P.dma("pool", W[:], wd_v[:, :, idx * 512:(idx + 1) * 512], writes=[wres])

    PF = 2
    for u in range(min(PF, len(wunits))):
        load_unit(u)
    for u, (ex_i, kind, idx) in enumerate(wunits):
        if u + PF < len(wunits):
            load_unit(u + PF)
        W = ring[u % NSLOT]
        wres = "ring%d" % (u % NSLOT)
        cb = cwB[ex_i % 2]
        cres = "cwB%d" % (ex_i % 2)
        if kind == "gu" and idx == 0:
            P.dma("sp", cb[:], cwT_d[ex_i:ex_i + 1, :].partition_broadcast(128), reads=["dram:cwT"], writes=[cres])
        if kind == "gu":
            q = idx
            for c in range(NCH):
                t0 = c * N
                for jj in range(2):
                    j = 2 * q + jj
                    b = it % 3
                    it += 1
                    for k in range(8):
                        P.op("pe", lambda e, W=W, k=k, jj=jj, b=b, t0=t0: e.matmul(psG[b][:, :N], W[:, k, jj * 128:(jj + 1) * 128], h16[:, k, t0:t0 + N],
                                                                                  start=(k == 0), stop=(k == 7)),
                             reads=[wres, "h16"], writes=["psG%d" % b])
                    for k in range(8):
                        P.op("pe", lambda e, W=W, k=k, jj=jj, b=b, t0=t0: e.matmul(psU[b][:, :N], W[:, k, 256 + jj * 128:256 + (jj + 1) * 128], h16[:, k, t0:t0 + N],
                                                                                  start=(k == 0), stop=(k == 7)),
                             reads=[wres, "h16"], writes=["psU%d" % b])
                    P.op("dve", lambda e, b=b, j=j, ex_i=ex_i: e.tensor_scalar(out=gc[b][:], in0=psG[b][:, :N], scalar1=bgu[:, ex_i, j:j + 1], scalar2=SW_LIMIT,
                                                                               op0=ALU.add, op1=ALU.min),
                         reads=["psG%d" % b, "bgu"], writes=["gc%d" % b])
                    P.op("act", lambda e, b=b: e.activation(out=sg[b][:], in_=gc[b][:], func=AF.Sigmoid, scale=SW_ALPHA), reads=["gc%d" % b], writes=["sg%d" % b])
                    P.op("dve", lambda e, b=b, j=j, ex_i=ex_i: e.tensor_scalar(out=uc[b][:], in0=psU[b][:, :N], scalar1=bgu[:, ex_i, 8 + j:9 + j], scalar2=SW_LIMIT + 1.0,
                                                                               op0=ALU.add, op1=ALU.min),
                         reads=["psU%d" % b, "bgu"], writes=["uc%d" % b])
                    P.op("pool", lambda e, b=b: e.tensor_tensor(out=tt[b][:], in0=gc[b][:], in1=sg[b][:], op=ALU.mult),
                         reads=["gc%d" % b, "sg%d" % b], writes=["tt%d" % b])
                    P.op("dve", lambda e, b=b: e.scalar_tensor_tensor(out=hd[b][:], in0=uc[b][:], scalar=-SW_LIMIT + 1.0, in1=tt[b][:], op0=ALU.max, op1=ALU.mult),
                         reads=["uc%d" % b, "tt%d" % b], writes=["hd%d" % b])
                    P.op("pool", lambda e, b=b, j=j, cb=cb, t0=t0: e.tensor_tensor(out=hid[:, j, t0:t0 + N], in0=hd[b][:], in1=cb[:, t0:t0 + N], op=ALU.mult),
                         reads=["hd%d" % b, cres], writes=["hid%d" % c])
        else:
            mh = idx
            for c in range(NCH):
                t0 = c * N
                for mm in range(4):
                    m = 4 * mh + mm
                    b = io % 2
                    io += 1
                    for j in range(8):
                        P.op("pe", lambda e, W=W, j=j, mm=mm, b=b, t0=t0: e.matmul(psO[b][:, :N], W[:, j, mm * 128:(mm + 1) * 128], hid[:, j, t0:t0 + N],
                                                                                  start=(j == 0), stop=(j == 7)),
                             reads=[wres, "hid%d" % c], writes=["psO%d" % b])
                    P.op("dve", lambda e, m=m, b=b, t0=t0: e.tensor_tensor(out=acc[:, m, t0:t0 + N], in0=acc[:, m, t0:t0 + N], in1=psO[b][:, :N], op=ALU.add),
                         reads=["acc%d" % c, "psO%d" % b], writes=["acc%d" % c])
    P.new_phase(keep_mid=True)
    ln_alloc(P)
    o32 = [P.sb("o32_%d" % i, [128, 8, N], F32) for i in range(2)]
    for c in range(NCH):
        t0 = c * N
        ob = o32[c % 2]
        ores = "o32_%d" % (c % 2)
        emit_layernorm(P, acc[:, :, t0:t0 + N], "acc%d" % c, N, ones_f, lg2, lb2, ob, ores, "2")
        P.dma("sp", outT_v[:, :, t0:t0 + N], ob[:], reads=[ores], writes=["dram:outT"])


B_INPUTS = [("hT", [D, TOK_PER_CORE]), ("ssmT", [SSM_W, TOK_PER_CORE]), ("convT", [CONV_W, TOK_PER_CORE]), ("attnT", [ATT_W, TOK_PER_CORE]),
            ("w_glu", [SSM_W, SSM_W]), ("w_sout", [SSM_W, D]), ("w_cout", [CONV_W, D]), ("w_aout", [ATT_W, D]), ("w_gate", [D, 3 * D]),
            ("gate_b_c", [128, 24]), ("w_o", [D, D]), ("ln1_g_c", [128, 8]), ("ln1_b_c", [128, 8]), ("router_w", [D, NE]),
            ("router_b_bc", [128, NE]), ("w_gu", [NE, D, 2 * DFF]), ("bgu_c", [128, NE, 16]), ("w_down", [NE, DFF, D]), ("b_down", [NE, D]),
            ("ln2_g_c", [128, 8]), ("ln2_b_c", [128, 8])]


def build_B(n_experts=NE):
    nc = bass.Bass("TRN2", target_bir_lowering=False)
    dr = {name: nc.dram_tensor(name, shape, F32, kind="ExternalInput").ap() for name, shape in B_INPUTS}
    outT = nc.dram_tensor("outT", [D, TOK_PER_CORE], F32, kind="ExternalOutput").ap()
    h1T = nc.dram_tensor("h1T_scr", [D, TOK_PER_CORE], F32, kind="Internal").ap()
    cwT_d = nc.dram_tensor("cwT_scr", [NE, TOK_PER_CORE], F32, kind="Internal").ap()
    P = Prog(nc)
    emit_B1(P, dr, h1T)
    P.new_phase()
    emit_B2(P, dr, h1T, cwT_d, outT, n_experts=n_experts)
    P.emit()
    return nc


def host_B_weights(inp, l):
    f = np.float32
    bgu = np.asarray(inp["expert_b_gu"][l], f)
    return {
        "w_glu": np.ascontiguousarray(inp["ssm_w_glu"][l], f), "w_sout": np.ascontiguousarray(inp["ssm_w_out"][l], f),
        "w_cout": np.ascontiguousarray(inp["conv_w_out"][l], f), "w_aout": np.ascontiguousarray(inp["attn_w_out"][l], f),
        "w_gate": np.ascontiguousarray(inp["gate_w"][l], f), "gate_b_c": col_layout(inp["gate_b"][l]),
        "w_o": np.ascontiguousarray(inp["w_o"][l], f), "ln1_g_c": col_layout(inp["ln1_g"][l]), "ln1_b_c": col_layout(inp["ln1_b"][l]),
        "router_w": np.ascontiguousarray(inp["router_w"][l], f),
        "router_b_bc": np.ascontiguousarray(np.broadcast_to(np.asarray(inp["router_b"][l], f)[None, :], (128, NE))),
        "w_gu": np.ascontiguousarray(inp["expert_w_gu"][l], f),
        "bgu_c": np.ascontiguousarray(bgu.reshape(NE, 16, 128).transpose(2, 0, 1)),
        "w_down": np.ascontiguousarray(inp["expert_w_down"][l], f), "b_down": np.ascontiguousarray(inp["expert_b_down"][l], f),
        "ln2_g_c": col_layout(inp["ln2_g"][l]), "ln2_b_c": col_layout(inp["ln2_b"][l]),
    }


CW1 = 6.28125
CW2 = TWO_PI - 6.28125


def sincos_alloc(P, shape, tag):
    p, n = shape
    return [P.sb("sc_%s_%s" % (x, tag), [p, n], I32 if x == "k" else F32) for x in ("y", "k", "kf", "r", "r2", "m")]


def emit_sincos(P, scr, ang, ares, cos_out, cres, sin_out, sres, tag):
    y, ki, kf, r, r2, m = scr
    ry, rk, rkf, rr, rr2, rm = ["sc_%s_%s" % (x, tag) for x in ("y", "k", "kf", "r", "r2", "m")]
    P.op("dve", lambda e: e.tensor_scalar(out=y[:], in0=ang, scalar1=1.0 / TWO_PI, scalar2=None, op0=ALU.mult), reads=[ares], writes=[ry])
    P.op("dve", lambda e: e.tensor_copy(out=ki[:], in_=y[:]), reads=[ry], writes=[rk])
    P.op("dve", lambda e: e.tensor_copy(out=kf[:], in_=ki[:]), reads=[rk], writes=[rkf])
    P.op("dve", lambda e: e.scalar_tensor_tensor(out=r[:], in0=kf[:], scalar=-CW1, in1=ang, op0=ALU.mult, op1=ALU.add), reads=[rkf, ares], writes=[rr])
    P.op("dve", lambda e: e.scalar_tensor_tensor(out=r[:], in0=kf[:], scalar=-CW2, in1=r[:], op0=ALU.mult, op1=ALU.add), reads=[rkf, rr], writes=[rr])
    P.op("dve", lambda e: e.tensor_scalar(out=m[:], in0=r[:], scalar1=math.pi / 2, scalar2=-TWO_PI, op0=ALU.is_gt, op1=ALU.mult), reads=[rr], writes=[rm])
    P.op("dve", lambda e: e.scalar_tensor_tensor(out=r2[:], in0=r[:], scalar=math.pi / 2, in1=m[:], op0=ALU.add, op1=ALU.add), reads=[rr, rm], writes=[rr2])
    P.op("dve", lambda e: e.tensor_scalar(out=r[:], in0=r[:], scalar1=-math.pi, scalar2=math.pi, op0=ALU.max, op1=ALU.min), reads=[rr], writes=[rr])
    P.op("dve", lambda e: e.tensor_scalar(out=r2[:], in0=r2[:], scalar1=-math.pi, scalar2=math.pi, op0=ALU.max, op1=ALU.min), reads=[rr2], writes=[rr2])
    P.op("act", lambda e: e.activation(out=sin_out, in_=r[:], func=AF.Sin), reads=[rr], writes=[sres])
    P.op("act", lambda e: e.activation(out=cos_out, in_=r2[:], func=AF.Sin), reads=[rr2], writes=[cres])


NCHA = 17
NSUB = 3
SSM_EVERY = 10
NEG = -1.0e30


def chunkA(c):
    t0 = c * 512
    return t0, min(512, LP - t0)


def emit_A(P, dr, lam_init):
    nc = P.nc
    hT_v = dr["hT"].rearrange("(k p) t -> p k t", p=128)
    uT = P.sbm("uT", [128, LP], BF16)
    qT = [P.sbm("qT%d" % h, [128, LP], BF16) for h in range(2)]
    kT = [P.sbm("kT%d" % h, [128, LP], BF16) for h in range(2)]
    Va = [P.sbm("Va%d" % h, [128, NBLK, 130], BF16) for h in range(2)]
    ident = P.sbm("ident", [128, 128], F32)
    ident16 = P.sbm("ident16", [128, 128], BF16)
    emit_ident(P, ident)
    P.op("act", lambda e: e.copy(out=ident16[:], in_=ident[:]), reads=["ident"], writes=["ident16"])
    w16 = P.sb("w16", [128, 8, 1280], BF16)
    wv = dr["w_sel"].rearrange("(k p) n -> p k n", p=128)
    for i in range(2):
        P.dma("pool", w16[:, :, i * 640:(i + 1) * 640], wv[:, :, i * 640:(i + 1) * 640], writes=["w16_%d" % i])
    WR = ["w16_0", "w16_1"]
    wrot = P.sb("wrot", [128, 8, 512], BF16)
    for t in range(4):
        src0 = 512 + t * 128
        for c2 in range(2):
            s = src0 + c2 * 64
            d = t * 128 + c2 * 64
            P.op("act", lambda e, s=s, d=d: e.activation(out=wrot[:, :, d:d + 32], in_=w16[:, :, s + 32:s + 64], func=AF.Copy, scale=-1.0),
                 reads=WR, writes=["wrot"])
            P.op("act", lambda e, s=s, d=d: e.copy(out=wrot[:, :, d + 32:d + 64], in_=w16[:, :, s:s + 32]), reads=WR, writes=["wrot"])
    invf = P.sb("invf", [128, 1], F32)
    P.dma("sp", invf[:], dr["inv_c"], writes=["invf"])
    cw3 = P.sb("cw3", [128, 3], F32)
    P.dma("sp", cw3[:], dr["conv_w_c"], writes=["cw3"])
    h16 = [P.sb("h16_%d" % i, [128, 8, 512], BF16) for i in range(2)]
    posi = P.sb("posi", [128, 512], I32)
    posf = P.sb("posf", [128, 512], F32)
    ang = P.sb("ang", [128, 512], F32)
    cosT = P.sb("cosT", [128, 512], F32)
    sinT = P.sb("sinT", [128, 512], F32)
    sc_rope = sincos_alloc(P, [128, 512], "rope")
    zb = P.sb("zb", [128, 514], F32)
    ccs = P.sb("ccs", [128, 512], F32)
    cy = P.sb("cy", [128, 512], F32)
    cyo = [P.sb("cyo%d" % i, [128, 512], F32) for i in range(2)]
    rq1 = P.sb("rq1", [128, 512], F32)
    rq2 = P.sb("rq2", [128, 512], F32)
    psA = [P.ps("psA%d" % i, [128, 512]) for i in range(4)]
    psV = [P.ps("psV%d" % i, [128, 512]) for i in range(2)]
    P.op("dve", lambda e: e.memset(zb[:, 0:2], 0.0), writes=["zb"])
    for h in range(2):
        P.op("pool", lambda e, h=h: e.memset(Va[h][:, :, 128:130], 1.0), writes=["Va%d" % h])
    P.op("pool", lambda e: e.iota(posi[:], pattern=[[1, 512]], base=0, channel_multiplier=0), writes=["posi"])
    P.op("dve", lambda e: e.tensor_copy(out=posf[:], in_=posi[:]), reads=["posi"], writes=["posf"])
    pa = 0
    convT_out = dr["convT_out"]

    def proj(wt, wres, col0, hb, hres, n, ps, pres):
        for k in range(8):
            P.op("pe", lambda e, k=k: e.matmul(ps[:, :n], wt[:, k, col0:col0 + 128], hb[:, k, :n], start=(k == 0), stop=(k == 7)),
                 reads=wres + [hres], writes=[pres])

    for c in range(NCHA):
        t0, n = chunkA(c)
        hb = h16[c % 2]
        hres = "h16_%d" % (c % 2)
        if c == 0:
            P.dma("pool", hb[:, :, :n], hT_v[:, :, t0:t0 + n], writes=[hres])
        if c + 1 < NCHA:
            t0n, nn = chunkA(c + 1)
            P.dma("pool", h16[(c + 1) % 2][:, :, :nn], hT_v[:, :, t0n:t0n + nn], writes=["h16_%d" % ((c + 1) % 2)])
        P.op("dve", lambda e, t0=t0: e.tensor_scalar(out=ang[:], in0=posf[:], scalar1=float(t0), scalar2=invf[:, 0:1], op0=ALU.add, op1=ALU.mult),
             reads=["posf", "invf"], writes=["ang"])
        emit_sincos(P, sc_rope, ang[:], "ang", cosT[:], "cosT", sinT[:], "sinT", "rope")
        ps = psA[pa % 4]; pres = "psA%d" % (pa % 4); pa += 1
        proj(w16, WR, 0, hb, hres, n, ps, pres)
        P.op("act", lambda e, ps=ps, t0=t0, n=n: e.copy(out=uT[:, t0:t0 + n], in_=ps[:, :n]), reads=[pres], writes=["uT"])
        psb = psA[pa % 4]; rb_ = "psA%d" % (pa % 4); pa += 1
        proj(w16, WR, 128, hb, hres, n, psb, rb_)
        psc = psA[pa % 4]; rc_ = "psA%d" % (pa % 4); pa += 1
        proj(w16, WR, 256, hb, hres, n, psc, rc_)
        psh = psA[pa % 4]; rh_ = "psA%d" % (pa % 4); pa += 1
        proj(w16, WR, 384, hb, hres, n, psh, rh_)
        P.op("act", lambda e, psc=psc, n=n: e.copy(out=ccs[:, :n], in_=psc[:, :n]), reads=[rc_], writes=["ccs"])
        P.op("dve", lambda e, psh=psh, n=n: e.tensor_tensor(out=zb[:, 2:2 + n], in0=ccs[:, :n], in1=psh[:, :n], op=ALU.mult), reads=["ccs", rh_, "zb"], writes=["zb"])
        P.op("dve", lambda e, n=n: e.tensor_scalar(out=cy[:, :n], in0=zb[:, 2:2 + n], scalar1=cw3[:, 2:3], scalar2=None, op0=ALU.mult), reads=["zb", "cw3"], writes=["cy"])
        P.op("dve", lambda e, n=n: e.scalar_tensor_tensor(out=cy[:, :n], in0=zb[:, 1:1 + n], scalar=cw3[:, 1:2], in1=cy[:, :n], op0=ALU.mult, op1=ALU.add),
             reads=["zb", "cw3", "cy"], writes=["cy"])
        P.op("dve", lambda e, n=n: e.scalar_tensor_tensor(out=cy[:, :n], in0=zb[:, 0:n], scalar=cw3[:, 0:1], in1=cy[:, :n], op0=ALU.mult, op1=ALU.add),
             reads=["zb", "cw3", "cy"], writes=["cy"])
        co = cyo[c % 2]; cores_ = "cyo%d" % (c % 2)
        P.op("dve", lambda e, psb=psb, n=n, co=co: e.tensor_tensor(out=co[:, :n], in0=cy[:, :n], in1=psb[:, :n], op=ALU.mult), reads=["cy", rb_], writes=[cores_])
        nv = min(n, L - t0)
        if nv > 0:
            P.dma("sp", convT_out[:, t0:t0 + nv], co[:, :nv], reads=[cores_], writes=["dram:convT_out"])
        P.op("act", lambda e, n=n: e.copy(out=zb[:, 0:2], in_=zb[:, n:n + 2]), reads=["zb"], writes=["zb"])
        for t in range(4):
            dst = (qT, kT)[t // 2][t % 2]
            dres = ("qT%d", "kT%d")[t // 2] % (t % 2)
            p1 = psA[pa % 4]; r1 = "psA%d" % (pa % 4); pa += 1
            proj(w16, WR, 512 + t * 128, hb, hres, n, p1, r1)
            p2 = psA[pa % 4]; r2 = "psA%d" % (pa % 4); pa += 1
            proj(wrot, ["wrot"], t * 128, hb, hres, n, p2, r2)
            P.op("dve", lambda e, p1=p1, n=n: e.tensor_tensor(out=rq1[:, :n], in0=cosT[:, :n], in1=p1[:, :n], op=ALU.mult), reads=["cosT", r1], writes=["rq1"])
            P.op("dve", lambda e, p2=p2, n=n: e.tensor_tensor(out=rq2[:, :n], in0=sinT[:, :n], in1=p2[:, :n], op=ALU.mult), reads=["sinT", r2], writes=["rq2"])
            P.op("pool", lambda e, dst=dst, t0=t0, n=n: e.tensor_tensor(out=dst[:, t0:t0 + n], in0=rq1[:, :n], in1=rq2[:, :n], op=ALU.add),
                 reads=["rq1", "rq2"], writes=[dres])
        for s in range(n // 128):
            pv = psV[s % 2]; rv = "psV%d" % (s % 2)
            for k in range(8):
                P.op("pe", lambda e, k=k, s=s, pv=pv, hb=hb: e.matmul(pv[:, :256], hb[:, k, s * 128:(s + 1) * 128], w16[:, k, 1024:1280], start=(k == 0), stop=(k == 7)),
                     reads=WR + [hres], writes=[rv])
            blk = t0 // 128 + s
            for h in range(2):
                P.op("act", lambda e, h=h, blk=blk, pv=pv: e.copy(out=Va[h][:, blk, 0:128], in_=pv[:, h * 128:(h + 1) * 128]), reads=[rv], writes=["Va%d" % h])
    P.new_phase(keep_mid=True)
    T = emit_A2_ssm(P, dr, uT, ident)
    P.new_phase(keep_mid=True)
    it_s = ssm_steps(P, dr, uT, T)
    k = 0
    for _ in attn_steps(P, dr, qT, kT, Va, ident16, lam_init):
        k += 1
        if k % SSM_EVERY == 0:
            next(it_s, None)
    for _ in it_s:
        pass


TC = 256
NCHS = (LP + TC - 1) // TC


def chunkS(c):
    t0 = c * TC
    return t0, min(TC, LP - t0)


def emit_A2_ssm(P, dr, uT, ident):
    nc = P.nc
    ssmT_out = dr["ssmT_out"]
    lr = P.sb("lr", [128, 4], F32)
    li = P.sb("li", [128, 4], F32)
    ldt = P.sb("ldt", [128, 4], F32)
    bre = P.sb("bre", [128, 4, 16], F32)
    bim = P.sb("bim", [128, 4, 16], F32)
    cre = P.sb("cre", [128, 4, 16], F32)
    cim = P.sb("cim", [128, 4, 16], F32)
    dcol = P.sb("dcol", [128, 1], F32)
    for t, nm, src in ((lr, "lr", "lam_re_c"), (li, "li", "lam_im_c"), (ldt, "ldt", "log_dt_c"), (bre, "bre", "b_re_c"), (bim, "bim", "b_im_c"),
                       (cre, "cre", "c_reT_c"), (cim, "cim", "c_imT_c"), (dcol, "dcol", "d_c")):
        P.dma("sp", t[:], dr[src], writes=[nm])
    V = lambda name: P.sb(name, [128, 4], F32)
    dt, x, mag, th, cth, sth, ar, ai, den, cfr, cfi, t1, t2 = [V(n) for n in ("dt", "x", "mag", "th", "cth", "sth", "ar", "ai", "den", "cfr", "cfi", "t1", "t2")]
    thT = V("thT")
    cTc = P.sbm("cTc", [128, 4], F32)
    sTc = P.sbm("sTc", [128, 4], F32)

    def ts(out, in0, s1, s2, op0, op1=None, rd=(), wr=()):
        if op1 is None:
            P.op("dve", lambda e: e.tensor_scalar(out=out, in0=in0, scalar1=s1, scalar2=None, op0=op0), reads=rd, writes=wr)
        else:
            P.op("dve", lambda e: e.tensor_scalar(out=out, in0=in0, scalar1=s1, scalar2=s2, op0=op0, op1=op1), reads=rd, writes=wr)

    def tt(out, a, b, op, rd=(), wr=(), eng="dve"):
        P.op(eng, lambda e: e.tensor_tensor(out=out, in0=a, in1=b, op=op), reads=rd, writes=wr)

    P.op("act", lambda e: e.activation(out=dt[:], in_=ldt[:], func=AF.Exp), reads=["ldt"], writes=["dt"])
    tt(x[:], lr[:], dt[:], ALU.mult, ["lr", "dt"], ["x"])
    ts(mag[:], x[:], 1.0 / 720, 1.0 / 120, ALU.mult, ALU.add, ["x"], ["mag"])
    for cf in (1.0 / 24, 1.0 / 6, 0.5, 1.0, 1.0):
        tt(mag[:], mag[:], x[:], ALU.mult, ["mag", "x"], ["mag"])
        ts(mag[:], mag[:], cf, None, ALU.add, None, ["mag"], ["mag"])
    tt(th[:], li[:], dt[:], ALU.mult, ["li", "dt"], ["th"])
    sc_s = sincos_alloc(P, [128, 4], "ssm4")
    emit_sincos(P, sc_s, th[:], "th", cth[:], "cth", sth[:], "sth", "ssm4")
    ts(thT[:], th[:], float(TC), None, ALU.mult, None, ["th"], ["thT"])
    emit_sincos(P, sc_s, thT[:], "thT", cTc[:], "cTc", sTc[:], "sTc", "ssm4")
    tt(ar[:], mag[:], cth[:], ALU.mult, ["mag", "cth"], ["ar"])
    tt(ai[:], mag[:], sth[:], ALU.mult, ["mag", "sth"], ["ai"])
    tt(den[:], lr[:], lr[:], ALU.mult, ["lr"], ["den"])
    tt(t1[:], li[:], li[:], ALU.mult, ["li"], ["t1"])
    tt(den[:], den[:], t1[:], ALU.add, ["den", "t1"], ["den"])
    P.op("dve", lambda e: e.reciprocal(out=den[:], in_=den[:]), reads=["den"], writes=["den"])
    ts(ar[:], ar[:], -1.0, None, ALU.add, None, ["ar"], ["ar"])
    tt(t1[:], ar[:], lr[:], ALU.mult, ["ar", "lr"], ["t1"])
    tt(t2[:], ai[:], li[:], ALU.mult, ["ai", "li"], ["t2"])
    tt(cfr[:], t1[:], t2[:], ALU.add, ["t1", "t2"], ["cfr"])
    tt(cfr[:], cfr[:], den[:], ALU.mult, ["cfr", "den"], ["cfr"])
    tt(t1[:], ai[:], lr[:], ALU.mult, ["ai", "lr"], ["t1"])
    tt(t2[:], ar[:], li[:], ALU.mult, ["ar", "li"], ["t2"])
    tt(cfi[:], t1[:], t2[:], ALU.subtract, ["t1", "t2"], ["cfi"])
    tt(cfi[:], cfi[:], den[:], ALU.mult, ["cfi", "den"], ["cfi"])
    BTr = P.sbm("BTr", [128, 4, 128], BF16)
    BTi = P.sbm("BTi", [128, 4, 128], BF16)
    CTr = P.sbm("CTr", [128, 4, 128], BF16)
    CTi = P.sbm("CTi", [128, 4, 128], BF16)
    Dd = P.sbm("Dd", [128, 128], BF16)
    bfull = P.sb("bfull", [128, 128], F32)
    b16a = P.sb("b16a", [128, 16], F32)
    b16b = P.sb("b16b", [128, 16], F32)
    psT = P.ps("psT", [128, 512])
    P.op("dve", lambda e: e.memset(CTr[:], 0.0), writes=["CTr"])
    P.op("dve", lambda e: e.memset(CTi[:], 0.0), writes=["CTi"])
    P.op("dve", lambda e: e.tensor_scalar(out=Dd[:], in0=ident[:], scalar1=dcol[:, 0:1], scalar2=None, op0=ALU.mult), reads=["ident", "dcol"], writes=["Dd"])
    for i in range(4):
        for (dst, sa, ca, sb_, cb_, opc) in ((BTr, bre, cfr, bim, cfi, ALU.subtract), (BTi, bim, cfr, bre, cfi, ALU.add)):
            ts(b16a[:], sa[:, i, :], ca[:, i:i + 1], None, ALU.mult, None, ["bre", "bim", "cfr"], ["b16a"])
            ts(b16b[:], sb_[:, i, :], cb_[:, i:i + 1], None, ALU.mult, None, ["bre", "bim", "cfi"], ["b16b"])
            tt(b16a[:], b16a[:], b16b[:], opc, ["b16a", "b16b"], ["b16a"])
            P.op("dve", lambda e: e.memset(bfull[:], 0.0), writes=["bfull"])
            P.op("dve", lambda e, i=i: e.tensor_copy(out=bfull[0:64, 32 * i:32 * i + 16], in_=b16a[0:64, :]), reads=["b16a"], writes=["bfull"])
            P.op("dve", lambda e, i=i: e.tensor_copy(out=bfull[64:128, 32 * i + 16:32 * i + 32], in_=b16a[64:128, :]), reads=["b16a"], writes=["bfull"])
            P.op("pe", lambda e: e.transpose(psT[:, :128], bfull[:], ident[:]), reads=["bfull", "ident"], writes=["psT"])
            P.op("act", lambda e, dst=dst, i=i: e.copy(out=dst[:, i, :], in_=psT[:, :128]), reads=["psT"], writes=[("BTr" if dst is BTr else "BTi")])
        P.op("dve", lambda e, i=i: e.tensor_copy(out=CTr[0:64, i, 32 * i:32 * i + 16], in_=cre[0:64, i, :]), reads=["cre"], writes=["CTr"])
        P.op("dve", lambda e, i=i: e.tensor_copy(out=CTr[64:128, i, 32 * i + 16:32 * i + 32], in_=cre[64:128, i, :]), reads=["cre"], writes=["CTr"])
        P.op("dve", lambda e, i=i: e.tensor_scalar(out=CTi[0:64, i, 32 * i:32 * i + 16], in0=cim[0:64, i, :], scalar1=-1.0, scalar2=None, op0=ALU.mult),
             reads=["cim"], writes=["CTi"])
        P.op("dve", lambda e, i=i: e.tensor_scalar(out=CTi[64:128, i, 32 * i + 16:32 * i + 32], in0=cim[64:128, i, :], scalar1=-1.0, scalar2=None, op0=ALU.mult),
             reads=["cim"], writes=["CTi"])
    posi = P.sb("posi", [128, TC], I32)
    posf = P.sb("posf", [128, TC], F32)
    angj = P.sb("angj", [128, TC], F32)
    P.op("pool", lambda e: e.iota(posi[:], pattern=[[1, TC]], base=0, channel_multiplier=0), writes=["posi"])
    P.op("dve", lambda e: e.tensor_copy(out=posf[:], in_=posi[:]), reads=["posi"], writes=["posf"])
    sc_t = sincos_alloc(P, [128, TC], "ssmT")
    cosJ = [P.sbm("cosJ%d" % i, [128, TC], F32) for i in range(4)]
    sinJ = [P.sbm("sinJ%d" % i, [128, TC], F32) for i in range(4)]
    rfull = [P.sbm("rfull%d" % i, [128, TC], F32) for i in range(4)]
    for i in range(4):
        ts(angj[:], posf[:], th[:, i:i + 1], None, ALU.mult, None, ["posf", "th"], ["angj"])
        emit_sincos(P, sc_t, angj[:], "angj", cosJ[i][:], "cosJ%d" % i, sinJ[i][:], "sinJ%d" % i, "ssmT")
        P.op("dve", lambda e, i=i: e.memset(rfull[i][:], 1.0), writes=["rfull%d" % i])
        ts(rfull[i][:], rfull[i][:], mag[:, i:i + 1], None, ALU.mult, None, ["rfull%d" % i, "mag"], ["rfull%d" % i])
    return dict(BTr=BTr, BTi=BTi, CTr=CTr, CTi=CTi, Dd=Dd, cosJ=cosJ, sinJ=sinJ, rfull=rfull, cTc=cTc, sTc=sTc)


def ssm_steps(P, dr, uT, T):
    ssmT_out = dr["ssmT_out"]
    BTr, BTi, CTr, CTi, Dd = T["BTr"], T["BTi"], T["CTr"], T["CTi"], T["Dd"]
    cosJ, sinJ, rfull, cTc, sTc = T["cosJ"], T["sinJ"], T["rfull"], T["cTc"], T["sTc"]

    def ts(out, in0, s1, s2, op0, op1=None, rd=(), wr=()):
        if op1 is None:
            P.op("dve", lambda e: e.tensor_scalar(out=out, in0=in0, scalar1=s1, scalar2=None, op0=op0), reads=rd, writes=wr)
        else:
            P.op("dve", lambda e: e.tensor_scalar(out=out, in0=in0, scalar1=s1, scalar2=s2, op0=op0, op1=op1), reads=rd, writes=wr)

    def tt(out, a, b, op, rd=(), wr=(), eng="dve"):
        P.op(eng, lambda e: e.tensor_tensor(out=out, in0=a, in1=b, op=op), reads=rd, writes=wr)

    br32 = [P.sb("br32_%d" % i, [128, TC], F32) for i in range(2)]
    bi32 = [P.sb("bi32_%d" % i, [128, TC], F32) for i in range(2)]
    m1 = P.sb("m1", [128, TC], F32)
    m2 = P.sb("m2", [128, TC], F32)
    m3 = P.sb("m3", [128, TC], F32)
    m4 = P.sb("m4", [128, TC], F32)
    rbr = P.sb("rbr", [128, TC], F32)
    rbi = P.sb("rbi", [128, TC], F32)
    wre = [P.sb("wre%d" % i, [128, TC], F32) for i in range(4)]
    wim = [P.sb("wim%d" % i, [128, TC], F32) for i in range(4)]
    xre = [P.sb("xre%d" % i, [128, TC], BF16) for i in range(2)]
    xim = [P.sb("xim%d" % i, [128, TC], BF16) for i in range(2)]
    ini = P.sb("ini", [128, 4, 2], F32)
    itmp = P.sb("itmp", [128, 2], F32)
    gx = [P.sb("gx%d" % i, [128, TC], F32) for i in range(2)]
    g2 = [P.sb("g2%d" % i, [128, TC], F32) for i in range(2)]
    gs = [P.sb("gs%d" % i, [128, TC], F32) for i in range(2)]
    yo = [P.sb("yo%d" % i, [128, TC], F32) for i in range(2)]
    psB = P.ps("psB", [128, 2, TC])
    py = P.ps("psYs", [128, 512])
    pyr = "psYs"
    P.op("dve", lambda e: e.memset(ini[:], 0.0), writes=["ini"])
    steps = [(c, i) for c in range(NCHS) for i in range(4)]
    NS = len(steps)

    def stA(k):
        c, i = steps[k]
        t0, n = chunkS(c)
        b = k % 2
        P.op("pe", lambda e, i=i, t0=t0, n=n: e.matmul(psB[:, 0, :n], BTr[:, i, :], uT[:, t0:t0 + n], start=True, stop=True), reads=["BTr", "uT"], writes=["psB"])
        P.op("pe", lambda e, i=i, t0=t0, n=n: e.matmul(psB[:, 1, :n], BTi[:, i, :], uT[:, t0:t0 + n], start=True, stop=True, skip_group_check=True), reads=["BTi", "uT"], writes=["psB"])
        P.op("act", lambda e, n=n, b=b: e.copy(out=br32[b][:, :n], in_=psB[:, 0, :n]), reads=["psB"], writes=["br32_%d" % b])
        P.op("act", lambda e, n=n, b=b: e.copy(out=bi32[b][:, :n], in_=psB[:, 1, :n]), reads=["psB"], writes=["bi32_%d" % b])

    def stB(k):
        c, i = steps[k]
        t0, n = chunkS(c)
        b = k % 2
        brb, bib = br32[b], bi32[b]
        brn, bin_ = "br32_%d" % b, "bi32_%d" % b
        cj, sj = cosJ[i], sinJ[i]
        cr, sr = "cosJ%d" % i, "sinJ%d" % i
        tt(m1[:, :n], cj[:, :n], brb[:, :n], ALU.mult, [cr, brn], ["m1"], "dve")
        tt(m2[:, :n], sj[:, :n], bib[:, :n], ALU.mult, [sr, bin_], ["m2"], "pool")
        tt(m3[:, :n], cj[:, :n], bib[:, :n], ALU.mult, [cr, bin_], ["m3"], "pool")
        tt(m4[:, :n], sj[:, :n], brb[:, :n], ALU.mult, [sr, brn], ["m4"], "dve")
        tt(rbr[:, :n], m1[:, :n], m2[:, :n], ALU.add, ["m1", "m2"], ["rbr"], "dve")
        tt(rbi[:, :n], m3[:, :n], m4[:, :n], ALU.subtract, ["m3", "m4"], ["rbi"], "pool")
        wr_, wi_ = wre[i], wim[i]
        wrn, win = "wre%d" % i, "wim%d" % i
        if c > 0:
            ts(itmp[:, 0:1], wi_[:, TC - 1:TC], sTc[:, i:i + 1], None, ALU.mult, None, [win, "sTc"], ["itmp"])
            P.op("dve", lambda e, i=i, wr_=wr_: e.scalar_tensor_tensor(out=ini[:, i, 0:1], in0=wr_[:, TC - 1:TC], scalar=cTc[:, i:i + 1], in1=itmp[:, 0:1],
                                                                      op0=ALU.mult, op1=ALU.subtract), reads=[wrn, "cTc", "itmp"], writes=["ini"])
            ts(itmp[:, 1:2], wr_[:, TC - 1:TC], sTc[:, i:i + 1], None, ALU.mult, None, [wrn, "sTc"], ["itmp"])
            P.op("dve", lambda e, i=i, wi_=wi_: e.scalar_tensor_tensor(out=ini[:, i, 1:2], in0=wi_[:, TC - 1:TC], scalar=cTc[:, i:i + 1], in1=itmp[:, 1:2],
                                                                      op0=ALU.mult, op1=ALU.add), reads=[win, "cTc", "itmp"], writes=["ini"])
        P.op("dve", lambda e, i=i, wr_=wr_, n=n: e.tensor_tensor_scan(out=wr_[:, :n], data0=rfull[i][:, :n], data1=rbr[:, :n], initial=ini[:, i, 0:1],
                                                                     op0=ALU.mult, op1=ALU.add), reads=["rfull%d" % i, "rbr", "ini"], writes=[wrn])
        P.op("dve", lambda e, i=i, wi_=wi_, n=n: e.tensor_tensor_scan(out=wi_[:, :n], data0=rfull[i][:, :n], data1=rbi[:, :n], initial=ini[:, i, 1:2],
                                                                     op0=ALU.mult, op1=ALU.add), reads=["rfull%d" % i, "rbi", "ini"], writes=[win])
        xr, xi = xre[b], xim[b]
        xrn, xin = "xre%d" % b, "xim%d" % b
        tt(m1[:, :n], cj[:, :n], wr_[:, :n], ALU.mult, [cr, wrn], ["m1"], "dve")
        tt(m2[:, :n], sj[:, :n], wi_[:, :n], ALU.mult, [sr, win], ["m2"], "pool")
        tt(m3[:, :n], sj[:, :n], wr_[:, :n], ALU.mult, [sr, wrn], ["m3"], "pool")
        tt(m4[:, :n], cj[:, :n], wi_[:, :n], ALU.mult, [cr, win], ["m4"], "dve")
        tt(xr[:, :n], m1[:, :n], m2[:, :n], ALU.subtract, ["m1", "m2"], [xrn], "dve")
        tt(xi[:, :n], m3[:, :n], m4[:, :n], ALU.add, ["m3", "m4"], [xin], "pool")

    def stC(k):
        c, i = steps[k]
        t0, n = chunkS(c)
        b = k % 2
        xr, xi = xre[b], xim[b]
        xrn, xin = "xre%d" % b, "xim%d" % b
        P.op("pe", lambda e, i=i, xr=xr, n=n: e.matmul(py[:, :n], CTr[:, i, :], xr[:, :n], start=(i == 0), stop=False), reads=["CTr", xrn], writes=[pyr])
        P.op("pe", lambda e, i=i, xi=xi, n=n: e.matmul(py[:, :n], CTi[:, i, :], xi[:, :n], start=False, stop=False), reads=["CTi", xin], writes=[pyr])
        if i == 3:
            g = c % 2
            P.op("pe", lambda e, t0=t0, n=n: e.matmul(py[:, :n], Dd[:], uT[:, t0:t0 + n], start=False, stop=True), reads=["Dd", "uT"], writes=[pyr])
            P.op("act", lambda e, n=n, g=g: e.copy(out=gx[g][:, :n], in_=py[:, :n]), reads=[pyr], writes=["gx%d" % g])
            P.op("act", lambda e, n=n, g=g: e.activation(out=g2[g][:, :n], in_=py[:, :n], func=AF.Square), reads=[pyr], writes=["g2%d" % g])

    def stD2(c):
        t0, n = chunkS(c)
        g = c % 2
        ts(g2[g][:, :n], g2[g][:, :n], 0.044715, 1.0, ALU.mult, ALU.add, ["g2%d" % g], ["g2%d" % g])
        tt(g2[g][:, :n], g2[g][:, :n], gx[g][:, :n], ALU.mult, ["g2%d" % g, "gx%d" % g], ["g2%d" % g], "pool")

    def stD3(c):
        t0, n = chunkS(c)
        g = c % 2
        P.op("act", lambda e, n=n, g=g: e.activation(out=gs[g][:, :n], in_=g2[g][:, :n], func=AF.Sigmoid, scale=1.5957691216057308), reads=["g2%d" % g], writes=["gs%d" % g])

    def stD4(c):
        t0, n = chunkS(c)
        g = c % 2
        tt(yo[g][:, :n], gx[g][:, :n], gs[g][:, :n], ALU.mult, ["gx%d" % g, "gs%d" % g], ["yo%d" % g], "pool")
        nv = min(n, L - t0)
        if nv > 0:
            P.dma("sp", ssmT_out[:, t0:t0 + nv], yo[g][:, :nv], reads=["yo%d" % g], writes=["dram:ssmT_out"])

    tails = {}
    t = 0
    while t < NS + 2 or any(k >= t for k in tails):
        if t < NS:
            stA(t)
        if 0 <= t - 1 < NS:
            stB(t - 1)
        if 0 <= t - 2 < NS:
            stC(t - 2)
            c, i = steps[t - 2]
            if i == 3:
                tails.setdefault(t + 1, []).append(lambda c=c: (stD2(c), stD3(c)))
                tails.setdefault(t + 2, []).append(lambda c=c: stD4(c))
        for f in tails.pop(t, []):
            f()
        t += 1
        yield


def attn_steps(P, dr, qT, kT, Va, ident16, lam_init):
    nc = P.nc
    attn_out = dr["attn_out"]
    scale = HD ** -0.5
    nm_d = P.sb("nm_d", [128, 128], BF16)
    nm_n = P.sb("nm_n", [128, 128], BF16)
    P.op("dve", lambda e: e.memset(nm_d[:], NEG), writes=["nm_d"])
    P.op("dve", lambda e: e.memset(nm_d[0:16, :], 0.0), writes=["nm_d"])
    P.op("dve", lambda e: e.memset(nm_d[0:80, 16:128], 0.0), writes=["nm_d"])
    P.op("dve", lambda e: e.memset(nm_d[:, 80:128], 0.0), writes=["nm_d"])
    P.op("dve", lambda e: e.memset(nm_n[:], NEG), writes=["nm_n"])
    mhalf = P.sb("mhalf", [128, 1], F32)
    P.op("dve", lambda e: e.memset(mhalf[:], -0.5), writes=["mhalf"])
    P.op("dve", lambda e: e.memset(nm_n[0:16, 80:128], 0.0), writes=["nm_n"])
    lq = P.sb("lq", [128, 4, 64], F32)
    P.dma("sp", lq[:], dr["lam_qk_bc"], writes=["lq"])
    lp = P.sb("lp", [128, 2, 64], F32)
    lsum = P.sb("lsum", [128, 2], F32)
    nlam = P.sb("nlam", [128, 1], F32)
    P.op("dve", lambda e: e.tensor_tensor(out=lp[:, 0, :], in0=lq[:, 0, :], in1=lq[:, 1, :], op=ALU.mult), reads=["lq"], writes=["lp"])
    P.op("dve", lambda e: e.tensor_tensor(out=lp[:, 1, :], in0=lq[:, 2, :], in1=lq[:, 3, :], op=ALU.mult), reads=["lq"], writes=["lp"])
    P.op("dve", lambda e: e.reduce_sum(out=lsum[:], in_=lp[:], axis=AX.X), reads=["lp"], writes=["lsum"])
    P.op("act", lambda e: e.activation(out=lsum[:], in_=lsum[:], func=AF.Exp), reads=["lsum"], writes=["lsum"])
    P.op("dve", lambda e: e.tensor_tensor(out=nlam[:], in0=lsum[:, 1:2], in1=lsum[:, 0:1], op=ALU.subtract), reads=["lsum"], writes=["nlam"])
    P.op("dve", lambda e: e.tensor_scalar(out=nlam[:], in0=nlam[:], scalar1=-lam_init, scalar2=None, op0=ALU.add), reads=["nlam"], writes=["nlam"])
    gsc = P.sb("gsc", [128, 128], F32)
    P.dma("sp", gsc[:], dr["subln_g_bc"], writes=["gsc"])
    P.op("dve", lambda e: e.tensor_scalar(out=gsc[:], in0=gsc[:], scalar1=1.0 - lam_init, scalar2=None, op0=ALU.mult), reads=["gsc"], writes=["gsc"])
    psSS = [P.ps("psSS_%d" % b, [128, 2, 512]) for b in range(2)]
    psS = [[psSS[b][:, c, :] for b in range(2)] for c in range(2)]
    psO1 = [P.ps("psO%d" % c, [128, NSUB, 129]) for c in range(2)]
    sbO = [[P.sb("sbO%d_%d" % (c, b), [128, NSUB, 129], F32) for b in range(2)] for c in range(2)]
    PtP = [P.sb("PtP_%d" % b, [128, 2, NSUB * 128], BF16) for b in range(2)]
    Pt = [[PtP[b][:, c, :] for b in range(2)] for c in range(2)]
    sq16 = P.sb("sq16", [128, 512], BF16)
    ones_c = [P.sb("ones_c%d" % c, [128, 128], BF16) for c in range(2)]
    for c in range(2):
        P.op("dve", lambda e, c=c: e.memset(ones_c[c][:], 0.0), writes=["ones_c%d" % c])
        P.op("dve", lambda e, c=c: e.memset(ones_c[c][c * 64:(c + 1) * 64, :], 1.0), writes=["ones_c%d" % c])
    mx = P.sb("mx", [128, 2, 2, NCHA], F32)
    mxr = P.sb("mxr", [128, 2, 2], F32)
    negM = P.sb("negM", [128, 2], F32)
    negMc = P.sb("negMc", [128, 1], F32)
    r0 = P.sb("r0", [128, 1], F32)
    r1 = P.sb("r1", [128, 1], F32)
    ot = P.sb("ot", [128, 128], F32)
    junk = P.sb("junk", [128, 128], F32)
    ss = P.sb("ss", [128, 1], F32)
    ob = [P.sb("ob%d" % i, [128, 128], F32) for i in range(2)]
    nsb = (NBLK + NSUB - 1) // NSUB
    sbuf_i = 0
    obi = 0
    for h in range(2):
        for which, src, sres in ((0, qT[h], "qT%d" % h), (1, kT[h], "kT%d" % h)):
            for c in range(NCHA):
                t0, n = chunkA(c)
                P.op("dve", lambda e, src=src, t0=t0, n=n: e.tensor_tensor(out=sq16[:, :n], in0=src[:, t0:t0 + n], in1=src[:, t0:t0 + n], op=ALU.mult),
                     reads=[sres], writes=["sq16"])
                for m in range(2):
                    ps = psS[m][0]
                    P.op("pe", lambda e, m=m, ps=ps, n=n: e.matmul(ps[:, :n], ones_c[m][:], sq16[:, :n], start=True, stop=True),
                         reads=["ones_c%d" % m, "sq16"], writes=["psS%d_0" % m])
                    P.op("dve", lambda e, m=m, ps=ps, n=n, which=which, c=c: e.reduce_max(out=mx[:, which, m, c:c + 1], in_=ps[:, :n], axis=AX.X),
                         reads=["psS%d_0" % m], writes=["mx"])
        P.op("dve", lambda e: e.reduce_max(out=mxr[:], in_=mx[:], axis=AX.X), reads=["mx"], writes=["mxr"])
        P.op("dve", lambda e: e.tensor_tensor(out=negM[:], in0=mxr[:, 0, :], in1=mxr[:, 1, :], op=ALU.add), reads=["mxr"], writes=["negM"])
        P.op("dve", lambda e: e.tensor_scalar(out=negM[:], in0=negM[:], scalar1=-0.5 * scale, scalar2=None, op0=ALU.mult), reads=["negM"], writes=["negM"])
        P.op("dve", lambda e: e.tensor_tensor(out=negMc[:], in0=negM[:, 0:1], in1=negM[:, 1:2], op=ALU.min), reads=["negM"], writes=["negMc"])
        if "dbg_q" in dr and h == 0:
            P.dma("sp", dr["dbg_q"], qT[0][:], reads=["qT0"], writes=["dram:dbg_q"], semkey="dbg1")
            P.dma("sp", dr["dbg_k"], kT[0][:], reads=["kT0"], writes=["dram:dbg_k"], semkey="dbg2")
            P.dma("sp", dr["dbg_v"], Va[0][:], reads=["Va0"], writes=["dram:dbg_v"], semkey="dbg3")
            P.dma("sp", dr["dbg_m"], negM[:], reads=["negM"], writes=["dram:dbg_m"], semkey="dbg4")
            P.dma("sp", dr["dbg_l"], nlam[:], reads=["nlam"], writes=["dram:dbg_l"], semkey="dbg5")
        for I in range(nsb):
            i0 = I * NSUB
            nq = min(NSUB, NBLK - i0)
            q0 = i0 * 128
            jmax = min(i0 + nq, NBLK - 1)
            ab = sbuf_i % 2
            sbuf_i += 1
            units = [(j, c) for j in range(jmax + 1) for c in range(2)]

            def geom(j):
                s_lo = max(0, j - 1 - i0)
                return s_lo, s_lo * 128, nq * 128

            def emit_S(u, h=h, i0=i0, nq=nq, q0=q0):
                j, c = units[u]
                s_lo, c0, c1 = geom(j)
                S = psS[c][j % 2]
                sres = "psS%d_%d" % (c, j % 2)
                masks = [(s, nm_d if i0 + s == j else nm_n) for s in range(s_lo, nq) if i0 + s in (j, j - 1)]
                P.op("pe", lambda e, c=c, S=S, j=j, c0=c0, c1=c1, q0=q0, h=h, last=(len(masks) == 0): e.matmul(
                    S[:, c0:c1], kT[h][c * 64:(c + 1) * 64, j * 128:(j + 1) * 128], qT[h][c * 64:(c + 1) * 64, q0 + c0:q0 + c1], start=True, stop=last, skip_group_check=True),
                    reads=["kT%d" % h, "qT%d" % h], writes=[sres])
                for mi, (s, nm) in enumerate(masks):
                    P.op("pe", lambda e, S=S, s=s, nm=nm, last=(mi == len(masks) - 1): e.matmul(S[:, s * 128:(s + 1) * 128], ident16[:], nm[:], start=False, stop=last, skip_group_check=True),
                         reads=["ident16", "nm_d", "nm_n"], writes=[sres])

            def emit_E(u):
                j, c = units[u]
                s_lo, c0, c1 = geom(j)
                b = j % 2
                P.op("act", lambda e, b=b, c0=c0, c1=c1: e.activation(out=PtP[b][:, :, c0:c1], in_=psSS[b][:, :, c0:c1], func=AF.Exp, scale=scale, bias=negMc[:, 0:1]),
                     reads=["psS0_%d" % b, "psS1_%d" % b, "negMc"], writes=["Pt0_%d" % b, "Pt1_%d" % b])

            def emit_PV(u, h=h, ab=ab, nq=nq):
                j, c = units[u]
                s_lo, c0, c1 = geom(j)
                pt = Pt[c][j % 2]
                for s in range(s_lo, nq):
                    P.op("pe", lambda e, c=c, s=s, pt=pt, j=j, h=h, first=(j == 0 and s == s_lo): e.matmul(
                        psO1[c][:, s, :], pt[:, s * 128:(s + 1) * 128], Va[h][:, j, 0:129], start=first, stop=True, skip_group_check=True),
                         reads=["Pt%d_%d" % (c, j % 2), "Va%d" % h], writes=["psO%d" % c])

            emit_S(0)
            emit_S(1)
            for u in range(0, len(units), 2):
                emit_E(u)
                if u + 2 < len(units):
                    emit_S(u + 2)
                    emit_S(u + 3)
                emit_PV(u)
                emit_PV(u + 1)
                yield
            for c in range(2):
                P.op("act", lambda e, c=c, ab=ab: e.copy(out=sbO[c][ab][:], in_=psO1[c][:]), reads=["psO%d" % c], writes=["sbO%d_%d" % (c, ab)])
            psO = [[sbO[0][0], sbO[0][1]], [sbO[1][0], sbO[1][1]]]
            for s in range(nq):
                blk = i0 + s
                o0 = "sbO0_%d" % ab
                o1 = "sbO1_%d" % ab
                P.op("dve", lambda e, ab=ab, s=s: e.reciprocal(out=r0[:], in_=psO[0][ab][:, s, 128:129]), reads=[o0], writes=["r0"])
                P.op("dve", lambda e, ab=ab, s=s: e.reciprocal(out=r1[:], in_=psO[1][ab][:, s, 128:129]), reads=[o1], writes=["r1"])
                P.op("dve", lambda e: e.tensor_tensor(out=r1[:], in0=r1[:], in1=nlam[:], op=ALU.mult), reads=["r1", "nlam"], writes=["r1"])
                P.op("dve", lambda e, ab=ab, s=s: e.tensor_scalar(out=ot[:], in0=psO[0][ab][:, s, 0:128], scalar1=r0[:, 0:1], scalar2=None, op0=ALU.mult),
                     reads=[o0, "r0"], writes=["ot"])
                P.op("dve", lambda e, ab=ab, s=s: e.scalar_tensor_tensor(out=ot[:], in0=psO[1][ab][:, s, 0:128], scalar=r1[:, 0:1], in1=ot[:], op0=ALU.mult, op1=ALU.add),
                     reads=[o1, "r1", "ot"], writes=["ot"])
                P.op("dve", lambda e: e.scalar_tensor_tensor(out=junk[:], in0=ot[:], scalar=1.0, in1=ot[:], op0=ALU.mult, op1=ALU.mult, accum_out=ss[:]),
                     reads=["ot"], writes=["junk", "ss"])
                P.op("dve", lambda e: e.tensor_scalar(out=ss[:], in0=ss[:], scalar1=1.0 / VD, scalar2=RMS_EPS, op0=ALU.mult, op1=ALU.add), reads=["ss"], writes=["ss"])
                P.op("pool", lambda e: e.tensor_tensor(out=ss[:], in0=ss[:], in1=mhalf[:], op=ALU.pow), reads=["ss", "mhalf"], writes=["ss"])
                o = ob[obi % 2]
                ores = "ob%d" % (obi % 2)
                obi += 1
                P.op("dve", lambda e, o=o: e.scalar_tensor_tensor(out=o[:], in0=ot[:], scalar=ss[:, 0:1], in1=gsc[:], op0=ALU.mult, op1=ALU.mult),
                     reads=["ot", "ss", "gsc"], writes=[ores])
                P.dma("sp", attn_out[blk * 128:(blk + 1) * 128, h * 128:(h + 1) * 128], o[:], reads=[ores], writes=["dram:attn_out"])


A_INPUTS = [("hT", [D, LP]), ("w_sel", [D, 1280]), ("inv_c", [128, 1]), ("conv_w_c", [128, 3]),
            ("lam_re_c", [128, 4]), ("lam_im_c", [128, 4]), ("log_dt_c", [128, 4]), ("b_re_c", [128, 4, 16]), ("b_im_c", [128, 4, 16]),
            ("c_reT_c", [128, 4, 16]), ("c_imT_c", [128, 4, 16]), ("d_c", [128, 1]), ("lam_qk_bc", [128, 4, 64]), ("subln_g_bc", [128, 128])]


def build_A(layer, debug=False):
    nc = bass.Bass("TRN2", target_bir_lowering=False)
    dr = {name: nc.dram_tensor(name, shape, F32, kind="ExternalInput").ap() for name, shape in A_INPUTS}
    if debug:
        dr["dbg_q"] = nc.dram_tensor("dbg_q", [128, LP], BF16, kind="ExternalOutput").ap()
        dr["dbg_k"] = nc.dram_tensor("dbg_k", [128, LP], BF16, kind="ExternalOutput").ap()
        dr["dbg_v"] = nc.dram_tensor("dbg_v", [128, NBLK, 130], BF16, kind="ExternalOutput").ap()
        dr["dbg_m"] = nc.dram_tensor("dbg_m", [128, 2], F32, kind="ExternalOutput").ap()
        dr["dbg_l"] = nc.dram_tensor("dbg_l", [128, 1], F32, kind="ExternalOutput").ap()
    dr["ssmT_out"] = nc.dram_tensor("ssmT_out", [128, L], F32, kind="ExternalOutput").ap()
    dr["convT_out"] = nc.dram_tensor("convT_out", [128, L], F32, kind="ExternalOutput").ap()
    dr["attn_out"] = nc.dram_tensor("attn_out", [LP, 256], F32, kind="ExternalOutput").ap()
    P = Prog(nc)
    emit_A(P, dr, 0.8 - 0.6 * math.exp(-0.3 * layer))
    P.emit()
    return nc


def host_A_inputs(inp, l, core, hT_b):
    f = np.float32
    j = core % 4
    w_in = np.asarray(inp["w_in"][l], f)
    s0, s1, s2, s3 = SSM_W, SSM_W + CONV_W, SSM_W + 2 * CONV_W, SSM_W + 3 * CONV_W
    s4, s5 = s3 + ATT_W, s3 + 2 * ATT_W
    cols = [np.arange(j * 128, (j + 1) * 128)]
    for base in (s0, s1, s2):
        cols.append(base + np.arange(j * 128, (j + 1) * 128))
    for base in (s3, s4, s5):
        cols.append(base + np.arange(j * 256, (j + 1) * 256))
    cols = np.concatenate(cols)
    g0 = 8 * j

    def st(a):
        a = np.asarray(a, f)[g0:g0 + 8]
        return np.ascontiguousarray(a.reshape(4, 2, 64).transpose(1, 2, 0).reshape(128, 4))
    ldt = np.repeat(np.asarray(inp["ssm_log_dt"][l], f)[:, None], 64, axis=1)

    def sb3(a):
        a = np.asarray(a, f)[g0:g0 + 8]
        return np.ascontiguousarray(a.reshape(4, 2, 64, 16).transpose(1, 2, 0, 3).reshape(128, 4, 16))
    i = np.arange(128) % 64 % 32
    inv = (np.float32(ROPE_THETA) ** (-(2 * i).astype(np.float32) / np.float32(HD))).astype(f)
    lam_qk = np.stack([np.asarray(inp[k][l], f) for k in ("attn_lambda_q1", "attn_lambda_k1", "attn_lambda_q2", "attn_lambda_k2")])
    return {
        "hT": hT_b, "w_sel": np.ascontiguousarray(w_in[:, cols]), "inv_c": np.ascontiguousarray(inv[:, None]),
        "conv_w_c": np.ascontiguousarray(np.asarray(inp["conv_w"][l], f)[:, j * 128:(j + 1) * 128].T),
        "lam_re_c": st(inp["ssm_lambda_re"][l]), "lam_im_c": st(inp["ssm_lambda_im"][l]), "log_dt_c": st(ldt),
        "b_re_c": sb3(inp["ssm_b_re"][l]), "b_im_c": sb3(inp["ssm_b_im"][l]),
        "c_reT_c": sb3(np.asarray(inp["ssm_c_re"][l], f).transpose(0, 2, 1)), "c_imT_c": sb3(np.asarray(inp["ssm_c_im"][l], f).transpose(0, 2, 1)),
        "d_c": np.ascontiguousarray(np.asarray(inp["ssm_d"][l], f)[j * 128:(j + 1) * 128, None]),
        "lam_qk_bc": np.ascontiguousarray(np.broadcast_to(lam_qk[None], (128, 4, 64))),
        "subln_g_bc": np.ascontiguousarray(np.broadcast_to(np.asarray(inp["attn_subln_g"][l], f)[None], (128, 128))),
    }


def build_LN():
    nc = bass.Bass("TRN2", target_bir_lowering=False)
    xT = nc.dram_tensor("xT", [D, TOK_PER_CORE], F32, kind="ExternalInput").ap()
    g_c = nc.dram_tensor("g_c", [128, 8], F32, kind="ExternalInput").ap()
    b_c = nc.dram_tensor("b_c", [128, 8], F32, kind="ExternalInput").ap()
    oT = nc.dram_tensor("oT", [D, TOK_PER_CORE], F32, kind="ExternalOutput").ap()
    P = Prog(nc)
    N = CH
    xv = xT.rearrange("(k p) t -> p k t", p=128)
    ov = oT.rearrange("(k p) t -> p k t", p=128)
    lg = P.sb("lg", [128, 8], F32)
    lb = P.sb("lb", [128, 8], F32)
    P.dma("sp", lg[:], g_c, writes=["ln_gb_0"])
    P.dma("sp", lb[:], b_c, writes=["ln_gb_0"], semkey="lnb0")
    ones_f = P.sb("ones_f", [128, 128], F32)
    P.op("dve", lambda e: e.memset(ones_f[:], 1.0 / D), writes=["ones_f"])
    ln_alloc(P)
    x32 = [P.sb("x32_%d" % i, [128, 8, N], F32) for i in range(2)]
    o32 = [P.sb("o32_%d" % i, [128, 8, N], F32) for i in range(2)]
    for c in range(NCH):
        t0 = c * N
        xb, xr = x32[c % 2], "x32_%d" % (c % 2)
        ob, orr = o32[c % 2], "o32_%d" % (c % 2)
        P.dma("sp", xb[:], xv[:, :, t0:t0 + N], writes=[xr])
        emit_layernorm(P, xb, xr, N, ones_f, lg, lb, ob, orr, "0")
        P.dma("sp", ov[:, :, t0:t0 + N], ob[:], reads=[orr], writes=["dram:oT"])
    P.emit()
    return nc


_CACHE = {}


def _prog(key, fn):
    if key not in _CACHE:
        _CACHE[key] = fn()
    return _CACHE[key]


def kernel(**inp):
    f = np.float32
    x = np.asarray(inp["x"], f)
    meta = np.asarray(inp["meta_tokens"], f)
    hin = np.concatenate([np.broadcast_to(meta[None], (BATCH, N_META, D)), x], axis=1)
    xT_all = np.ascontiguousarray(hin.reshape(BATCH * L, D).T)
    cores = list(range(NCORES))
    T = TOK_PER_CORE
    maps = [{"xT": np.ascontiguousarray(xT_all[:, c * T:(c + 1) * T]), "g_c": col_layout(inp["ln_in_g"]), "b_c": col_layout(inp["ln_in_b"])} for c in cores]
    res = run_bass_kernel_spmd(_prog("ln", build_LN), maps, core_ids=cores)
    hT_all = np.concatenate([r["oT"] for r in res.results], axis=1)
    for l in range(DEPTH):
        maps = []
        for c in cores:
            b = c // 4
            hT = np.zeros((D, LP), f)
            hT[:, :L] = hT_all[:, b * L:(b + 1) * L]
            maps.append(host_A_inputs(inp, l, c, hT))
        res = run_bass_kernel_spmd(_prog(("A", l), lambda: build_A(l)), maps, core_ids=cores)
        ssmT = np.zeros((SSM_W, BATCH * L), f)
        convT = np.zeros((CONV_W, BATCH * L), f)
        attnT = np.zeros((ATT_W, BATCH * L), f)
        for c in cores:
            b, j = c // 4, c % 4
            r = res.results[c]
            ssmT[j * 128:(j + 1) * 128, b * L:(b + 1) * L] = r["ssmT_out"]
            convT[j * 128:(j + 1) * 128, b * L:(b + 1) * L] = r["convT_out"]
            attnT[j * 256:(j + 1) * 256, b * L:(b + 1) * L] = r["attn_out"][:L].T
        W = host_B_weights(inp, l)
        maps = []
        for c in cores:
            sl = slice(c * T, (c + 1) * T)
            m = dict(W)
            m.update(hT=np.ascontiguousarray(hT_all[:, sl]), ssmT=np.ascontiguousarray(ssmT[:, sl]),
                     convT=np.ascontiguousarray(convT[:, sl]), attnT=np.ascontiguousarray(attnT[:, sl]))
            maps.append(m)
        res = run_bass_kernel_spmd(_prog("B", build_B), maps, core_ids=cores)
        hT_all = np.concatenate([r["outT"] for r in res.results], axis=1)
    out = hT_all.T.reshape(BATCH, L, D)[:, N_META:]
    return np.ascontiguousarray(out, dtype=f)
```

```python
import math
from contextlib import ExitStack

import numpy as np
import concourse.bass as bass
import concourse.mybir as mybir
from concourse.bass_utils import run_bass_kernel_spmd

F32 = mybir.dt.float32
BF16 = mybir.dt.bfloat16
I32 = mybir.dt.int32
AF = mybir.ActivationFunctionType
ALU = mybir.AluOpType
AX = mybir.AxisListType

D = 1024
BATCH = 2
SEQ = 8192
DEPTH = 2
N_META = 16
L = SEQ + N_META
LP = 8320
NBLK = LP // 128
SSM_W = 512
CONV_W = 512
HEADS = 8
HD = 64
VD = 128
ATT_W = 1024
IN_W = 5120
NE = 32
TOPK = 4
DFF = 1024
SW_LIMIT = 7.0
SW_ALPHA = 1.702
DN_ALPHA = (2.0 * DEPTH) ** 0.25
LN_EPS = 1e-5
RMS_EPS = 1e-5
ROPE_THETA = 10000.0
NCORES = 8
TOK_PER_CORE = (BATCH * L) // NCORES
TWO_PI = 2.0 * math.pi


STRICT_SAME_ENGINE = False


class Prog:
    ENGS = ("pe", "act", "dve", "pool", "sp")

    def __init__(self, nc):
        self.nc = nc
        self.es = ExitStack()
        self.ops = []
        self.pes = ExitStack()
        self.mes = ExitStack()
        self.nphase = 0

    def sbm(self, name, shape, dtype):
        return self.mes.enter_context(self.nc.sbuf_tensor("m%d_%s" % (self.nphase, name), list(shape), dtype, side="right"))

    def sb(self, name, shape, dtype):
        return self.pes.enter_context(self.nc.sbuf_tensor("p%d_%s" % (self.nphase, name), list(shape), dtype, side="left"))

    def ps(self, name, shape, dtype=F32):
        return self.pes.enter_context(self.nc.psum_tensor("p%d_%s" % (self.nphase, name), list(shape), dtype))

    def new_phase(self, keep_mid=False):
        self.pes.close()
        self.pes = ExitStack()
        if not keep_mid:
            self.mes.close()
            self.mes = ExitStack()
        self.nphase += 1
        self.ops.append(dict(barrier=True, eng=None, dma=False))

    def op(self, eng, fn, reads=(), writes=(), dma=False, semkey=None):
        assert eng in self.ENGS
        self.ops.append(dict(eng=eng, fn=fn, reads=tuple(reads), writes=tuple(writes), dma=dma, semkey=semkey))

    def dma(self, eng, out, in_, reads=(), writes=(), semkey=None):
        self.op(eng, lambda e: e.dma_start(out=out, in_=in_), reads, writes, dma=True, semkey=semkey)

    def emit(self, final_dram_writes=True):
        nc = self.nc
        ops = self.ops
        n = len(ops)
        last_writer = {}
        readers = {}
        deps = [dict() for _ in range(n)]
        last_on_eng = {}
        last_dma = {}
        pending = {}
        for i, o in enumerate(ops):
            if o.get("barrier"):
                bl = list(last_on_eng.values()) + list(last_dma.values())
                pending = {e: bl for e in self.ENGS}
                last_writer = {}
                readers = {}
                continue
            if pending.get(o["eng"]):
                for j in pending[o["eng"]]:
                    deps[i][j] = "raw"
                pending[o["eng"]] = None
            if o["dma"]:
                last_dma[(o["semkey"], o["writes"], o["reads"])] = i
            else:
                last_on_eng[o["eng"]] = i
            for r in o["reads"]:
                j = last_writer.get(r)
                if j is not None:
                    deps[i][j] = "raw"
            for w in o["writes"]:
                j = last_writer.get(w)
                if j is not None and deps[i].get(j) != "raw":
                    deps[i][j] = "war"
                for j in readers.get(w, ()):
                    if j != i and deps[i].get(j) != "raw":
                        deps[i][j] = "war"
            for r in o["reads"]:
                readers.setdefault(r, []).append(i)
            for w in o["writes"]:
                last_writer[w] = i
                readers[w] = []
        for i, o in enumerate(ops):
            if o.get("barrier"):
                continue
            for j in list(deps[i].keys()):
                p = ops[j]
                if p["dma"]:
                    continue
                if p["eng"] == o["eng"] and not o["dma"]:
                    if o["eng"] == "pe" or (deps[i][j] == "war" and not STRICT_SAME_ENGINE):
                        del deps[i][j]
        dma_keys = {}
        for i, o in enumerate(ops):
            if o.get("barrier"):
                continue
            if o["dma"]:
                k = o["semkey"]
                if k is None:
                    k = o["writes"][0] if (o["writes"] and not o["writes"][0].startswith("dram:")) else o["reads"][0]
                o["_key"] = k
                dma_keys.setdefault(k, 0)
                dma_keys[k] += 1
                o["_cnt"] = 16 * dma_keys[k]
        needed = set()
        for i in range(n):
            for j in deps[i]:
                if not ops[j]["dma"]:
                    needed.add(j)
        cnt = {e: 0 for e in self.ENGS}
        for i, o in enumerate(ops):
            if o.get("barrier"):
                continue
            if not o["dma"] and i in needed:
                cnt[o["eng"]] += 1
                o["_cnt"] = cnt[o["eng"]]
        sems = {}
        for e in self.ENGS:
            sems["eng:" + e] = self.es.enter_context(nc.semaphore("sem_" + e))
        for k in dma_keys:
            sems["dma:" + k] = self.es.enter_context(nc.semaphore("dsem_%d" % len(sems)))
        assert len(sems) <= 100, "too many semaphores: %d" % len(sems)
        per_eng = {e: [] for e in self.ENGS}
        for i, o in enumerate(ops):
            if not o.get("barrier"):
                per_eng[o["eng"]].append(i)
        final_waits = [("dma:" + k, 16 * v) for k, v in dma_keys.items()]

        def run_engine(ename, eobj):
            waited = {}
            for i in per_eng[ename]:
                o = ops[i]
                need = {}
                for j in deps[i]:
                    p = ops[j]
                    sk = ("dma:" + p["_key"]) if p["dma"] else ("eng:" + p["eng"])
                    need[sk] = max(need.get(sk, 0), p["_cnt"])
                for sk, v in need.items():
                    if waited.get(sk, 0) < v:
                        eobj.wait_ge(sems[sk], v)
                        waited[sk] = v
                ins = o["fn"](eobj)
                if o["dma"]:
                    ins.then_inc(sems["dma:" + o["_key"]], 16)
                elif i in needed:
                    ins.then_inc(sems["eng:" + ename], 1)
            if ename == "sp":
                for sk, v in final_waits:
                    if waited.get(sk, 0) < v:
                        eobj.wait_ge(sems[sk], v)

        with nc.Block() as block:
            @block.tensor
            def _(e):
                run_engine("pe", e)

            @block.scalar
            def _(e):
                run_engine("act", e)

            @block.vector
            def _(e):
                run_engine("dve", e)

            @block.gpsimd
            def _(e):
                run_engine("pool", e)

            @block.sync
            def _(e):
                run_engine("sp", e)
        self.pes.close()
        self.mes.close()
        self.es.close()


CH = 342
NCH = TOK_PER_CORE // CH


def col_layout(v):
    v = np.ascontiguousarray(v, dtype=np.float32)
    return np.ascontiguousarray(v.reshape(-1, 128).T)


def emit_layernorm(P, x32, xres, N, ones_f, g_col, b_col, out32, outres, tag, out16=None, out16res=None):
    ps_m, rm = P._ln_ps[0]
    ps_q, rq = P._ln_ps[1]
    sq = P._ln_sq
    mean = P._ln_mean
    rstd = P._ln_rstd
    tmp = P._ln_tmp
    for k in range(8):
        P.op("pe", lambda e, k=k: e.matmul(ps_m[:, :N], ones_f[:], x32[:, k, :N], start=(k == 0), stop=(k == 7)),
             reads=[xres, "ones_f"], writes=[rm])
    for k in range(8):
        sb_ = sq[k % 2]
        sr = "ln_sq%d" % (k % 2)
        P.op("act", lambda e, k=k, sb_=sb_: e.activation(out=sb_[:, :N], in_=x32[:, k, :N], func=AF.Square), reads=[xres], writes=[sr])
        P.op("pe", lambda e, k=k, sb_=sb_: e.matmul(ps_q[:, :N], ones_f[:], sb_[:, :N], start=(k == 0), stop=(k == 7)),
             reads=[sr, "ones_f"], writes=[rq])
    P.op("act", lambda e: e.copy(out=mean[:, :N], in_=ps_m[:, :N]), reads=[rm], writes=["ln_mean"])
    P.op("dve", lambda e: e.tensor_tensor(out=rstd[:, :N], in0=mean[:, :N], in1=mean[:, :N], op=ALU.mult), reads=["ln_mean"], writes=["ln_rstd"])
    P.op("dve", lambda e: e.tensor_tensor(out=rstd[:, :N], in0=ps_q[:, :N], in1=rstd[:, :N], op=ALU.subtract), reads=[rq, "ln_rstd"], writes=["ln_rstd"])
    P.op("dve", lambda e: e.tensor_scalar(out=rstd[:, :N], in0=rstd[:, :N], scalar1=LN_EPS, scalar2=None, op0=ALU.add), reads=["ln_rstd"], writes=["ln_rstd"])
    P.op("act", lambda e: e.sqrt(out=rstd[:, :N], in_=rstd[:, :N]), reads=["ln_rstd"], writes=["ln_rstd"])
    P.op("dve", lambda e: e.reciprocal(out=rstd[:, :N], in_=rstd[:, :N]), reads=["ln_rstd"], writes=["ln_rstd"])
    for k in range(8):
        eng = "dve" if k % 2 == 0 else "pool"
        tb = tmp[k % 2]
        tr = "ln_tmp%d" % (k % 2)
        P.op(eng, lambda e, k=k, tb=tb: e.tensor_tensor(out=tb[:, :N], in0=x32[:, k, :N], in1=mean[:, :N], op=ALU.subtract),
             reads=[xres, "ln_mean"], writes=[tr])
        P.op(eng, lambda e, k=k, tb=tb: e.tensor_tensor(out=tb[:, :N], in0=tb[:, :N], in1=rstd[:, :N], op=ALU.mult),
             reads=[tr, "ln_rstd"], writes=[tr])
        P.op("dve", lambda e, k=k, tb=tb: e.tensor_scalar(out=out32[:, k, :N], in0=tb[:, :N], scalar1=g_col[:, k:k + 1], scalar2=b_col[:, k:k + 1],
                                                          op0=ALU.mult, op1=ALU.add),
             reads=[tr, "ln_gb_" + tag], writes=[outres])
        if out16 is not None:
            P.op("act", lambda e, k=k: e.copy(out=out16[:, k, :N], in_=out32[:, k, :N]), reads=[outres], writes=[out16res])


def ln_alloc(P, ps=None):
    if ps is None:
        ps = [(P.ps("ln_ps_m", [128, 512]), "ln_ps_m"), (P.ps("ln_ps_q", [128, 512]), "ln_ps_q")]
    P._ln_ps = ps
    P._ln_sq = [P.sb("ln_sq%d" % i, [128, CH], F32) for i in range(2)]
    P._ln_mean = P.sb("ln_mean", [128, CH], F32)
    P._ln_rstd = P.sb("ln_rstd", [128, CH], F32)
    P._ln_tmp = [P.sb("ln_tmp%d" % i, [128, CH], F32) for i in range(2)]


def emit_ident(P, ident, res="ident"):
    it = P.sb("ident_i", [128, 128], I32)
    P.op("pool", lambda e: e.iota(it[:], pattern=[[1, 128]], base=0, channel_multiplier=-1), writes=["ident_i"])
    P.op("dve", lambda e: e.tensor_single_scalar(out=ident[:], in_=it[:], scalar=0, op=ALU.is_equal), reads=["ident_i"], writes=[res])


def emit_B1(P, dr, h1T):
    nc = P.nc
    N = CH
    hT_v = dr["hT"].rearrange("(k p) t -> p k t", p=128)
    ssm_v = dr["ssmT"].rearrange("(k p) t -> p k t", p=128)
    conv_v = dr["convT"].rearrange("(k p) t -> p k t", p=128)
    attn_v = dr["attnT"].rearrange("(k p) t -> p k t", p=128)
    h1T_v = h1T.rearrange("(k p) t -> p k t", p=128)
    wglu = P.sb("wglu", [128, 4, 512], BF16)
    wsout = P.sb("wsout", [128, 4, 1024], BF16)
    wcout = P.sb("wcout", [128, 4, 1024], BF16)
    waout = P.sb("waout", [128, 8, 1024], BF16)
    wgate = P.sb("wgate", [128, 8, 3072], BF16)
    wo = P.sb("wo", [128, 8, 1024], BF16)
    for t, name, src in ((wglu, "wglu", "w_glu"), (wsout, "wsout", "w_sout"), (wcout, "wcout", "w_cout"),
                         (waout, "waout", "w_aout"), (wo, "wo", "w_o")):
        P.dma("pool", t[:], dr[src].rearrange("(k p) n -> p k n", p=128), writes=[name])
    gv = dr["w_gate"].rearrange("(k p) n -> p k n", p=128)
    for i in range(3):
        P.dma("pool", wgate[:, :, i * 1024:(i + 1) * 1024], gv[:, :, i * 1024:(i + 1) * 1024], writes=["wgate%d" % i])
    gb = P.sb("gb", [128, 24], F32)
    P.dma("sp", gb[:], dr["gate_b_c"], writes=["gb"])
    lg = P.sb("lng", [128, 8], F32)
    lb = P.sb("lnb", [128, 8], F32)
    P.dma("sp", lg[:], dr["ln1_g_c"], writes=["ln_gb_1"])
    P.dma("sp", lb[:], dr["ln1_b_c"], writes=["ln_gb_1"], semkey="lnb1")
    ones_f = P.sb("ones_f", [128, 128], F32)
    P.op("dve", lambda e: e.memset(ones_f[:], 1.0 / D), writes=["ones_f"])
    ln_alloc(P)
    h32_b = [P.sb("h32_0", [128, 8, N], F32)] * 2
    h16 = P.sb("h16", [128, 8, N], BF16)
    s32_b = [P.sb("s32_%d" % i, [128, 4, N], F32) for i in range(2)]
    s16 = P.sb("s16", [128, 4, N], BF16)
    c16_b = [P.sb("c16_%d" % i, [128, 4, N], BF16) for i in range(2)]
    a16_b = [P.sb("a16_%d" % i, [128, 8, N], BF16) for i in range(2)]

    def b1_load(c):
        t0 = c * N
        i = c % 2
        P.dma("sp", s32_b[i][:], ssm_v[:, :, t0:t0 + N], writes=["s32_%d" % i])
        P.dma("pool", c16_b[i][:], conv_v[:, :, t0:t0 + N], writes=["c16_%d" % i])
        P.dma("pool", a16_b[i][:], attn_v[:, :, t0:t0 + N], writes=["a16_%d" % i])
    sg16 = P.sb("sg16", [128, 4, N], BF16)
    sig = [P.sb("sig%d" % i, [128, N], F32) for i in range(2)]
    gt = [P.sb("gt%d" % i, [128, N], F32) for i in range(2)]
    macc = P.sb("macc", [128, N], F32)
    mtmp = [P.sb("mtmp%d" % i, [128, N], F32) for i in range(2)]
    m16 = P.sb("m16", [128, 8, N], BF16)
    res32 = P.sb("res32", [128, 8, N], F32)
    o32 = P.sb("o32", [128, 8, N], F32)
    psY = [P.ps("psY%d" % i, [128, 512]) for i in range(2)]
    psG = [P.ps("psG%d" % i, [128, 512]) for i in range(2)]
    cnt = 0
    b1_load(0)
    P.dma("sp", h32_b[0][:], hT_v[:, :, 0:N], writes=["h32_0"])
    for c in range(NCH):
        t0 = c * N
        if c + 1 < NCH:
            b1_load(c + 1)
        h32, s32, c16, a16 = h32_b[c % 2], s32_b[c % 2], c16_b[c % 2], a16_b[c % 2]
        H32, S32, C16, A16 = "h32_0", "s32_%d" % (c % 2), "c16_%d" % (c % 2), "a16_%d" % (c % 2)
        P.op("act", lambda e, h32=h32: e.copy(out=h16[:], in_=h32[:]), reads=[H32], writes=["h16"])
        P.op("act", lambda e, s32=s32: e.copy(out=s16[:], in_=s32[:]), reads=[S32], writes=["s16"])
        for j in range(4):
            b = cnt % 2
            cnt += 1
            for k in range(4):
                P.op("pe", lambda e, j=j, k=k, b=b: e.matmul(psY[b][:, :N], wglu[:, k, j * 128:(j + 1) * 128], s16[:, k, :], start=(k == 0), stop=(k == 3)),
                     reads=["wglu", "s16"], writes=["psY%d" % b])
            P.op("act", lambda e, b=b: e.activation(out=sig[b][:], in_=psY[b][:, :N], func=AF.Sigmoid), reads=["psY%d" % b], writes=["sig%d" % b])
            P.op("dve", lambda e, j=j, b=b, s32=s32: e.tensor_tensor(out=sg16[:, j, :], in0=s32[:, j, :], in1=sig[b][:], op=ALU.mult),
                 reads=[S32, "sig%d" % b], writes=["sg16"])
        branches = ((wsout, "wsout", sg16, "sg16", 4), (wcout, "wcout", c16, C16, 4), (waout, "waout", a16, A16, 8))
        for m in range(8):
            for br, (wt, wname, xt, xname, nk) in enumerate(branches):
                b = cnt % 2
                cnt += 1
                for k in range(nk):
                    P.op("pe", lambda e, wt=wt, xt=xt, k=k, m=m, b=b, nk=nk: e.matmul(psY[b][:, :N], wt[:, k, m * 128:(m + 1) * 128], xt[:, k, :],
                                                                                    start=(k == 0), stop=(k == nk - 1)),
                         reads=[wname, xname], writes=["psY%d" % b])
                gc0 = br * 1024 + m * 128
                for k in range(8):
                    P.op("pe", lambda e, k=k, gc0=gc0, b=b: e.matmul(psG[b][:, :N], wgate[:, k, gc0:gc0 + 128], h16[:, k, :], start=(k == 0), stop=(k == 7)),
                         reads=["wgate%d" % br, "h16"], writes=["psG%d" % b])
                P.op("act", lambda e, b=b, br=br, m=m: e.activation(out=gt[b][:], in_=psG[b][:, :N], func=AF.Sigmoid, bias=gb[:, br * 8 + m:br * 8 + m + 1]),
                     reads=["psG%d" % b, "gb"], writes=["gt%d" % b])
                if br == 0:
                    P.op("dve", lambda e, b=b: e.tensor_tensor(out=macc[:], in0=gt[b][:], in1=psY[b][:, :N], op=ALU.mult),
                         reads=["gt%d" % b, "psY%d" % b], writes=["macc"])
                else:
                    P.op("dve", lambda e, b=b: e.tensor_tensor(out=mtmp[b][:], in0=gt[b][:], in1=psY[b][:, :N], op=ALU.mult),
                         reads=["gt%d" % b, "psY%d" % b], writes=["mtmp%d" % b])
                    if br == 1:
                        P.op("pool", lambda e, b=b: e.tensor_tensor(out=macc[:], in0=macc[:], in1=mtmp[b][:], op=ALU.add),
                             reads=["macc", "mtmp%d" % b], writes=["macc"])
                    else:
                        P.op("pool", lambda e, b=b, m=m: e.tensor_tensor(out=m16[:, m, :], in0=macc[:], in1=mtmp[b][:], op=ALU.add),
                             reads=["macc", "mtmp%d" % b], writes=["m16"])
        for m in range(8):
            b = cnt % 2
            cnt += 1
            for k in range(8):
                P.op("pe", lambda e, k=k, m=m, b=b: e.matmul(psY[b][:, :N], wo[:, k, m * 128:(m + 1) * 128], m16[:, k, :], start=(k == 0), stop=(k == 7)),
                     reads=["wo", "m16"], writes=["psY%d" % b])
            P.op("dve", lambda e, m=m, b=b, h32=h32: e.scalar_tensor_tensor(out=res32[:, m, :], in0=h32[:, m, :], scalar=DN_ALPHA, in1=psY[b][:, :N],
                                                                   op0=ALU.mult, op1=ALU.add),
                 reads=[H32, "psY%d" % b], writes=["res32"])
        if c + 1 < NCH:
            P.dma("sp", h32_b[0][:], hT_v[:, :, t0 + N:t0 + 2 * N], writes=["h32_0"])
        emit_layernorm(P, res32, "res32", N, ones_f, lg, lb, o32, "o32", "1")
        P.dma("sp", h1T_v[:, :, t0:t0 + N], o32[:], reads=["o32"], writes=["dram:h1T"])


def emit_B2(P, dr, h1T, cwT_d, outT, n_experts=NE):
    nc = P.nc
    N = CH
    TOK = TOK_PER_CORE
    h1T_v = h1T.rearrange("(k p) t -> p k t", p=128)
    outT_v = outT.rearrange("(k p) t -> p k t", p=128)
    h16 = P.sbm("h16", [128, 8, TOK], BF16)
    acc = P.sbm("acc", [128, 8, TOK], F32)
    bgu = P.sbm("bgu", [128, NE, 16], F32)
    lg2 = P.sbm("lng2", [128, 8], F32)
    lb2 = P.sbm("lnb2", [128, 8], F32)
    ones_f = P.sbm("ones_f", [128, 128], F32)
    P.dma("sp", bgu[:], dr["bgu_c"], writes=["bgu"])
    P.dma("sp", lg2[:], dr["ln2_g_c"], writes=["ln_gb_2"])
    P.dma("sp", lb2[:], dr["ln2_b_c"], writes=["ln_gb_2"], semkey="lnb2")
    P.op("dve", lambda e: e.memset(ones_f[:], 1.0 / D), writes=["ones_f"])
    P.op("dve", lambda e: e.tensor_scalar(out=bgu[:, :, 8:16], in0=bgu[:, :, 8:16], scalar1=1.0, scalar2=None, op0=ALU.add),
         reads=["bgu"], writes=["bgu"])
    ident = P.sb("ident", [128, 128], F32)
    emit_ident(P, ident)
    rw = P.sb("rw", [128, 8, NE], F32)
    P.dma("sp", rw[:], dr["router_w"].rearrange("(k p) e -> p k e", p=128), writes=["rw"])
    rb = P.sb("rb", [128, NE], F32)
    P.dma("sp", rb[:], dr["router_b_bc"], writes=["rb"])
    bd = P.sb("bd", [NE, D], F32)
    P.dma("sp", bd[:], dr["b_down"], writes=["bd"])
    cwT = P.sb("cwT", [NE, TOK], F32)
    h32 = [P.sb("h32_%d" % i, [128, 8, N], F32) for i in range(2)]
    lgt = P.sb("lgt", [128, NE], F32)
    m8 = P.sb("m8", [128, 8], F32)
    negm = P.sb("negm", [128, 1], F32)
    ex = P.sb("ex", [128, NE], F32)
    exm = P.sb("exm", [128, NE], F32)
    ssum = P.sb("ssum", [128, 1], F32)
    cw = P.sb("cw", [128, NE], F32)
    ps_r = P.ps("ps_r", [128, 512])
    ps_t = P.ps("ps_t", [128, 512])
    ps_b = [P.ps("ps_b%d" % i, [128, 512]) for i in range(2)]
    for c in range(NCH):
        t0 = c * N
        hb = h32[c % 2]
        hres = "h32_%d" % (c % 2)
        P.dma("sp", hb[:], h1T_v[:, :, t0:t0 + N], reads=["dram:h1T"], writes=[hres])
        P.op("act", lambda e, hb=hb, t0=t0: e.copy(out=h16[:, :, t0:t0 + N], in_=hb[:]), reads=[hres], writes=["h16"])
        off = 0
        while off < N:
            ts = min(128, N - off)
            for k in range(8):
                P.op("pe", lambda e, hb=hb, k=k, off=off, ts=ts: e.matmul(ps_r[:ts, :NE], hb[:, k, off:off + ts], rw[:, k, :], start=(k == 0), stop=(k == 7)),
                     reads=[hres, "rw"], writes=["ps_r"])
            P.op("dve", lambda e, ts=ts: e.tensor_tensor(out=lgt[:ts, :], in0=ps_r[:ts, :NE], in1=rb[:ts, :], op=ALU.add), reads=["ps_r", "rb"], writes=["lgt"])
            P.op("dve", lambda e, ts=ts: e.max(out=m8[:ts, :], in_=lgt[:ts, :]), reads=["lgt"], writes=["m8"])
            P.op("dve", lambda e, ts=ts: e.tensor_scalar(out=negm[:ts, :], in0=m8[:ts, 0:1], scalar1=-1.0, scalar2=None, op0=ALU.mult), reads=["m8"], writes=["negm"])
            P.op("act", lambda e, ts=ts: e.activation(out=ex[:ts, :], in_=lgt[:ts, :], func=AF.Exp, bias=negm[:ts, :]), reads=["lgt", "negm"], writes=["ex"])
            P.op("dve", lambda e, ts=ts: e.scalar_tensor_tensor(out=exm[:ts, :], in0=lgt[:ts, :], scalar=m8[:ts, 3:4], in1=ex[:ts, :], op0=ALU.is_ge, op1=ALU.mult,
                                                                accum_out=ssum[:ts, :]),
                 reads=["lgt", "m8", "ex"], writes=["exm", "ssum"])
            P.op("dve", lambda e, ts=ts: e.reciprocal(out=ssum[:ts, :], in_=ssum[:ts, :]), reads=["ssum"], writes=["ssum"])
            P.op("dve", lambda e, ts=ts: e.tensor_scalar(out=cw[:ts, :], in0=exm[:ts, :], scalar1=ssum[:ts, 0:1], scalar2=None, op0=ALU.mult), reads=["exm", "ssum"], writes=["cw"])
            P.op("pe", lambda e, ts=ts: e.transpose(ps_t[:NE, :ts], cw[:ts, :], ident[:ts, :ts]), reads=["cw", "ident"], writes=["ps_t"])
            P.op("act", lambda e, ts=ts, a=t0 + off: e.copy(out=cwT[:, a:a + ts], in_=ps_t[:NE, :ts]), reads=["ps_t"], writes=["cwT"])
            off += ts
        for m in range(8):
            b = m % 2
            P.op("pe", lambda e, m=m, b=b, t0=t0: e.matmul(ps_b[b][:, :N], bd[:, m * 128:(m + 1) * 128], cwT[:, t0:t0 + N], start=True, stop=True),
                 reads=["bd", "cwT"], writes=["ps_b%d" % b])
            P.op("dve", lambda e, m=m, b=b, hb=hb, t0=t0: e.scalar_tensor_tensor(out=acc[:, m, t0:t0 + N], in0=hb[:, m, :], scalar=DN_ALPHA, in1=ps_b[b][:, :N],
                                                                                 op0=ALU.mult, op1=ALU.add),
                 reads=[hres, "ps_b%d" % b], writes=["acc%d" % c])
    P.dma("sp", cwT_d, cwT[:], reads=["cwT"], writes=["dram:cwT"])
    P.new_phase(keep_mid=True)
    hid = P.sb("hid", [128, 8, TOK], BF16)
    NSLOT = 4
    ring = [P.sb("ring%d" % i, [128, 8, 512], BF16) for i in range(NSLOT)]
    cwB = [P.sb("cwB%d" % i, [128, TOK], F32) for i in range(2)]
    gc = [P.sb("gc%d" % i, [128, N], F32) for i in range(3)]
    sg = [P.sb("sg%d" % i, [128, N], F32) for i in range(3)]
    uc = [P.sb("uc%d" % i, [128, N], F32) for i in range(3)]
    tt = [P.sb("tt%d" % i, [128, N], F32) for i in range(3)]
    hd = [P.sb("hd%d" % i, [128, N], F32) for i in range(3)]
    psG = [P.ps("psG%d" % i, [128, 512]) for i in range(3)]
    psU = [P.ps("psU%d" % i, [128, 512]) for i in range(3)]
    psO = [P.ps("psO%d" % i, [128, 512]) for i in range(2)]
    it = 0
    io = 0
    wunits = [(ex_i, kind, idx) for ex_i in range(n_experts) for kind, idx in (("gu", 0), ("gu", 1), ("gu", 2), ("gu", 3), ("dn", 0), ("dn", 1))]

    def load_unit(u):
        ex_i, kind, idx = wunits[u]
        W = ring[u % NSLOT]
        wres = "ring%d" % (u % NSLOT)
        if kind == "gu":
            wgu_v = dr["w_gu"][ex_i].rearrange("(k p) n -> p k n", p=128)
            P.dma("pool", W[:, :, 0:256], wgu_v[:, :, idx * 256:(idx + 1) * 256], writes=[wres])
            P.dma("pool", W[:, :, 256:512], wgu_v[:, :, DFF + idx * 256:DFF + (idx + 1) * 256], writes=[wres], semkey=wres + "u")
        else:
            wd_v = dr["w_down"][ex_i].rearrange("(k p) n -> p k n", p=128)
            P.dma("pool", W[:], wd_v[:, :, idx * 512:(idx + 1) * 512], writes=[wres])

    PF = 2
    for u in range(min(PF, len(wunits))):
        load_unit(u)
    for u, (ex_i, kind, idx) in enumerate(wunits):
        if u + PF < len(wunits):
            load_unit(u + PF)
        W = ring[u % NSLOT]
        wres = "ring%d" % (u % NSLOT)
        cb = cwB[ex_i % 2]
        cres = "cwB%d" % (ex_i % 2)
        if kind == "gu" and idx == 0:
            P.dma("sp", cb[:], cwT_d[ex_i:ex_i + 1, :].partition_broadcast(128), reads=["dram:cwT"], writes=[cres])
        if kind == "gu":
            q = idx
            for c in range(NCH):
                t0 = c * N
                for jj in range(2):
                    j = 2 * q + jj
                    b = it % 3
                    it += 1
                    for k in range(8):
                        P.op("pe", lambda e, W=W, k=k, jj=jj, b=b, t0=t0: e.matmul(psG[b][:, :N], W[:, k, jj * 128:(jj + 1) * 128], h16[:, k, t0:t0 + N],
                                                                                  start=(k == 0), stop=(k == 7)),
                             reads=[wres, "h16"], writes=["psG%d" % b])
                    for k in range(8):
                        P.op("pe", lambda e, W=W, k=k, jj=jj, b=b, t0=t0: e.matmul(psU[b][:, :N], W[:, k, 256 + jj * 128:256 + (jj + 1) * 128], h16[:, k, t0:t0 + N],
                                                                                  start=(k == 0), stop=(k == 7)),
                             reads=[wres, "h16"], writes=["psU%d" % b])
                    P.op("dve", lambda e, b=b, j=j, ex_i=ex_i: e.tensor_scalar(out=gc[b][:], in0=psG[b][:, :N], scalar1=bgu[:, ex_i, j:j + 1], scalar2=SW_LIMIT,
                                                                               op0=ALU.add, op1=ALU.min),
                         reads=["psG%d" % b, "bgu"], writes=["gc%d" % b])
                    P.op("act", lambda e, b=b: e.activation(out=sg[b][:], in_=gc[b][:], func=AF.Sigmoid, scale=SW_ALPHA), reads=["gc%d" % b], writes=["sg%d" % b])
                    P.op("dve", lambda e, b=b, j=j, ex_i=ex_i: e.tensor_scalar(out=uc[b][:], in0=psU[b][:, :N], scalar1=bgu[:, ex_i, 8 + j:9 + j], scalar2=SW_LIMIT + 1.0,
                                                                               op0=ALU.add, op1=ALU.min),
                         reads=["psU%d" % b, "bgu"], writes=["uc%d" % b])
                    P.op("pool", lambda e, b=b: e.tensor_tensor(out=tt[b][:], in0=gc[b][:], in1=sg[b][:], op=ALU.mult),
                         reads=["gc%d" % b, "sg%d" % b], writes=["tt%d" % b])
                    P.op("dve", lambda e, b=b: e.scalar_tensor_tensor(out=hd[b][:], in0=uc[b][:], scalar=-SW_LIMIT + 1.0, in1=tt[b][:], op0=ALU.max, op1=ALU.mult),
                         reads=["uc%d" % b, "tt%d" % b], writes=["hd%d" % b])
                    P.op("pool", lambda e, b=b, j=j, cb=cb, t0=t0: e.tensor_tensor(out=hid[:, j, t0:t0 + N], in0=hd[b][:], in1=cb[:, t0:t0 + N], op=ALU.mult),
                         reads=["hd%d" % b, cres], writes=["hid%d" % c])
        else:
            mh = idx
            for c in range(NCH):
                t0 = c * N
                for mm in range(4):
                    m = 4 * mh + mm
                    b = io % 2
                    io += 1
                    for j in range(8):
                        P.op("pe", lambda e, W=W, j=j, mm=mm, b=b, t0=t0: e.matmul(psO[b][:, :N], W[:, j, mm * 128:(mm + 1) * 128], hid[:, j, t0:t0 + N],
                                                                                  start=(j == 0), stop=(j == 7)),
                             reads=[wres, "hid%d" % c], writes=["psO%d" % b])
                    P.op("dve", lambda e, m=m, b=b, t0=t0: e.tensor_tensor(out=acc[:, m, t0:t0 + N], in0=acc[:, m, t0:t0 + N], in1=psO[b][:, :N], op=ALU.add),
                         reads=["acc%d" % c, "psO%d" % b], writes=["acc%d" % c])
    P.new_phase(keep_mid=True)
    ln_alloc(P)
    o32 = [P.sb("o32_%d" % i, [128, 8, N], F32) for i in range(2)]
    for c in range(NCH):
        t0 = c * N
        ob = o32[c % 2]
        ores = "o32_%d" % (c % 2)
        emit_layernorm(P, acc[:, :, t0:t0 + N], "acc%d" % c, N, ones_f, lg2, lb2, ob, ores, "2")
        P.dma("sp", outT_v[:, :, t0:t0 + N], ob[:], reads=[ores], writes=["dram:outT"])


B_INPUTS = [("hT", [D, TOK_PER_CORE]), ("ssmT", [SSM_W, TOK_PER_CORE]), ("convT", [CONV_W, TOK_PER_CORE]), ("attnT", [ATT_W, TOK_PER_CORE]),
            ("w_glu", [SSM_W, SSM_W]), ("w_sout", [SSM_W, D]), ("w_cout", [CONV_W, D]), ("w_aout", [ATT_W, D]), ("w_gate", [D, 3 * D]),
            ("gate_b_c", [128, 24]), ("w_o", [D, D]), ("ln1_g_c", [128, 8]), ("ln1_b_c", [128, 8]), ("router_w", [D, NE]),
            ("router_b_bc", [128, NE]), ("w_gu", [NE, D, 2 * DFF]), ("bgu_c", [128, NE, 16]), ("w_down", [NE, DFF, D]), ("b_down", [NE, D]),
            ("ln2_g_c", [128, 8]), ("ln2_b_c", [128, 8])]


def build_B(n_experts=NE):
    nc = bass.Bass("TRN2", target_bir_lowering=False)
    dr = {name: nc.dram_tensor(name, shape, F32, kind="ExternalInput").ap() for name, shape in B_INPUTS}
    outT = nc.dram_tensor("outT", [D, TOK_PER_CORE], F32, kind="ExternalOutput").ap()
    h1T = nc.dram_tensor("h1T_scr", [D, TOK_PER_CORE], F32, kind="Internal").ap()
    cwT_d = nc.dram_tensor("cwT_scr", [NE, TOK_PER_CORE], F32, kind="Internal").ap()
    P = Prog(nc)
    emit_B1(P, dr, h1T)
    P.new_phase()
    emit_B2(P, dr, h1T, cwT_d, outT, n_experts=n_experts)
    P.emit()
    return nc


def host_B_weights(inp, l):
    f = np.float32
    bgu = np.asarray(inp["expert_b_gu"][l], f)
    return {
        "w_glu": np.ascontiguousarray(inp["ssm_w_glu"][l], f), "w_sout": np.ascontiguousarray(inp["ssm_w_out"][l], f),
        "w_cout": np.ascontiguousarray(inp["conv_w_out"][l], f), "w_aout": np.ascontiguousarray(inp["attn_w_out"][l], f),
        "w_gate": np.ascontiguousarray(inp["gate_w"][l], f), "gate_b_c": col_layout(inp["gate_b"][l]),
        "w_o": np.ascontiguousarray(inp["w_o"][l], f), "ln1_g_c": col_layout(inp["ln1_g"][l]), "ln1_b_c": col_layout(inp["ln1_b"][l]),
        "router_w": np.ascontiguousarray(inp["router_w"][l], f),
        "router_b_bc": np.ascontiguousarray(np.broadcast_to(np.asarray(inp["router_b"][l], f)[None, :], (128, NE))),
        "w_gu": np.ascontiguousarray(inp["expert_w_gu"][l], f),
        "bgu_c": np.ascontiguousarray(bgu.reshape(NE, 16, 128).transpose(2, 0, 1)),
        "w_down": np.ascontiguousarray(inp["expert_w_down"][l], f), "b_down": np.ascontiguousarray(inp["expert_b_down"][l], f),
        "ln2_g_c": col_layout(inp["ln2_g"][l]), "ln2_b_c": col_layout(inp["ln2_b"][l]),
    }


CW1 = 6.28125
CW2 = TWO_PI - 6.28125


def sincos_alloc(P, shape, tag):
    p, n = shape
    return [P.sb("sc_%s_%s" % (x, tag), [p, n], I32 if x == "k" else F32) for x in ("y", "k", "kf", "r", "r2", "m")]


def emit_sincos(P, scr, ang, ares, cos_out, cres, sin_out, sres, tag):
    y, ki, kf, r, r2, m = scr
    ry, rk, rkf, rr, rr2, rm = ["sc_%s_%s" % (x, tag) for x in ("y", "k", "kf", "r", "r2", "m")]
    P.op("dve", lambda e: e.tensor_scalar(out=y[:], in0=ang, scalar1=1.0 / TWO_PI, scalar2=None, op0=ALU.mult), reads=[ares], writes=[ry])
    P.op("dve", lambda e: e.tensor_copy(out=ki[:], in_=y[:]), reads=[ry], writes=[rk])
    P.op("dve", lambda e: e.tensor_copy(out=kf[:], in_=ki[:]), reads=[rk], writes=[rkf])
    P.op("dve", lambda e: e.scalar_tensor_tensor(out=r[:], in0=kf[:], scalar=-CW1, in1=ang, op0=ALU.mult, op1=ALU.add), reads=[rkf, ares], writes=[rr])
    P.op("dve", lambda e: e.scalar_tensor_tensor(out=r[:], in0=kf[:], scalar=-CW2, in1=r[:], op0=ALU.mult, op1=ALU.add), reads=[rkf, rr], writes=[rr])
    P.op("dve", lambda e: e.tensor_scalar(out=m[:], in0=r[:], scalar1=math.pi / 2, scalar2=-TWO_PI, op0=ALU.is_gt, op1=ALU.mult), reads=[rr], writes=[rm])
    P.op("dve", lambda e: e.scalar_tensor_tensor(out=r2[:], in0=r[:], scalar=math.pi / 2, in1=m[:], op0=ALU.add, op1=ALU.add), reads=[rr, rm], writes=[rr2])
    P.op("dve", lambda e: e.tensor_scalar(out=r[:], in0=r[:], scalar1=-math.pi, scalar2=math.pi, op0=ALU.max, op1=ALU.min), reads=[rr], writes=[rr])
    P.op("dve", lambda e: e.tensor_scalar(out=r2[:], in0=r2[:], scalar1=-math.pi, scalar2=math.pi, op0=ALU.max, op1=ALU.min), reads=[rr2], writes=[rr2])
    P.op("act", lambda e: e.activation(out=sin_out, in_=r[:], func=AF.Sin), reads=[rr], writes=[sres])
    P.op("act", lambda e: e.activation(out=cos_out, in_=r2[:], func=AF.Sin), reads=[rr2], writes=[cres])


NCHA = 17
NSUB = 3
SSM_EVERY = 10
NEG = -1.0e30


def chunkA(c):
    t0 = c * 512
    return t0, min(512, LP - t0)


def emit_A(P, dr, lam_init):
    nc = P.nc
    hT_v = dr["hT"].rearrange("(k p) t -> p k t", p=128)
    uT = P.sbm("uT", [128, LP], BF16)
    qT = [P.sbm("qT%d" % h, [128, LP], BF16) for h in range(2)]
    kT = [P.sbm("kT%d" % h, [128, LP], BF16) for h in range(2)]
    Va = [P.sbm("Va%d" % h, [128, NBLK, 130], BF16) for h in range(2)]
    ident = P.sbm("ident", [128, 128], F32)
    ident16 = P.sbm("ident16", [128, 128], BF16)
    emit_ident(P, ident)
    P.op("act", lambda e: e.copy(out=ident16[:], in_=ident[:]), reads=["ident"], writes=["ident16"])
    w16 = P.sb("w16", [128, 8, 1280], BF16)
    wv = dr["w_sel"].rearrange("(k p) n -> p k n", p=128)
    for i in range(2):
        P.dma("pool", w16[:, :, i * 640:(i + 1) * 640], wv[:, :, i * 640:(i + 1) * 640], writes=["w16_%d" % i])
    WR = ["w16_0", "w16_1"]
    wrot = P.sb("wrot", [128, 8, 512], BF16)
    for t in range(4):
        src0 = 512 + t * 128
        for c2 in range(2):
            s = src0 + c2 * 64
            d = t * 128 + c2 * 64
            P.op("act", lambda e, s=s, d=d: e.activation(out=wrot[:, :, d:d + 32], in_=w16[:, :, s + 32:s + 64], func=AF.Copy, scale=-1.0),
                 reads=WR, writes=["wrot"])
            P.op("act", lambda e, s=s, d=d: e.copy(out=wrot[:, :, d + 32:d + 64], in_=w16[:, :, s:s + 32]), reads=WR, writes=["wrot"])
    invf = P.sb("invf", [128, 1], F32)
    P.dma("sp", invf[:], dr["inv_c"], writes=["invf"])
    cw3 = P.sb("cw3", [128, 3], F32)
    P.dma("sp", cw3[:], dr["conv_w_c"], writes=["cw3"])
    h16 = [P.sb("h16_%d" % i, [128, 8, 512], BF16) for i in range(2)]
    posi = P.sb("posi", [128, 512], I32)
    posf = P.sb("posf", [128, 512], F32)
    ang = P.sb("ang", [128, 512], F32)
    cosT = P.sb("cosT", [128, 512], F32)
    sinT = P.sb("sinT", [128, 512], F32)
    sc_rope = sincos_alloc(P, [128, 512], "rope")
    zb = P.sb("zb", [128, 514], F32)
    ccs = P.sb("ccs", [128, 512], F32)
    cy = P.sb("cy", [128, 512], F32)
    cyo = [P.sb("cyo%d" % i, [128, 512], F32) for i in range(2)]
    rq1 = P.sb("rq1", [128, 512], F32)
    rq2 = P.sb("rq2", [128, 512], F32)
    psA = [P.ps("psA%d" % i, [128, 512]) for i in range(4)]
    psV = [P.ps("psV%d" % i, [128, 512]) for i in range(2)]
    P.op("dve", lambda e: e.memset(zb[:, 0:2], 0.0), writes=["zb"])
    for h in range(2):
        P.op("pool", lambda e, h=h: e.memset(Va[h][:, :, 128:130], 1.0), writes=["Va%d" % h])
    P.op("pool", lambda e: e.iota(posi[:], pattern=[[1, 512]], base=0, channel_multiplier=0), writes=["posi"])
    P.op("dve", lambda e: e.tensor_copy(out=posf[:], in_=posi[:]), reads=["posi"], writes=["posf"])
    pa = 0
    convT_out = dr["convT_out"]

    def proj(wt, wres, col0, hb, hres, n, ps, pres):
        for k in range(8):
            P.op("pe", lambda e, k=k: e.matmul(ps[:, :n], wt[:, k, col0:col0 + 128], hb[:, k, :n], start=(k == 0), stop=(k == 7)),
                 reads=wres + [hres], writes=[pres])

    for c in range(NCHA):
        t0, n = chunkA(c)
        hb = h16[c % 2]
        hres = "h16_%d" % (c % 2)
        if c == 0:
            P.dma("pool", hb[:, :, :n], hT_v[:, :, t0:t0 + n], writes=[hres])
        if c + 1 < NCHA:
            t0n, nn = chunkA(c + 1)
            P.dma("pool", h16[(c + 1) % 2][:, :, :nn], hT_v[:, :, t0n:t0n + nn], writes=["h16_%d" % ((c + 1) % 2)])
        P.op("dve", lambda e, t0=t0: e.tensor_scalar(out=ang[:], in0=posf[:], scalar1=float(t0), scalar2=invf[:, 0:1], op0=ALU.add, op1=ALU.mult),
             reads=["posf", "invf"], writes=["ang"])
        emit_sincos(P, sc_rope, ang[:], "ang", cosT[:], "cosT", sinT[:], "sinT", "rope")
        ps = psA[pa % 4]; pres = "psA%d" % (pa % 4); pa += 1
        proj(w16, WR, 0, hb, hres, n, ps, pres)
        P.op("act", lambda e, ps=ps, t0=t0, n=n: e.copy(out=uT[:, t0:t0 + n], in_=ps[:, :n]), reads=[pres], writes=["uT"])
        psb = psA[pa % 4]; rb_ = "psA%d" % (pa % 4); pa += 1
        proj(w16, WR, 128, hb, hres, n, psb, rb_)
        psc = psA[pa % 4]; rc_ = "psA%d" % (pa % 4); pa += 1
        proj(w16, WR, 256, hb, hres, n, psc, rc_)
        psh = psA[pa % 4]; rh_ = "psA%d" % (pa % 4); pa += 1
        proj(w16, WR, 384, hb, hres, n, psh, rh_)
        P.op("act", lambda e, psc=psc, n=n: e.copy(out=ccs[:, :n], in_=psc[:, :n]), reads=[rc_], writes=["ccs"])
        P.op("dve", lambda e, psh=psh, n=n: e.tensor_tensor(out=zb[:, 2:2 + n], in0=ccs[:, :n], in1=psh[:, :n], op=ALU.mult), reads=["ccs", rh_, "zb"], writes=["zb"])
        P.op("dve", lambda e, n=n: e.tensor_scalar(out=cy[:, :n], in0=zb[:, 2:2 + n], scalar1=cw3[:, 2:3], scalar2=None, op0=ALU.mult), reads=["zb", "cw3"], writes=["cy"])
        P.op("dve", lambda e, n=n: e.scalar_tensor_tensor(out=cy[:, :n], in0=zb[:, 1:1 + n], scalar=cw3[:, 1:2], in1=cy[:, :n], op0=ALU.mult, op1=ALU.add),
             reads=["zb", "cw3", "cy"], writes=["cy"])
        P.op("dve", lambda e, n=n: e.scalar_tensor_tensor(out=cy[:, :n], in0=zb[:, 0:n], scalar=cw3[:, 0:1], in1=cy[:, :n], op0=ALU.mult, op1=ALU.add),
             reads=["zb", "cw3", "cy"], writes=["cy"])
        co = cyo[c % 2]; cores_ = "cyo%d" % (c % 2)
        P.op("dve", lambda e, psb=psb, n=n, co=co: e.tensor_tensor(out=co[:, :n], in0=cy[:, :n], in1=psb[:, :n], op=ALU.mult), reads=["cy", rb_], writes=[cores_])
        nv = min(n, L - t0)
        if nv > 0:
            P.dma("sp", convT_out[:, t0:t0 + nv], co[:, :nv], reads=[cores_], writes=["dram:convT_out"])
        P.op("act", lambda e, n=n: e.copy(out=zb[:, 0:2], in_=zb[:, n:n + 2]), reads=["zb"], writes=["zb"])
        for t in range(4):
            dst = (qT, kT)[t // 2][t % 2]
            dres = ("qT%d", "kT%d")[t // 2] % (t % 2)
            p1 = psA[pa % 4]; r1 = "psA%d" % (pa % 4); pa += 1
            proj(w16, WR, 512 + t * 128, hb, hres, n, p1, r1)
            p2 = psA[pa % 4]; r2 = "psA%d" % (pa % 4); pa += 1
            proj(wrot, ["wrot"], t * 128, hb, hres, n, p2, r2)
            P.op("dve", lambda e, p1=p1, n=n: e.tensor_tensor(out=rq1[:, :n], in0=cosT[:, :n], in1=p1[:, :n], op=ALU.mult), reads=["cosT", r1], writes=["rq1"])
            P.op("dve", lambda e, p2=p2, n=n: e.tensor_tensor(out=rq2[:, :n], in0=sinT[:, :n], in1=p2[:, :n], op=ALU.mult), reads=["sinT", r2], writes=["rq2"])
            P.op("pool", lambda e, dst=dst, t0=t0, n=n: e.tensor_tensor(out=dst[:, t0:t0 + n], in0=rq1[:, :n], in1=rq2[:, :n], op=ALU.add),
                 reads=["rq1", "rq2"], writes=[dres])
        for s in range(n // 128):
            pv = psV[s % 2]; rv = "psV%d" % (s % 2)
            for k in range(8):
                P.op("pe", lambda e, k=k, s=s, pv=pv, hb=hb: e.matmul(pv[:, :256], hb[:, k, s * 128:(s + 1) * 128], w16[:, k, 1024:1280], start=(k == 0), stop=(k == 7)),
                     reads=WR + [hres], writes=[rv])
            blk = t0 // 128 + s
            for h in range(2):
                P.op("act", lambda e, h=h, blk=blk, pv=pv: e.copy(out=Va[h][:, blk, 0:128], in_=pv[:, h * 128:(h + 1) * 128]), reads=[rv], writes=["Va%d" % h])
    P.new_phase(keep_mid=True)
    T = emit_A2_ssm(P, dr, uT, ident)
    P.new_phase(keep_mid=True)
    it_s = ssm_steps(P, dr, uT, T)
    k = 0
    for _ in attn_steps(P, dr, qT, kT, Va, ident16, lam_init):
        k += 1
        if k % SSM_EVERY == 0:
            next(it_s, None)
    for _ in it_s:
        pass


TC = 256
NCHS = (LP + TC - 1) // TC


def chunkS(c):
    t0 = c * TC
    return t0, min(TC, LP - t0)


def emit_A2_ssm(P, dr, uT, ident):
    nc = P.nc
    ssmT_out = dr["ssmT_out"]
    lr = P.sb("lr", [128, 4], F32)
    li = P.sb("li", [128, 4], F32)
    ldt = P.sb("ldt", [128, 4], F32)
    bre = P.sb("bre", [128, 4, 16], F32)
    bim = P.sb("bim", [128, 4, 16], F32)
    cre = P.sb("cre", [128, 4, 16], F32)
    cim = P.sb("cim", [128, 4, 16], F32)
    dcol = P.sb("dcol", [128, 1], F32)
    for t, nm, src in ((lr, "lr", "lam_re_c"), (li, "li", "lam_im_c"), (ldt, "ldt", "log_dt_c"), (bre, "bre", "b_re_c"), (bim, "bim", "b_im_c"),
                       (cre, "cre", "c_reT_c"), (cim, "cim", "c_imT_c"), (dcol, "dcol", "d_c")):
        P.dma("sp", t[:], dr[src], writes=[nm])
    V = lambda name: P.sb(name, [128, 4], F32)
    dt, x, mag, th, cth, sth, ar, ai, den, cfr, cfi, t1, t2 = [V(n) for n in ("dt", "x", "mag", "th", "cth", "sth", "ar", "ai", "den", "cfr", "cfi", "t1", "t2")]
    thT = V("thT")
    cTc = P.sbm("cTc", [128, 4], F32)
    sTc = P.sbm("sTc", [128, 4], F32)

    def ts(out, in0, s1, s2, op0, op1=None, rd=(), wr=()):
        if op1 is None:
            P.op("dve", lambda e: e.tensor_scalar(out=out, in0=in0, scalar1=s1, scalar2=None, op0=op0), reads=rd, writes=wr)
        else:
            P.op("dve", lambda e: e.tensor_scalar(out=out, in0=in0, scalar1=s1, scalar2=s2, op0=op0, op1=op1), reads=rd, writes=wr)

    def tt(out, a, b, op, rd=(), wr=(), eng="dve"):
        P.op(eng, lambda e: e.tensor_tensor(out=out, in0=a, in1=b, op=op), reads=rd, writes=wr)

    P.op("act", lambda e: e.activation(out=dt[:], in_=ldt[:], func=AF.Exp), reads=["ldt"], writes=["dt"])
    tt(x[:], lr[:], dt[:], ALU.mult, ["lr", "dt"], ["x"])
    ts(mag[:], x[:], 1.0 / 720, 1.0 / 120, ALU.mult, ALU.add, ["x"], ["mag"])
    for cf in (1.0 / 24, 1.0 / 6, 0.5, 1.0, 1.0):
        tt(mag[:], mag[:], x[:], ALU.mult, ["mag", "x"], ["mag"])
        ts(mag[:], mag[:], cf, None, ALU.add, None, ["mag"], ["mag"])
    tt(th[:], li[:], dt[:], ALU.mult, ["li", "dt"], ["th"])
    sc_s = sincos_alloc(P, [128, 4], "ssm4")
    emit_sincos(P, sc_s, th[:], "th", cth[:], "cth", sth[:], "sth", "ssm4")
    ts(thT[:], th[:], float(TC), None, ALU.mult, None, ["th"], ["thT"])
    emit_sincos(P, sc_s, thT[:], "thT", cTc[:], "cTc", sTc[:], "sTc", "ssm4")
    tt(ar[:], mag[:], cth[:], ALU.mult, ["mag", "cth"], ["ar"])
    tt(ai[:], mag[:], sth[:], ALU.mult, ["mag", "sth"], ["ai"])
    tt(den[:], lr[:], lr[:], ALU.mult, ["lr"], ["den"])
    tt(t1[:], li[:], li[:], ALU.mult, ["li"], ["t1"])
    tt(den[:], den[:], t1[:], ALU.add, ["den", "t1"], ["den"])
    P.op("dve", lambda e: e.reciprocal(out=den[:], in_=den[:]), reads=["den"], writes=["den"])
    ts(ar[:], ar[:], -1.0, None, ALU.add, None, ["ar"], ["ar"])
    tt(t1[:], ar[:], lr[:], ALU.mult, ["ar", "lr"], ["t1"])
    tt(t2[:], ai[:], li[:], ALU.mult, ["ai", "li"], ["t2"])
    tt(cfr[:], t1[:], t2[:], ALU.add, ["t1", "t2"], ["cfr"])
    tt(cfr[:], cfr[:], den[:], ALU.mult, ["cfr", "den"], ["cfr"])
    tt(t1[:], ai[:], lr[:], ALU.mult, ["ai", "lr"], ["t1"])
    tt(t2[:], ar[:], li[:], ALU.mult, ["ar", "li"], ["t2"])
    tt(cfi[:], t1[:], t2[:], ALU.subtract, ["t1", "t2"], ["cfi"])
    tt(cfi[:], cfi[:], den[:], ALU.mult, ["cfi", "den"], ["cfi"])
    BTr = P.sbm("BTr", [128, 4, 128], BF16)
    BTi = P.sbm("BTi", [128, 4, 128], BF16)
    CTr = P.sbm("CTr", [128, 4, 128], BF16)
    CTi = P.sbm("CTi", [128, 4, 128], BF16)
    Dd = P.sbm("Dd", [128, 128], BF16)
    bfull = P.sb("bfull", [128, 128], F32)
    b16a = P.sb("b16a", [128, 16], F32)
    b16b = P.sb("b16b", [128, 16], F32)
    psT = P.ps("psT", [128, 512])
    P.op("dve", lambda e: e.memset(CTr[:], 0.0), writes=["CTr"])
    P.op("dve", lambda e: e.memset(CTi[:], 0.0), writes=["CTi"])
    P.op("dve", lambda e: e.tensor_scalar(out=Dd[:], in0=ident[:], scalar1=dcol[:, 0:1], scalar2=None, op0=ALU.mult), reads=["ident", "dcol"], writes=["Dd"])
    for i in range(4):
        for (dst, sa, ca, sb_, cb_, opc) in ((BTr, bre, cfr, bim, cfi, ALU.subtract), (BTi, bim, cfr, bre, cfi, ALU.add)):
            ts(b16a[:], sa[:, i, :], ca[:, i:i + 1], None, ALU.mult, None, ["bre", "bim", "cfr"], ["b16a"])
            ts(b16b[:], sb_[:, i, :], cb_[:, i:i + 1], None, ALU.mult, None, ["bre", "bim", "cfi"], ["b16b"])
            tt(b16a[:], b16a[:], b16b[:], opc, ["b16a", "b16b"], ["b16a"])
            P.op("dve", lambda e: e.memset(bfull[:], 0.0), writes=["bfull"])
            P.op("dve", lambda e, i=i: e.tensor_copy(out=bfull[0:64, 32 * i:32 * i + 16], in_=b16a[0:64, :]), reads=["b16a"], writes=["bfull"])
            P.op("dve", lambda e, i=i: e.tensor_copy(out=bfull[64:128, 32 * i + 16:32 * i + 32], in_=b16a[64:128, :]), reads=["b16a"], writes=["bfull"])
            P.op("pe", lambda e: e.transpose(psT[:, :128], bfull[:], ident[:]), reads=["bfull", "ident"], writes=["psT"])
            P.op("act", lambda e, dst=dst, i=i: e.copy(out=dst[:, i, :], in_=psT[:, :128]), reads=["psT"], writes=[("BTr" if dst is BTr else "BTi")])
        P.op("dve", lambda e, i=i: e.tensor_copy(out=CTr[0:64, i, 32 * i:32 * i + 16], in_=cre[0:64, i, :]), reads=["cre"], writes=["CTr"])
        P.op("dve", lambda e, i=i: e.tensor_copy(out=CTr[64:128, i, 32 * i + 16:32 * i + 32], in_=cre[64:128, i, :]), reads=["cre"], writes=["CTr"])
        P.op("dve", lambda e, i=i: e.tensor_scalar(out=CTi[0:64, i, 32 * i:32 * i + 16], in0=cim[0:64, i, :], scalar1=-1.0, scalar2=None, op0=ALU.mult),
             reads=["cim"], writes=["CTi"])
        P.op("dve", lambda e, i=i: e.tensor_scalar(out=CTi[64:128, i, 32 * i + 16:32 * i + 32], in0=cim[64:128, i, :], scalar1=-1.0, scalar2=None, op0=ALU.mult),
             reads=["cim"], writes=["CTi"])
    posi = P.sb("posi", [128, TC], I32)
    posf = P.sb("posf", [128, TC], F32)
    angj = P.sb("angj", [128, TC], F32)
    P.op("pool", lambda e: e.iota(posi[:], pattern=[[1, TC]], base=0, channel_multiplier=0), writes=["posi"])
    P.op("dve", lambda e: e.tensor_copy(out=posf[:], in_=posi[:]), reads=["posi"], writes=["posf"])
    sc_t = sincos_alloc(P, [128, TC], "ssmT")
    cosJ = [P.sbm("cosJ%d" % i, [128, TC], F32) for i in range(4)]
    sinJ = [P.sbm("sinJ%d" % i, [128, TC], F32) for i in range(4)]
    rfull = [P.sbm("rfull%d" % i, [128, TC], F32) for i in range(4)]
    for i in range(4):
        ts(angj[:], posf[:], th[:, i:i + 1], None, ALU.mult, None, ["posf", "th"], ["angj"])
        emit_sincos(P, sc_t, angj[:], "angj", cosJ[i][:], "cosJ%d" % i, sinJ[i][:], "sinJ%d" % i, "ssmT")
        P.op("dve", lambda e, i=i: e.memset(rfull[i][:], 1.0), writes=["rfull%d" % i])
        ts(rfull[i][:], rfull[i][:], mag[:, i:i + 1], None, ALU.mult, None, ["rfull%d" % i, "mag"], ["rfull%d" % i])
    return dict(BTr=BTr, BTi=BTi, CTr=CTr, CTi=CTi, Dd=Dd, cosJ=cosJ, sinJ=sinJ, rfull=rfull, cTc=cTc, sTc=sTc)


def ssm_steps(P, dr, uT, T):
    ssmT_out = dr["ssmT_out"]
    BTr, BTi, CTr, CTi, Dd = T["BTr"], T["BTi"], T["CTr"], T["CTi"], T["Dd"]
    cosJ, sinJ, rfull, cTc, sTc = T["cosJ"], T["sinJ"], T["rfull"], T["cTc"], T["sTc"]

    def ts(out, in0, s1, s2, op0, op1=None, rd=(), wr=()):
        if op1 is None:
            P.op("dve", lambda e: e.tensor_scalar(out=out, in0=in0, scalar1=s1, scalar2=None, op0=op0), reads=rd, writes=wr)
        else:
            P.op("dve", lambda e: e.tensor_scalar(out=out, in0=in0, scalar1=s1, scalar2=s2, op0=op0, op1=op1), reads=rd, writes=wr)

    def tt(out, a, b, op, rd=(), wr=(), eng="dve"):
        P.op(eng, lambda e: e.tensor_tensor(out=out, in0=a, in1=b, op=op), reads=rd, writes=wr)

    br32 = [P.sb("br32_%d" % i, [128, TC], F32) for i in range(2)]
    bi32 = [P.sb("bi32_%d" % i, [128, TC], F32) for i in range(2)]
    m1 = P.sb("m1", [128, TC], F32)
    m2 = P.sb("m2", [128, TC], F32)
    m3 = P.sb("m3", [128, TC], F32)
    m4 = P.sb("m4", [128, TC], F32)
    rbr = P.sb("rbr", [128, TC], F32)
    rbi = P.sb("rbi", [128, TC], F32)
    wre = [P.sb("wre%d" % i, [128, TC], F32) for i in range(4)]
    wim = [P.sb("wim%d" % i, [128, TC], F32) for i in range(4)]
    xre = [P.sb("xre%d" % i, [128, TC], BF16) for i in range(2)]
    xim = [P.sb("xim%d" % i, [128, TC], BF16) for i in range(2)]
    ini = P.sb("ini", [128, 4, 2], F32)
    itmp = P.sb("itmp", [128, 2], F32)
    gx = [P.sb("gx%d" % i, [128, TC], F32) for i in range(2)]
    g2 = [P.sb("g2%d" % i, [128, TC], F32) for i in range(2)]
    gs = [P.sb("gs%d" % i, [128, TC], F32) for i in range(2)]
    yo = [P.sb("yo%d" % i, [128, TC], F32) for i in range(2)]
    psB = P.ps("psB", [128, 2, TC])
    py = P.ps("psYs", [128, 512])
    pyr = "psYs"
    P.op("dve", lambda e: e.memset(ini[:], 0.0), writes=["ini"])
    steps = [(c, i) for c in range(NCHS) for i in range(4)]
    NS = len(steps)

    def stA(k):
        c, i = steps[k]
        t0, n = chunkS(c)
        b = k % 2
        P.op("pe", lambda e, i=i, t0=t0, n=n: e.matmul(psB[:, 0, :n], BTr[:, i, :], uT[:, t0:t0 + n], start=True, stop=True), reads=["BTr", "uT"], writes=["psB"])
        P.op("pe", lambda e, i=i, t0=t0, n=n: e.matmul(psB[:, 1, :n], BTi[:, i, :], uT[:, t0:t0 + n], start=True, stop=True, skip_group_check=True), reads=["BTi", "uT"], writes=["psB"])
        P.op("act", lambda e, n=n, b=b: e.copy(out=br32[b][:, :n], in_=psB[:, 0, :n]), reads=["psB"], writes=["br32_%d" % b])
        P.op("act", lambda e, n=n, b=b: e.copy(out=bi32[b][:, :n], in_=psB[:, 1, :n]), reads=["psB"], writes=["bi32_%d" % b])

    def stB(k):
        c, i = steps[k]
        t0, n = chunkS(c)
        b = k % 2
        brb, bib = br32[b], bi32[b]
        brn, bin_ = "br32_%d" % b, "bi32_%d" % b
        cj, sj = cosJ[i], sinJ[i]
        cr, sr = "cosJ%d" % i, "sinJ%d" % i
        tt(m1[:, :n], cj[:, :n], brb[:, :n], ALU.mult, [cr, brn], ["m1"], "dve")
        tt(m2[:, :n], sj[:, :n], bib[:, :n], ALU.mult, [sr, bin_], ["m2"], "pool")
        tt(m3[:, :n], cj[:, :n], bib[:, :n], ALU.mult, [cr, bin_], ["m3"], "pool")
        tt(m4[:, :n], sj[:, :n], brb[:, :n], ALU.mult, [sr, brn], ["m4"], "dve")
        tt(rbr[:, :n], m1[:, :n], m2[:, :n], ALU.add, ["m1", "m2"], ["rbr"], "dve")
        tt(rbi[:, :n], m3[:, :n], m4[:, :n], ALU.subtract, ["m3", "m4"], ["rbi"], "pool")
        wr_, wi_ = wre[i], wim[i]
        wrn, win = "wre%d" % i, "wim%d" % i
        if c > 0:
            ts(itmp[:, 0:1], wi_[:, TC - 1:TC], sTc[:, i:i + 1], None, ALU.mult, None, [win, "sTc"], ["itmp"])
            P.op("dve", lambda e, i=i, wr_=wr_: e.scalar_tensor_tensor(out=ini[:, i, 0:1], in0=wr_[:, TC - 1:TC], scalar=cTc[:, i:i + 1], in1=itmp[:, 0:1],
                                                                      op0=ALU.mult, op1=ALU.subtract), reads=[wrn, "cTc", "itmp"], writes=["ini"])
            ts(itmp[:, 1:2], wr_[:, TC - 1:TC], sTc[:, i:i + 1], None, ALU.mult, None, [wrn, "sTc"], ["itmp"])
            P.op("dve", lambda e, i=i, wi_=wi_: e.scalar_tensor_tensor(out=ini[:, i, 1:2], in0=wi_[:, TC - 1:TC], scalar=cTc[:, i:i + 1], in1=itmp[:, 1:2],
                                                                      op0=ALU.mult, op1=ALU.add), reads=[win, "cTc", "itmp"], writes=["ini"])
        P.op("dve", lambda e, i=i, wr_=wr_, n=n: e.tensor_tensor_scan(out=wr_[:, :n], data0=rfull[i][:, :n], data1=rbr[:, :n], initial=ini[:, i, 0:1],
                                                                     op0=ALU.mult, op1=ALU.add), reads=["rfull%d" % i, "rbr", "ini"], writes=[wrn])
        P.op("dve", lambda e, i=i, wi_=wi_, n=n: e.tensor_tensor_scan(out=wi_[:, :n], data0=rfull[i][:, :n], data1=rbi[:, :n], initial=ini[:, i, 1:2],
                                                                     op0=ALU.mult, op1=ALU.add), reads=["rfull%d" % i, "rbi", "ini"], writes=[win])
        xr, xi = xre[b], xim[b]
        xrn, xin = "xre%d" % b, "xim%d" % b
        tt(m1[:, :n], cj[:, :n], wr_[:, :n], ALU.mult, [cr, wrn], ["m1"], "dve")
        tt(m2[:, :n], sj[:, :n], wi_[:, :n], ALU.mult, [sr, win], ["m2"], "pool")
        tt(m3[:, :n], sj[:, :n], wr_[:, :n], ALU.mult, [sr, wrn], ["m3"], "pool")
        tt(m4[:, :n], cj[:, :n], wi_[:, :n], ALU.mult, [cr, win], ["m4"], "dve")
        tt(xr[:, :n], m1[:, :n], m2[:, :n], ALU.subtract, ["m1", "m2"], [xrn], "dve")
        tt(xi[:, :n], m3[:, :n], m4[:, :n], ALU.add, ["m3", "m4"], [xin], "pool")

    def stC(k):
        c, i = steps[k]
        t0, n = chunkS(c)
        b = k % 2
        xr, xi = xre[b], xim[b]
        xrn, xin = "xre%d" % b, "xim%d" % b
        P.op("pe", lambda e, i=i, xr=xr, n=n: e.matmul(py[:, :n], CTr[:, i, :], xr[:, :n], start=(i == 0), stop=False), reads=["CTr", xrn], writes=[pyr])
        P.op("pe", lambda e, i=i, xi=xi, n=n: e.matmul(py[:, :n], CTi[:, i, :], xi[:, :n], start=False, stop=False), reads=["CTi", xin], writes=[pyr])
        if i == 3:
            g = c % 2
            P.op("pe", lambda e, t0=t0, n=n: e.matmul(py[:, :n], Dd[:], uT[:, t0:t0 + n], start=False, stop=True), reads=["Dd", "uT"], writes=[pyr])
            P.op("act", lambda e, n=n, g=g: e.copy(out=gx[g][:, :n], in_=py[:, :n]), reads=[pyr], writes=["gx%d" % g])
            P.op("act", lambda e, n=n, g=g: e.activation(out=g2[g][:, :n], in_=py[:, :n], func=AF.Square), reads=[pyr], writes=["g2%d" % g])

    def stD2(c):
        t0, n = chunkS(c)
        g = c % 2
        ts(g2[g][:, :n], g2[g][:, :n], 0.044715, 1.0, ALU.mult, ALU.add, ["g2%d" % g], ["g2%d" % g])
        tt(g2[g][:, :n], g2[g][:, :n], gx[g][:, :n], ALU.mult, ["g2%d" % g, "gx%d" % g], ["g2%d" % g], "pool")

    def stD3(c):
        t0, n = chunkS(c)
        g = c % 2
        P.op("act", lambda e, n=n, g=g: e.activation(out=gs[g][:, :n], in_=g2[g][:, :n], func=AF.Sigmoid, scale=1.5957691216057308), reads=["g2%d" % g], writes=["gs%d" % g])

    def stD4(c):
        t0, n = chunkS(c)
        g = c % 2
        tt(yo[g][:, :n], gx[g][:, :n], gs[g][:, :n], ALU.mult, ["gx%d" % g, "gs%d" % g], ["yo%d" % g], "pool")
        nv = min(n, L - t0)
        if nv > 0:
            P.dma("sp", ssmT_out[:, t0:t0 + nv], yo[g][:, :nv], reads=["yo%d" % g], writes=["dram:ssmT_out"])

    tails = {}
    t = 0
    while t < NS + 2 or any(k >= t for k in tails):
        if t < NS:
            stA(t)
        if 0 <= t - 1 < NS:
            stB(t - 1)
        if 0 <= t - 2 < NS:
            stC(t - 2)
            c, i = steps[t - 2]
            if i == 3:
                tails.setdefault(t + 1, []).append(lambda c=c: (stD2(c), stD3(c)))
                tails.setdefault(t + 2, []).append(lambda c=c: stD4(c))
        for f in tails.pop(t, []):
            f()
        t += 1
        yield


def attn_steps(P, dr, qT, kT, Va, ident16, lam_init):
    nc = P.nc
    attn_out = dr["attn_out"]
    scale = HD ** -0.5
    nm_d = P.sb("nm_d", [128, 128], BF16)
    nm_n = P.sb("nm_n", [128, 128], BF16)
    P.op("dve", lambda e: e.memset(nm_d[:], NEG), writes=["nm_d"])
    P.op("dve", lambda e: e.memset(nm_d[0:16, :], 0.0), writes=["nm_d"])
    P.op("dve", lambda e: e.memset(nm_d[0:80, 16:128], 0.0), writes=["nm_d"])
    P.op("dve", lambda e: e.memset(nm_d[:, 80:128], 0.0), writes=["nm_d"])
    P.op("dve", lambda e: e.memset(nm_n[:], NEG), writes=["nm_n"])
    mhalf = P.sb("mhalf", [128, 1], F32)
    P.op("dve", lambda e: e.memset(mhalf[:], -0.5), writes=["mhalf"])
    P.op("dve", lambda e: e.memset(nm_n[0:16, 80:128], 0.0), writes=["nm_n"])
    lq = P.sb("lq", [128, 4, 64], F32)
    P.dma("sp", lq[:], dr["lam_qk_bc"], writes=["lq"])
    lp = P.sb("lp", [128, 2, 64], F32)
    lsum = P.sb("lsum", [128, 2], F32)
    nlam = P.sb("nlam", [128, 1], F32)
    P.op("dve", lambda e: e.tensor_tensor(out=lp[:, 0, :], in0=lq[:, 0, :], in1=lq[:, 1, :], op=ALU.mult), reads=["lq"], writes=["lp"])
    P.op("dve", lambda e: e.tensor_tensor(out=lp[:, 1, :], in0=lq[:, 2, :], in1=lq[:, 3, :], op=ALU.mult), reads=["lq"], writes=["lp"])
    P.op("dve", lambda e: e.reduce_sum(out=lsum[:], in_=lp[:], axis=AX.X), reads=["lp"], writes=["lsum"])
    P.op("act", lambda e: e.activation(out=lsum[:], in_=lsum[:], func=AF.Exp), reads=["lsum"], writes=["lsum"])
    P.op("dve", lambda e: e.tensor_tensor(out=nlam[:], in0=lsum[:, 1:2], in1=lsum[:, 0:1], op=ALU.subtract), reads=["lsum"], writes=["nlam"])
    P.op("dve", lambda e: e.tensor_scalar(out=nlam[:], in0=nlam[:], scalar1=-lam_init, scalar2=None, op0=ALU.add), reads=["nlam"], writes=["nlam"])
    gsc = P.sb("gsc", [128, 128], F32)
    P.dma("sp", gsc[:], dr["subln_g_bc"], writes=["gsc"])
    P.op("dve", lambda e: e.tensor_scalar(out=gsc[:], in0=gsc[:], scalar1=1.0 - lam_init, scalar2=None, op0=ALU.mult), reads=["gsc"], writes=["gsc"])
    psSS = [P.ps("psSS_%d" % b, [128, 2, 512]) for b in range(2)]
    psS = [[psSS[b][:, c, :] for b in range(2)] for c in range(2)]
    psO1 = [P.ps("psO%d" % c, [128, NSUB, 129]) for c in range(2)]
    sbO = [[P.sb("sbO%d_%d" % (c, b), [128, NSUB, 129], F32) for b in range(2)] for c in range(2)]
    PtP = [P.sb("PtP_%d" % b, [128, 2, NSUB * 128], BF16) for b in range(2)]
    Pt = [[PtP[b][:, c, :] for b in range(2)] for c in range(2)]
    sq16 = P.sb("sq16", [128, 512], BF16)
    ones_c = [P.sb("ones_c%d" % c, [128, 128], BF16) for c in range(2)]
    for c in range(2):
        P.op("dve", lambda e, c=c: e.memset(ones_c[c][:], 0.0), writes=["ones_c%d" % c])
        P.op("dve", lambda e, c=c: e.memset(ones_c[c][c * 64:(c + 1) * 64, :], 1.0), writes=["ones_c%d" % c])
    mx = P.sb("mx", [128, 2, 2, NCHA], F32)
    mxr = P.sb("mxr", [128, 2, 2], F32)
    negM = P.sb("negM", [128, 2], F32)
    negMc = P.sb("negMc", [128, 1], F32)
    r0 = P.sb("r0", [128, 1], F32)
    r1 = P.sb("r1", [128, 1], F32)
    ot = P.sb("ot", [128, 128], F32)
    junk = P.sb("junk", [128, 128], F32)
    ss = P.sb("ss", [128, 1], F32)
    ob = [P.sb("ob%d" % i, [128, 128], F32) for i in range(2)]
    nsb = (NBLK + NSUB - 1) // NSUB
    sbuf_i = 0
    obi = 0
    for h in range(2):
        for which, src, sres in ((0, qT[h], "qT%d" % h), (1, kT[h], "kT%d" % h)):
            for c in range(NCHA):
                t0, n = chunkA(c)
                P.op("dve", lambda e, src=src, t0=t0, n=n: e.tensor_tensor(out=sq16[:, :n], in0=src[:, t0:t0 + n], in1=src[:, t0:t0 + n], op=ALU.mult),
                     reads=[sres], writes=["sq16"])
                for m in range(2):
                    ps = psS[m][0]
                    P.op("pe", lambda e, m=m, ps=ps, n=n: e.matmul(ps[:, :n], ones_c[m][:], sq16[:, :n], start=True, stop=True),
                         reads=["ones_c%d" % m, "sq16"], writes=["psS%d_0" % m])
                    P.op("dve", lambda e, m=m, ps=ps, n=n, which=which, c=c: e.reduce_max(out=mx[:, which, m, c:c + 1], in_=ps[:, :n], axis=AX.X),
                         reads=["psS%d_0" % m], writes=["mx"])
        P.op("dve", lambda e: e.reduce_max(out=mxr[:], in_=mx[:], axis=AX.X), reads=["mx"], writes=["mxr"])
        P.op("dve", lambda e: e.tensor_tensor(out=negM[:], in0=mxr[:, 0, :], in1=mxr[:, 1, :], op=ALU.add), reads=["mxr"], writes=["negM"])
        P.op("dve", lambda e: e.tensor_scalar(out=negM[:], in0=negM[:], scalar1=-0.5 * scale, scalar2=None, op0=ALU.mult), reads=["negM"], writes=["negM"])
        P.op("dve", lambda e: e.tensor_tensor(out=negMc[:], in0=negM[:, 0:1], in1=negM[:, 1:2], op=ALU.min), reads=["negM"], writes=["negMc"])
        if "dbg_q" in dr and h == 0:
            P.dma("sp", dr["dbg_q"], qT[0][:], reads=["qT0"], writes=["dram:dbg_q"], semkey="dbg1")
            P.dma("sp", dr["dbg_k"], kT[0][:], reads=["kT0"], writes=["dram:dbg_k"], semkey="dbg2")
            P.dma("sp", dr["dbg_v"], Va[0][:], reads=["Va0"], writes=["dram:dbg_v"], semkey="dbg3")
            P.dma("sp", dr["dbg_m"], negM[:], reads=["negM"], writes=["dram:dbg_m"], semkey="dbg4")
            P.dma("sp", dr["dbg_l"], nlam[:], reads=["nlam"], writes=["dram:dbg_l"], semkey="dbg5")
        for I in range(nsb):
            i0 = I * NSUB
            nq = min(NSUB, NBLK - i0)
            q0 = i0 * 128
            jmax = min(i0 + nq, NBLK - 1)
            ab = sbuf_i % 2
            sbuf_i += 1
            units = [(j, c) for j in range(jmax + 1) for c in range(2)]

            def geom(j):
                s_lo = max(0, j - 1 - i0)
                return s_lo, s_lo * 128, nq * 128

            def emit_S(u, h=h, i0=i0, nq=nq, q0=q0):
                j, c = units[u]
                s_lo, c0, c1 = geom(j)
                S = psS[c][j % 2]
                sres = "psS%d_%d" % (c, j % 2)
                masks = [(s, nm_d if i0 + s == j else nm_n) for s in range(s_lo, nq) if i0 + s in (j, j - 1)]
                P.op("pe", lambda e, c=c, S=S, j=j, c0=c0, c1=c1, q0=q0, h=h, last=(len(masks) == 0): e.matmul(
                    S[:, c0:c1], kT[h][c * 64:(c + 1) * 64, j * 128:(j + 1) * 128], qT[h][c * 64:(c + 1) * 64, q0 + c0:q0 + c1], start=True, stop=last, skip_group_check=True),
                    reads=["kT%d" % h, "qT%d" % h], writes=[sres])
                for mi, (s, nm) in enumerate(masks):
                    P.op("pe", lambda e, S=S, s=s, nm=nm, last=(mi == len(masks) - 1): e.matmul(S[:, s * 128:(s + 1) * 128], ident16[:], nm[:], start=False, stop=last, skip_group_check=True),
                         reads=["ident16", "nm_d", "nm_n"], writes=[sres])

            def emit_E(u):
                j, c = units[u]
                s_lo, c0, c1 = geom(j)
                b = j % 2
                P.op("act", lambda e, b=b, c0=c0, c1=c1: e.activation(out=PtP[b][:, :, c0:c1], in_=psSS[b][:, :, c0:c1], func=AF.Exp, scale=scale, bias=negMc[:, 0:1]),
                     reads=["psS0_%d" % b, "psS1_%d" % b, "negMc"], writes=["Pt0_%d" % b, "Pt1_%d" % b])

            def emit_PV(u, h=h, ab=ab, nq=nq):
                j, c = units[u]
                s_lo, c0, c1 = geom(j)
                pt = Pt[c][j % 2]
                for s in range(s_lo, nq):
                    P.op("pe", lambda e, c=c, s=s, pt=pt, j=j, h=h, first=(j == 0 and s == s_lo): e.matmul(
                        psO1[c][:, s, :], pt[:, s * 128:(s + 1) * 128], Va[h][:, j, 0:129], start=first, stop=True, skip_group_check=True),
                         reads=["Pt%d_%d" % (c, j % 2), "Va%d" % h], writes=["psO%d" % c])

            emit_S(0)
            emit_S(1)
            for u in range(0, len(units), 2):
                emit_E(u)
                if u + 2 < len(units):
                    emit_S(u + 2)
                    emit_S(u + 3)
                emit_PV(u)
                emit_PV(u + 1)
                yield
            for c in range(2):
                P.op("act", lambda e, c=c, ab=ab: e.copy(out=sbO[c][ab][:], in_=psO1[c][:]), reads=["psO%d" % c], writes=["sbO%d_%d" % (c, ab)])
            psO = [[sbO[0][0], sbO[0][1]], [sbO[1][0], sbO[1][1]]]
            for s in range(nq):
                blk = i0 + s
                o0 = "sbO0_%d" % ab
                o1 = "sbO1_%d" % ab
                P.op("dve", lambda e, ab=ab, s=s: e.reciprocal(out=r0[:], in_=psO[0][ab][:, s, 128:129]), reads=[o0], writes=["r0"])
                P.op("dve", lambda e, ab=ab, s=s: e.reciprocal(out=r1[:], in_=psO[1][ab][:, s, 128:129]), reads=[o1], writes=["r1"])
                P.op("dve", lambda e: e.tensor_tensor(out=r1[:], in0=r1[:], in1=nlam[:], op=ALU.mult), reads=["r1", "nlam"], writes=["r1"])
                P.op("dve", lambda e, ab=ab, s=s: e.tensor_scalar(out=ot[:], in0=psO[0][ab][:, s, 0:128], scalar1=r0[:, 0:1], scalar2=None, op0=ALU.mult),
                     reads=[o0, "r0"], writes=["ot"])
                P.op("dve", lambda e, ab=ab, s=s: e.scalar_tensor_tensor(out=ot[:], in0=psO[1][ab][:, s, 0:128], scalar=r1[:, 0:1], in1=ot[:], op0=ALU.mult, op1=ALU.add),
                     reads=[o1, "r1", "ot"], writes=["ot"])
                P.op("dve", lambda e: e.scalar_tensor_tensor(out=junk[:], in0=ot[:], scalar=1.0, in1=ot[:], op0=ALU.mult, op1=ALU.mult, accum_out=ss[:]),
                     reads=["ot"], writes=["junk", "ss"])
                P.op("dve", lambda e: e.tensor_scalar(out=ss[:], in0=ss[:], scalar1=1.0 / VD, scalar2=RMS_EPS, op0=ALU.mult, op1=ALU.add), reads=["ss"], writes=["ss"])
                P.op("pool", lambda e: e.tensor_tensor(out=ss[:], in0=ss[:], in1=mhalf[:], op=ALU.pow), reads=["ss", "mhalf"], writes=["ss"])
                o = ob[obi % 2]
                ores = "ob%d" % (obi % 2)
                obi += 1
                P.op("dve", lambda e, o=o: e.scalar_tensor_tensor(out=o[:], in0=ot[:], scalar=ss[:, 0:1], in1=gsc[:], op0=ALU.mult, op1=ALU.mult),
                     reads=["ot", "ss", "gsc"], writes=[ores])
                P.dma("sp", attn_out[blk * 128:(blk + 1) * 128, h * 128:(h + 1) * 128], o[:], reads=[ores], writes=["dram:attn_out"])


A_INPUTS = [("hT", [D, LP]), ("w_sel", [D, 1280]), ("inv_c", [128, 1]), ("conv_w_c", [128, 3]),
            ("lam_re_c", [128, 4]), ("lam_im_c", [128, 4]), ("log_dt_c", [128, 4]), ("b_re_c", [128, 4, 16]), ("b_im_c", [128, 4, 16]),
            ("c_reT_c", [128, 4, 16]), ("c_imT_c", [128, 4, 16]), ("d_c", [128, 1]), ("lam_qk_bc", [128, 4, 64]), ("subln_g_bc", [128, 128])]


def build_A(layer, debug=False):
    nc = bass.Bass("TRN2", target_bir_lowering=False)
    dr = {name: nc.dram_tensor(name, shape, F32, kind="ExternalInput").ap() for name, shape in A_INPUTS}
    if debug:
        dr["dbg_q"] = nc.dram_tensor("dbg_q", [128, LP], BF16, kind="ExternalOutput").ap()
        dr["dbg_k"] = nc.dram_tensor("dbg_k", [128, LP], BF16, kind="ExternalOutput").ap()
        dr["dbg_v"] = nc.dram_tensor("dbg_v", [128, NBLK, 130], BF16, kind="ExternalOutput").ap()
        dr["dbg_m"] = nc.dram_tensor("dbg_m", [128, 2], F32, kind="ExternalOutput").ap()
        dr["dbg_l"] = nc.dram_tensor("dbg_l", [128, 1], F32, kind="ExternalOutput").ap()
    dr["ssmT_out"] = nc.dram_tensor("ssmT_out", [128, L], F32, kind="ExternalOutput").ap()
    dr["convT_out"] = nc.dram_tensor("convT_out", [128, L], F32, kind="ExternalOutput").ap()
    dr["attn_out"] = nc.dram_tensor("attn_out", [LP, 256], F32, kind="ExternalOutput").ap()
    P = Prog(nc)
    emit_A(P, dr, 0.8 - 0.6 * math.exp(-0.3 * layer))
    P.emit()
    return nc


def host_A_inputs(inp, l, core, hT_b):
    f = np.float32
    j = core % 4
    w_in = np.asarray(inp["w_in"][l], f)
    s0, s1, s2, s3 = SSM_W, SSM_W + CONV_W, SSM_W + 2 * CONV_W, SSM_W + 3 * CONV_W
    s4, s5 = s3 + ATT_W, s3 + 2 * ATT_W
    cols = [np.arange(j * 128, (j + 1) * 128)]
    for base in (s0, s1, s2):
        cols.append(base + np.arange(j * 128, (j + 1) * 128))
    for base in (s3, s4, s5):
        cols.append(base + np.arange(j * 256, (j + 1) * 256))
    cols = np.concatenate(cols)
    g0 = 8 * j

    def st(a):
        a = np.asarray(a, f)[g0:g0 + 8]
        return np.ascontiguousarray(a.reshape(4, 2, 64).transpose(1, 2, 0).reshape(128, 4))
    ldt = np.repeat(np.asarray(inp["ssm_log_dt"][l], f)[:, None], 64, axis=1)

    def sb3(a):
        a = np.asarray(a, f)[g0:g0 + 8]
        return np.ascontiguousarray(a.reshape(4, 2, 64, 16).transpose(1, 2, 0, 3).reshape(128, 4, 16))
    i = np.arange(128) % 64 % 32
    inv = (np.float32(ROPE_THETA) ** (-(2 * i).astype(np.float32) / np.float32(HD))).astype(f)
    lam_qk = np.stack([np.asarray(inp[k][l], f) for k in ("attn_lambda_q1", "attn_lambda_k1", "attn_lambda_q2", "attn_lambda_k2")])
    return {
        "hT": hT_b, "w_sel": np.ascontiguousarray(w_in[:, cols]), "inv_c": np.ascontiguousarray(inv[:, None]),
        "conv_w_c": np.ascontiguousarray(np.asarray(inp["conv_w"][l], f)[:, j * 128:(j + 1) * 128].T),
        "lam_re_c": st(inp["ssm_lambda_re"][l]), "lam_im_c": st(inp["ssm_lambda_im"][l]), "log_dt_c": st(ldt),
        "b_re_c": sb3(inp["ssm_b_re"][l]), "b_im_c": sb3(inp["ssm_b_im"][l]),
        "c_reT_c": sb3(np.asarray(inp["ssm_c_re"][l], f).transpose(0, 2, 1)), "c_imT_c": sb3(np.asarray(inp["ssm_c_im"][l], f).transpose(0, 2, 1)),
        "d_c": np.ascontiguousarray(np.asarray(inp["ssm_d"][l], f)[j * 128:(j + 1) * 128, None]),
        "lam_qk_bc": np.ascontiguousarray(np.broadcast_to(lam_qk[None], (128, 4, 64))),
        "subln_g_bc": np.ascontiguousarray(np.broadcast_to(np.asarray(inp["attn_subln_g"][l], f)[None], (128, 128))),
    }


def build_LN():
    nc = bass.Bass("TRN2", target_bir_lowering=False)
    xT = nc.dram_tensor("xT", [D, TOK_PER_CORE], F32, kind="ExternalInput").ap()
    g_c = nc.dram_tensor("g_c", [128, 8], F32, kind="ExternalInput").ap()
    b_c = nc.dram_tensor("b_c", [128, 8], F32, kind="ExternalInput").ap()
    oT = nc.dram_tensor("oT", [D, TOK_PER_CORE], F32, kind="ExternalOutput").ap()
    P = Prog(nc)
    N = CH
    xv = xT.rearrange("(k p) t -> p k t", p=128)
    ov = oT.rearrange("(k p) t -> p k t", p=128)
    lg = P.sb("lg", [128, 8], F32)
    lb = P.sb("lb", [128, 8], F32)
    P.dma("sp", lg[:], g_c, writes=["ln_gb_0"])
    P.dma("sp", lb[:], b_c, writes=["ln_gb_0"], semkey="lnb0")
    ones_f = P.sb("ones_f", [128, 128], F32)
    P.op("dve", lambda e: e.memset(ones_f[:], 1.0 / D), writes=["ones_f"])
    ln_alloc(P)
    x32 = [P.sb("x32_%d" % i, [128, 8, N], F32) for i in range(2)]
    o32 = [P.sb("o32_%d" % i, [128, 8, N], F32) for i in range(2)]
    for c in range(NCH):
        t0 = c * N
        xb, xr = x32[c % 2], "x32_%d" % (c % 2)
        ob, orr = o32[c % 2], "o32_%d" % (c % 2)
        P.dma("sp", xb[:], xv[:, :, t0:t0 + N], writes=[xr])
        emit_layernorm(P, xb, xr, N, ones_f, lg, lb, ob, orr, "0")
        P.dma("sp", ov[:, :, t0:t0 + N], ob[:], reads=[orr], writes=["dram:oT"])
    P.emit()
    return nc


_CACHE = {}


def _prog(key, fn):
    if key not in _CACHE:
        _CACHE[key] = fn()
    return _CACHE[key]


def kernel(**inp):
    f = np.float32
    x = np.asarray(inp["x"], f)
    meta = np.asarray(inp["meta_tokens"], f)
    hin = np.concatenate([np.broadcast_to(meta[None], (BATCH, N_META, D)), x], axis=1)
    xT_all = np.ascontiguousarray(hin.reshape(BATCH * L, D).T)
    cores = list(range(NCORES))
    T = TOK_PER_CORE
    maps = [{"xT": np.ascontiguousarray(xT_all[:, c * T:(c + 1) * T]), "g_c": col_layout(inp["ln_in_g"]), "b_c": col_layout(inp["ln_in_b"])} for c in cores]
    res = run_bass_kernel_spmd(_prog("ln", build_LN), maps, core_ids=cores)
    hT_all = np.concatenate([r["oT"] for r in res.results], axis=1)
    for l in range(DEPTH):
        maps = []
        for c in cores:
            b = c // 4
            hT = np.zeros((D, LP), f)
            hT[:, :L] = hT_all[:, b * L:(b + 1) * L]
            maps.append(host_A_inputs(inp, l, c, hT))
        res = run_bass_kernel_spmd(_prog(("A", l), lambda: build_A(l)), maps, core_ids=cores)
        ssmT = np.zeros((SSM_W, BATCH * L), f)
        convT = np.zeros((CONV_W, BATCH * L), f)
        attnT = np.zeros((ATT_W, BATCH * L), f)
        for c in cores:
            b, j = c // 4, c % 4
            r = res.results[c]
            ssmT[j * 128:(j + 1) * 128, b * L:(b + 1) * L] = r["ssmT_out"]
            convT[j * 128:(j + 1) * 128, b * L:(b + 1) * L] = r["convT_out"]
            attnT[j * 256:(j + 1) * 256, b * L:(b + 1) * L] = r["attn_out"][:L].T
        W = host_B_weights(inp, l)
        maps = []
        for c in cores:
            sl = slice(c * T, (c + 1) * T)
            m = dict(W)
            m.update(hT=np.ascontiguousarray(hT_all[:, sl]), ssmT=np.ascontiguousarray(ssmT[:, sl]),
                     convT=np.ascontiguousarray(convT[:, sl]), attnT=np.ascontiguousarray(attnT[:, sl]))
            maps.append(m)
        res = run_bass_kernel_spmd(_prog("B", build_B), maps, core_ids=cores)
        hT_all = np.concatenate([r["outT"] for r in res.results], axis=1)
    out = hT_all.T.reshape(BATCH, L, D)[:, N_META:]
    return np.ascontiguousarray(out, dtype=f)
```
